# Optimizing a Trainium2 kernel written in Bass

```python
import math
import jax
import jax.numpy as jnp
from jax import lax
import numpy as np

D_MODEL = 1024
BATCH = 16
SEQ = 2048
DEPTH = 2

GRID_W = 64
CTX_LEN = 256
NA_HEADS = 8
NA_HEAD_DIM = 64
NA_WIN_H = 8
NA_WIN_W = 16
DIFF_HEADS = 4
DIFF_DIM = 64
A_WIDTH = NA_HEADS * NA_HEAD_DIM
B_WIDTH = DIFF_HEADS * 2 * DIFF_DIM
ATTN_IN = 3 * (A_WIDTH + B_WIDTH)
Q_BLOCK = 128
ROPE_THETA = 10000.0
SC_WIDTH = 512
SC_K = 3
CF_WIDTH = 512
CF_K = 31
CONV_IN = 3 * SC_WIDTH + 2 * CF_WIDTH
N_EXPERTS = 16
D_EXPERT = 2816
CAPACITY_FACTOR = 2
EPS = 1e-6

kernel_name = 'hybrid_flow_backbone'


def rmsnorm(x, g):
    xf = x.astype(jnp.float32)
    xf = xf * lax.rsqrt(jnp.mean(xf * xf, axis=-1, keepdims=True) + EPS)
    return xf.astype(x.dtype) * g


def layernorm(x, g, b):
    xf = x.astype(jnp.float32)
    mu = jnp.mean(xf, axis=-1, keepdims=True)
    var = jnp.mean(jnp.square(xf - mu), axis=-1, keepdims=True)
    return ((xf - mu) * lax.rsqrt(var + EPS)).astype(x.dtype) * g + b


def modulate(h, shift, scale):
    return h * (1 + scale[..., None, :]) + shift[..., None, :]


def axial_rope(x, row, col):
    half = x.shape[-1] // 2
    n_freq = half // 2
    freqs = ROPE_THETA ** (-jnp.arange(n_freq, dtype=jnp.float32) / n_freq)
    bshape = (x.shape[1],) + (1,) * (x.ndim - 3) + (n_freq,)

    def rotate(xa, pos):
        ang = (pos.astype(jnp.float32)[:, None] * freqs).reshape(bshape)
        cos = jnp.cos(ang).astype(x.dtype)
        sin = jnp.sin(ang).astype(x.dtype)
        x1, x2 = xa[..., :n_freq], xa[..., n_freq:]
        return jnp.concatenate([x1 * cos - x2 * sin, x1 * sin + x2 * cos], axis=-1)

    return jnp.concatenate([rotate(x[..., :half], row), rotate(x[..., half:], col)], axis=-1)


def neighbourhood_attention(q, k, v, k_ctx, v_ctx, rpb):
    bn, n, h, d = q.shape
    rows = n // GRID_W
    kh = min(NA_WIN_H, rows)
    kw = NA_WIN_W
    scale = d ** -0.5
    qg = q.reshape(bn, rows, GRID_W, h, d)
    kg = k.reshape(bn, rows, GRID_W, h, d)
    vg = v.reshape(bn, rows, GRID_W, h, d)
    col = np.arange(GRID_W)
    col_idx = np.clip(col - kw // 2, 0, GRID_W - kw)[:, None] + np.arange(kw)[None, :]
    dc = col_idx - col[:, None] + (NA_WIN_W - 1)
    rpb_c = rpb[:, :, dc]

    def row_fn(r):
        rs = jnp.clip(r - kh // 2, 0, rows - kh)
        q_r = lax.dynamic_index_in_dim(qg, r, axis=1, keepdims=False)
        k_band = lax.dynamic_slice_in_dim(kg, rs, kh, axis=1)
        v_band = lax.dynamic_slice_in_dim(vg, rs, kh, axis=1)
        k_win = k_band[:, :, col_idx]
        v_win = v_band[:, :, col_idx]
        dr = rs + jnp.arange(kh) - r + (NA_WIN_H - 1)
        bias = jnp.take(rpb_c, dr, axis=1).transpose(0, 2, 1, 3)
        s_win = (jnp.einsum('bqhd,brqchd->bhqrc', q_r, k_win).astype(jnp.float32) * scale
                 + bias[None].astype(jnp.float32))
        s_ctx = jnp.einsum('bqhd,bkhd->bhqk', q_r, k_ctx).astype(jnp.float32) * scale
        s = jnp.concatenate([s_win.reshape(bn, h, GRID_W, kh * kw), s_ctx], axis=-1)
        p = jax.nn.softmax(s, axis=-1).astype(v.dtype)
        p_win = p[..., :kh * kw].reshape(bn, h, GRID_W, kh, kw)
        p_ctx = p[..., kh * kw:]
        return (jnp.einsum('bhqrc,brqchd->bqhd', p_win, v_win)
                + jnp.einsum('bhqk,bkhd->bqhd', p_ctx, v_ctx))

    out = lax.map(row_fn, jnp.arange(rows))
    return out.transpose(1, 0, 2, 3, 4).reshape(bn, n, h, d)


def differential_attention(q, k, v, k_ctx, v_ctx, lam, lam_init, subln):
    bn, n, h, _, d = q.shape
    scale = d ** -0.5
    k_all = jnp.concatenate([k, k_ctx], axis=1)
    v_all = jnp.concatenate([v, v_ctx], axis=1)
    qb = q.reshape(bn, n // Q_BLOCK, Q_BLOCK, h, 2, d).transpose(1, 0, 2, 3, 4, 5)

    def block_fn(q_blk):
        s = jnp.einsum('bqhmd,bkhmd->bhmqk', q_blk, k_all).astype(jnp.float32) * scale
        p = jax.nn.softmax(s, axis=-1)
        a = (p[:, :, 0] - lam * p[:, :, 1]).astype(v_all.dtype)
        return jnp.einsum('bhqk,bkhe->bqhe', a, v_all)

    out = lax.map(block_fn, qb).transpose(1, 0, 2, 3, 4).reshape(bn, n, h, 2 * d)
    return rmsnorm(out, subln) * (1.0 - lam_init)


def attention_mixers(h, hc, w_in, w_out, rpb, lam_q1, lam_k1, lam_q2, lam_k2, subln, lam_init):
    bn, n, _ = h.shape
    nc = hc.shape[1]
    z = h @ w_in
    cuts = np.cumsum([A_WIDTH, B_WIDTH, A_WIDTH, B_WIDTH, A_WIDTH])
    aq, bq, ak, bk, av, bv = jnp.split(z, cuts, axis=-1)
    zc = hc @ w_in[:, A_WIDTH + B_WIDTH:]
    ak_c, bk_c, av_c, bv_c = jnp.split(zc, np.cumsum([A_WIDTH, B_WIDTH, A_WIDTH]), axis=-1)
    t = jnp.arange(n)
    row, col = t // GRID_W, t % GRID_W
    y_a = neighbourhood_attention(
        aq.reshape(bn, n, NA_HEADS, NA_HEAD_DIM), ak.reshape(bn, n, NA_HEADS, NA_HEAD_DIM),
        av.reshape(bn, n, NA_HEADS, NA_HEAD_DIM), ak_c.reshape(bn, nc, NA_HEADS, NA_HEAD_DIM),
        av_c.reshape(bn, nc, NA_HEADS, NA_HEAD_DIM), rpb)
    lam = (jnp.exp(jnp.sum((lam_q1 * lam_k1).astype(jnp.float32)))
           - jnp.exp(jnp.sum((lam_q2 * lam_k2).astype(jnp.float32))) + lam_init)
    qd = axial_rope(bq.reshape(bn, n, DIFF_HEADS, 2, DIFF_DIM), row, col)
    kd = axial_rope(bk.reshape(bn, n, DIFF_HEADS, 2, DIFF_DIM), row, col)
    y_b = differential_attention(
        qd, kd, bv.reshape(bn, n, DIFF_HEADS, 2 * DIFF_DIM),
        bk_c.reshape(bn, nc, DIFF_HEADS, 2, DIFF_DIM), bv_c.reshape(bn, nc, DIFF_HEADS, 2 * DIFF_DIM),
        lam, lam_init, subln)
    y = jnp.concatenate([y_a.reshape(bn, n, A_WIDTH), y_b.reshape(bn, n, B_WIDTH)], axis=-1)
    return y @ w_out


def depthwise_conv(x, w):
    kk = w.shape[0]
    return lax.conv_general_dilated(
        x, w[:, None, :], window_strides=(1,), padding=[(kk // 2, kk // 2)],
        dimension_numbers=('NWC', 'WIO', 'NWC'), feature_group_count=x.shape[-1])


def conv_mixers(h, w_in, w_out, sc_w, cf_w, cf_b, ln_g, ln_b):
    z = h @ w_in
    cuts = np.cumsum([SC_WIDTH, SC_WIDTH, SC_WIDTH, CF_WIDTH])
    xc, gate_b, gate_c, glu_a, glu_g = jnp.split(z, cuts, axis=-1)
    y_c = gate_b * depthwise_conv(gate_c * xc, sc_w)
    u = glu_a * jax.nn.sigmoid(glu_g)
    u = depthwise_conv(u, cf_w) + cf_b
    y_d = jax.nn.silu(layernorm(u, ln_g, ln_b))
    return jnp.concatenate([y_c, y_d], axis=-1) @ w_out


def expert_choice_ffn(h, w_router, w1, w3, w2):
    bn, n, d = h.shape
    cap = CAPACITY_FACTOR * n // N_EXPERTS
    aff = jax.nn.softmax((h @ w_router).astype(jnp.float32), axis=-1)
    g, idx = lax.top_k(aff.transpose(0, 2, 1), cap)
    xin = jax.vmap(lambda hb, ib: hb[ib])(h, idx)
    hid = (jax.nn.silu(jnp.einsum('becd,edf->becf', xin, w1))
           * jnp.einsum('becd,edf->becf', xin, w3))
    y = jnp.einsum('becf,efd->becd', hid, w2) * g[..., None].astype(h.dtype)
    return jax.vmap(
        lambda ib, yb: jnp.zeros((n, d), yb.dtype).at[ib.reshape(-1)].add(yb.reshape(-1, d))
    )(idx, y)


def setup_inputs(seed: int = 0) -> dict:
    key = jax.random.key(seed)
    keys = iter(jax.random.split(key, 48))
    D = D_MODEL

    def nrm(shape, scale):
        return jax.random.normal(next(keys), shape, jnp.float32) * scale

    def gain(n):
        return 1.0 + nrm((n,), 0.02)

    inp = {}
    inp['x'] = nrm((BATCH, SEQ, D), 1.0)
    inp['c'] = nrm((BATCH, D), 1.0)
    inp['ctx'] = nrm((BATCH, CTX_LEN, D), 1.0)
    inp['c_ctx'] = nrm((D,), 1.0)
    inp['l0_norm1'] = gain(D)
    inp['l0_w_mod'] = nrm((D, 6 * D), 0.5 * D ** -0.5)
    inp['l0_b_mod'] = nrm((6 * D,), 0.01)
    inp['l0_w_in'] = nrm((D, ATTN_IN), D ** -0.5)
    inp['l0_w_out'] = nrm((A_WIDTH + B_WIDTH, D), (A_WIDTH + B_WIDTH) ** -0.5)
    inp['l0_rpb'] = nrm((NA_HEADS, 2 * NA_WIN_H - 1, 2 * NA_WIN_W - 1), 0.1)
    inp['l0_lam_q1'] = nrm((DIFF_DIM,), 0.1)
    inp['l0_lam_k1'] = nrm((DIFF_DIM,), 0.1)
    inp['l0_lam_q2'] = nrm((DIFF_DIM,), 0.1)
    inp['l0_lam_k2'] = nrm((DIFF_DIM,), 0.1)
    inp['l0_subln'] = gain(2 * DIFF_DIM)
    inp['l0_norm2'] = gain(D)
    inp['l0_w_router'] = nrm((D, N_EXPERTS), D ** -0.5)
    inp['l0_w1'] = nrm((N_EXPERTS, D, D_EXPERT), D ** -0.5)
    inp['l0_w3'] = nrm((N_EXPERTS, D, D_EXPERT), D ** -0.5)
    inp['l0_w2'] = nrm((N_EXPERTS, D_EXPERT, D), D_EXPERT ** -0.5)
    inp['l1_norm1'] = gain(D)
    inp['l1_w_mod'] = nrm((D, 6 * D), 0.5 * D ** -0.5)
    inp['l1_b_mod'] = nrm((6 * D,), 0.01)
    inp['l1_w_in'] = nrm((D, CONV_IN), D ** -0.5)
    inp['l1_w_out'] = nrm((SC_WIDTH + CF_WIDTH, D), (SC_WIDTH + CF_WIDTH) ** -0.5)
    inp['l1_sc_w'] = nrm((SC_K, SC_WIDTH), SC_K ** -0.5)
    inp['l1_cf_w'] = nrm((CF_K, CF_WIDTH), CF_K ** -0.5)
    inp['l1_cf_b'] = nrm((CF_WIDTH,), 0.01)
    inp['l1_ln_g'] = gain(CF_WIDTH)
    inp['l1_ln_b'] = nrm((CF_WIDTH,), 0.01)
    inp['l1_norm2'] = gain(D)
    inp['l1_w_router'] = nrm((D, N_EXPERTS), D ** -0.5)
    inp['l1_w1'] = nrm((N_EXPERTS, D, D_EXPERT), D ** -0.5)
    inp['l1_w3'] = nrm((N_EXPERTS, D, D_EXPERT), D ** -0.5)
    inp['l1_w2'] = nrm((N_EXPERTS, D_EXPERT, D), D_EXPERT ** -0.5)
    inp['final_norm'] = gain(D)
    return inp


def reference(x, c, ctx, c_ctx,
              l0_norm1, l0_w_mod, l0_b_mod, l0_w_in, l0_w_out, l0_rpb,
              l0_lam_q1, l0_lam_k1, l0_lam_q2, l0_lam_k2, l0_subln,
              l0_norm2, l0_w_router, l0_w1, l0_w3, l0_w2,
              l1_norm1, l1_w_mod, l1_b_mod, l1_w_in, l1_w_out,
              l1_sc_w, l1_cf_w, l1_cf_b, l1_ln_g, l1_ln_b,
              l1_norm2, l1_w_router, l1_w1, l1_w3, l1_w2,
              final_norm):
    layers = [
        dict(norm1=l0_norm1, w_mod=l0_w_mod, b_mod=l0_b_mod, w_in=l0_w_in, w_out=l0_w_out,
             rpb=l0_rpb, lam_q1=l0_lam_q1, lam_k1=l0_lam_k1, lam_q2=l0_lam_q2, lam_k2=l0_lam_k2,
             subln=l0_subln, norm2=l0_norm2, w_router=l0_w_router, w1=l0_w1, w3=l0_w3, w2=l0_w2),
        dict(norm1=l1_norm1, w_mod=l1_w_mod, b_mod=l1_b_mod, w_in=l1_w_in, w_out=l1_w_out,
             sc_w=l1_sc_w, cf_w=l1_cf_w, cf_b=l1_cf_b, ln_g=l1_ln_g, ln_b=l1_ln_b,
             norm2=l1_norm2, w_router=l1_w_router, w1=l1_w1, w3=l1_w3, w2=l1_w2),
    ]
    silu_c = jax.nn.silu(c)
    silu_c_ctx = jax.nn.silu(c_ctx)
    for l in range(DEPTH):
        p = layers[l]
        mod = silu_c @ p['w_mod'] + p['b_mod']
        shift1, scale1, gate1, shift2, scale2, gate2 = jnp.split(mod, 6, axis=-1)
        h = modulate(rmsnorm(x, p['norm1']), shift1, scale1)
        if l % 2 == 0:
            ctx_mod = silu_c_ctx @ p['w_mod'][:, :2 * D_MODEL] + p['b_mod'][:2 * D_MODEL]
            ctx_shift, ctx_scale = jnp.split(ctx_mod, 2)
            hc = modulate(rmsnorm(ctx, p['norm1']), ctx_shift, ctx_scale)
            lam_init = 0.8 - 0.6 * math.exp(-0.3 * l)
            y = attention_mixers(h, hc, p['w_in'], p['w_out'], p['rpb'], p['lam_q1'], p['lam_k1'],
                                 p['lam_q2'], p['lam_k2'], p['subln'], lam_init)
        else:
            y = conv_mixers(h, p['w_in'], p['w_out'], p['sc_w'], p['cf_w'], p['cf_b'],
                            p['ln_g'], p['ln_b'])
        x = x + gate1[:, None, :] * y
        h = modulate(rmsnorm(x, p['norm2']), shift2, scale2)
        x = x + gate2[:, None, :] * expert_choice_ffn(h, p['w_router'], p['w1'], p['w3'], p['w2'])
    return rmsnorm(x, final_norm)
```

```python
import math
from contextlib import ExitStack
import numpy as np
import concourse.bass as bass
import concourse.mybir as mybir
from concourse.bass_utils import run_bass_kernel_spmd

F32 = mybir.dt.float32
BF16 = mybir.dt.bfloat16
AF = mybir.ActivationFunctionType
ALU = mybir.AluOpType
AX = mybir.AxisListType

D = 1024
SEQ = 2048
NCH = 16
CTX = 256
DE = 2816
NFC = 22
CAP = 256
EPS = 1e-6
GRID_W = 64
NEG = -30000.0


class _Rec:
    def __getattr__(self, name):
        def mk(*a, **k):
            return lambda e: getattr(e, name)(*a, **k)
        return mk


Q = _Rec()


class Prog:
    ENGS = ("pe", "act", "dve", "pool", "sp")
    INORDER = ("pe", "act", "dve")

    def __init__(self, nc):
        self.nc = nc
        self.ops = {e: [] for e in self.ENGS}
        self.last_w = {}
        self.readers = {}
        self.dma_since_barrier = []

    def op(self, eng, fn, reads=(), writes=(), dma=False):
        idx = len(self.ops[eng])
        deps = set()
        for k in reads:
            w = self.last_w.get(k)
            if w is not None:
                deps.add(w)
        for k in writes:
            w = self.last_w.get(k)
            if w is not None:
                deps.add(w)
            for r in self.readers.get(k, ()):
                deps.add(r)
        if eng == "pe":
            deps = {d for d in deps if not (d[0] == "pe" and not self.ops["pe"][d[1]]["dma"])}
        self.ops[eng].append(dict(fn=fn, deps=deps, inc=False, dma=dma))
        me = (eng, idx)
        for k in reads:
            lst = self.readers.setdefault(k, [])
            if not dma and eng in self.INORDER:
                lst[:] = [r for r in lst if not (r[0] == eng and not self.ops[eng][r[1]]["dma"])]
            lst.append(me)
        for k in writes:
            self.last_w[k] = me
            self.readers[k] = []
        if dma:
            self.dma_since_barrier.append(me)
        return me

    def barrier(self):
        targets = set(self.dma_since_barrier)
        for e in self.ENGS:
            for i in range(len(self.ops[e]) - 1, -1, -1):
                if not self.ops[e][i]["dma"] and self.ops[e][i]["fn"] is not None:
                    targets.add((e, i))
                    break
        for e in self.ENGS:
            self.ops[e].append(dict(fn=None, deps=set(targets), inc=False, dma=False))
        self.dma_since_barrier = []

    def emit(self, stack):
        nc = self.nc
        for e in self.ENGS:
            for o in self.ops[e]:
                for (de, di) in o["deps"]:
                    t = self.ops[de][di]
                    if not t["dma"]:
                        t["inc"] = True
        EPOCH = 30000
        ev = {}
        for e in self.ENGS:
            cnt = 0
            sem = None
            for i, o in enumerate(self.ops[e]):
                if o["dma"] or not o["inc"]:
                    continue
                if sem is None or cnt >= EPOCH:
                    sem = stack.enter_context(nc.semaphore(f"s_{e}_{i}"))
                    cnt = 0
                cnt += 1
                ev[(e, i)] = (sem, cnt)
                o["sem"] = sem
        NDS = 12
        for e in self.ENGS:
            if not any(o["dma"] for o in self.ops[e]):
                continue
            pool = [stack.enter_context(nc.semaphore(f"d_{e}_{k}")) for k in range(NDS)]
            vals = [0] * NDS
            prev = [None] * NDS
            k = 0
            for i, o in enumerate(self.ops[e]):
                if not o["dma"]:
                    continue
                slot = k % NDS
                k += 1
                vals[slot] += 16
                ev[(e, i)] = (pool[slot], vals[slot])
                o["sem"] = pool[slot]
                if prev[slot] is not None:
                    o["deps"].add(prev[slot])
                prev[slot] = (e, i)
        engobj = dict(pe=nc.tensor, act=nc.scalar, dve=nc.vector, pool=nc.gpsimd, sp=nc.sync)
        ops = self.ops

        def run(ename):
            def body(eng):
                waited = {}
                for o in ops[ename]:
                    for d in sorted(o["deps"]):
                        sem, val = ev[d]
                        key = id(sem)
                        if waited.get(key, 0) >= val:
                            continue
                        waited[key] = val
                        eng.wait_ge(sem, val)
                    if o["fn"] is None:
                        continue
                    ins = o["fn"](eng)
                    if o["dma"]:
                        ins.then_inc(o["sem"], 16)
                    elif o["inc"]:
                        ins.then_inc(o["sem"], 1)
            return body

        with nc.Block() as block:
            block.tensor(run("pe"))
            block.scalar(run("act"))
            block.vector(run("dve"))
            block.gpsimd(run("pool"))
            block.sync(run("sp"))


def _rope_tables():
    n_freq = 16
    freqs = (np.float32(10000.0) ** (-np.arange(n_freq, dtype=np.float32) / n_freq)).astype(np.float32)
    t = np.arange(SEQ)
    row = (t // GRID_W).astype(np.float32)
    col = (t % GRID_W).astype(np.float32)
    cos = np.zeros((64, SEQ), np.float32)
    sin = np.zeros((64, SEQ), np.float32)
    for f in range(64):
        pos = row if f < 32 else col
        i = f % 32
        ang = (pos * freqs[i % 16]).astype(np.float32)
        cos[f] = np.cos(ang)
        sin[f] = np.sin(ang) * (-1.0 if i < 16 else 1.0)
    return np.concatenate([cos, cos], 0), np.concatenate([sin, sin], 0)


def _rope_perm():
    p = np.zeros(64, np.int64)
    for f in range(64):
        base = 0 if f < 32 else 32
        i = f % 32
        p[f] = base + (i + 16 if i < 16 else i - 16)
    return p


def _na_blocks():
    out = []
    for b in range(8):
        if b == 0:
            out.append(list(range(0, 4)))
        elif b == 7:
            out.append(list(range(12, 16)))
        else:
            out.append(list(range(2 * b - 2, 2 * b + 4)))
    return out


def _na_class(b):
    return 0 if b == 0 else (2 if b == 7 else 1)


NA_CLS_OFF = [0, 4, 10]
NA_CLS_B = [0, 1, 7]


def _na_bias_tables(rpb):
    H = rpb.shape[0]
    tab = np.full((H, 128, 14, 256), NEG, np.float32)
    blocks = _na_blocks()
    kp = np.arange(128)
    kr_l = kp // 64
    kc = kp % 64
    qq = np.arange(256)
    qr_l = qq // 64
    qc = qq % 64
    cs = np.clip(qc - 8, 0, 48)
    for cls in range(3):
        b = NA_CLS_B[cls]
        for si, c in enumerate(blocks[b]):
            kr = 2 * c + kr_l
            r = 4 * b + qr_l
            rs = np.clip(r - 4, 0, 24)
            inrow = (kr[:, None] >= rs[None, :]) & (kr[:, None] <= rs[None, :] + 7)
            incol = (kc[:, None] >= cs[None, :]) & (kc[:, None] < cs[None, :] + 16)
            ok = inrow & incol
            dr = np.clip(kr[:, None] - r[None, :] + 7, 0, 14)
            dc = np.clip(kc[:, None] - qc[None, :] + 15, 0, 30)
            g = rpb[:, dr, dc]
            tab[:, :, NA_CLS_OFF[cls] + si, :] = np.where(ok[None], g, np.float32(NEG))
    return tab


class _Stop(Exception):
    pass


class Phase(ExitStack):
    def __exit__(self, et, ev, tb):
        super().__exit__(None, None, None)
        return False


def build(S, E, taps=(), stop=None):
    nc = bass.Bass("TRN2", target_bir_lowering=False)
    P = Prog(nc)
    st = ExitStack()

    def din(name, shape, dt=F32):
        return nc.dram_tensor(name, list(shape), dt, kind="ExternalInput").ap()

    x_d = din("x", [S, SEQ, D])
    c_d = din("cT", [128, 8, S + 1])
    ctx_d = din("ctx", [S, CTX, D])
    out_d = nc.dram_tensor("out", [S, SEQ, D], F32, kind="ExternalOutput").ap()
    tap_d = {t: nc.dram_tensor(f"tap_{t}", [SEQ, D], F32, kind="ExternalOutput").ap() for t in taps}
    L = []
    for l in range(2):
        p = {}
        p["normcols"] = din(f"l{l}_normcols", [128, 2, 8])
        p["w_mod"] = din(f"l{l}_w_mod", [D, 6 * D])
        p["b_mod"] = din(f"l{l}_b_mod", [1, 6 * D])
        p["w_out"] = din(f"l{l}_w_out", [D, D])
        p["w_router"] = din(f"l{l}_w_router", [D, 16])
        p["w1"] = din(f"l{l}_w1", [E, D, DE])
        p["w3"] = din(f"l{l}_w3", [E, D, DE])
        p["w2"] = din(f"l{l}_w2", [E, DE, D])
        if l == 0:
            p["w_na"] = din("l0_w_na", [4, D, 384])
            p["w_df"] = din("l0_w_df", [4, D, 640])
            p["na_bias"] = din("l0_na_bias", [8, 128, 14 * 256])
            p["lam"] = din("l0_lam", [1, 256])
            p["subln"] = din("l0_subln", [1, 128])
        else:
            p["w_c"] = din("l1_w_c", [4, D, 384])
            p["w_d"] = din("l1_w_d", [4, D, 256])
            p["sc_w"] = din("l1_sc_w", [128, 4, 3])
            p["cf_w"] = din("l1_cf_w", [128, 4, 31])
            p["cf_vec"] = din("l1_cf_vec", [128, 3, 4])
        L.append(p)
    fnorm_d = din("final_norm", [1, D])
    rope_d = din("rope", [2, 128, SEQ])
    cidx_d = din("cidx", [128, 16, 2], BF16)
    ident_d = din("ident", [128, 128])
    ltri_d = din("ltri", [128, 128])
    iota_d = din("iota", [1, SEQ])

    uid = [0]

    def un(name):
        uid[0] += 1
        return f"t{uid[0]}_{name}"

    def sb(name, shape, dt):
        return st.enter_context(nc.sbuf_tensor(un(name), list(shape), dt))

    def ps(name, shape, dt=F32):
        return st.enter_context(nc.psum_tensor(name, list(shape), dt))

    x = sb("x", [128, NCH, D], F32)
    ident_f = sb("ident_f", [128, 128], F32)
    ident_b = sb("ident_b", [128, 128], BF16)
    ones_f = sb("ones_f", [128, 128], F32)
    ltri_b = sb("ltri_b", [128, 128], BF16)
    ones_b = sb("ones_b", [128, 128], BF16)
    iota_t = sb("iota_t", [128, SEQ], F32)
    cidx = sb("cidx_sb", [128, 16, 2], BF16)
    modc = [sb(f"modc{l}", [128, 48, S + 1], F32) for l in range(2)]
    normc = [sb(f"normc{l}", [128, 2, 8], F32) for l in range(2)]
    siluc = sb("siluc", [128, 8, S + 1], BF16)
    fnorm_b = sb("fnorm_b", [128, D], F32)
    AB = sb("AB", [128, 2, 8], F32)
    gate_b = sb("gate_b", [128, D], F32)
    small = sb("small", [128, 64], F32)
    stat = sb("stat", [128, 32], F32)
    junk = sb("junk", [128, D], F32)

    PB = [ps(f"pb{k}", [128, 512], F32) for k in range(8)]
    PBb = [t[:].bitcast(BF16) for t in PB]

    def K(*a):
        return a

    def stage(n):
        if stop is not None and n == stop:
            raise _Stop()

    P.op("sp", Q.dma_start(out=ident_f[:], in_=ident_d), writes=[K("ident_f")], dma=True)
    P.op("pool", Q.dma_start(out=ident_b[:], in_=ident_d), writes=[K("ident_b")], dma=True)
    P.op("pool", Q.dma_start(out=ltri_b[:], in_=ltri_d), writes=[K("ltri_b")], dma=True)
    P.op("sp", Q.dma_start(out=iota_t[:], in_=iota_d.partition_broadcast(128)), writes=[K("iota_t")], dma=True)
    P.op("sp", Q.dma_start(out=cidx[:], in_=cidx_d), writes=[K("cidx")], dma=True)
    P.op("sp", Q.dma_start(out=fnorm_b[:], in_=fnorm_d.partition_broadcast(128)), writes=[K("fnorm_b")], dma=True)
    P.op("dve", Q.memset(ones_f[:], 1.0), writes=[K("ones_f")])
    P.op("dve", Q.memset(ones_b[:], 1.0), writes=[K("ones_b")])
    for l in range(2):
        P.op("sp", Q.dma_start(out=normc[l][:], in_=L[l]["normcols"]), writes=[K("normc", l)], dma=True)

    with Phase() as ph:
        cT = ph.enter_context(nc.sbuf_tensor(un("cT_sb"), [128, 8, S + 1], F32))
        sg = ph.enter_context(nc.sbuf_tensor(un("sg_sb"), [128, 8, S + 1], F32))
        bm = ph.enter_context(nc.sbuf_tensor(un("bm_sb"), [1, 6 * D], BF16))
        wm = [ph.enter_context(nc.sbuf_tensor(un(f"wm{k}"), [128, 8, 512], BF16)) for k in range(2)]
        P.op("sp", Q.dma_start(out=cT[:], in_=c_d), writes=[K("cT")], dma=True)
        P.op("act", Q.activation(out=sg[:], in_=cT[:], func=AF.Sigmoid), reads=[K("cT")], writes=[K("sg")])
        P.op("dve", Q.tensor_tensor(out=siluc[:], in0=cT[:], in1=sg[:], op=ALU.mult),
             reads=[K("cT"), K("sg")], writes=[K("siluc")])
        blk = 0
        for l in range(2):
            P.op("pool", Q.dma_start(out=bm[:], in_=L[l]["b_mod"]), writes=[K("bm")], dma=True)
            for cb in range(12):
                w = wm[blk % 2]
                wk = K("wm", blk % 2)
                blk += 1
                src = L[l]["w_mod"][:, cb * 512:(cb + 1) * 512].rearrange("(kc p) n -> p kc n", p=128)
                P.op("pool", Q.dma_start(out=w[:], in_=src), writes=[wk], dma=True)
                pt = PB[cb % 2]
                pk = K("pb", cb % 2)
                for fcl in range(4):
                    o = pt[:, fcl * (S + 1):(fcl + 1) * (S + 1)]
                    for kc in range(8):
                        P.op("pe", Q.matmul(
                            o, w[:, kc, fcl * 128:(fcl + 1) * 128], siluc[:, kc, :], start=(kc == 0), stop=False),
                            reads=[wk, K("siluc")], writes=[pk])
                    f0 = cb * 512 + fcl * 128
                    P.op("pe", Q.matmul(
                        o, bm[0:1, f0:f0 + 128], ones_b[0:1, 0:S + 1], start=False, stop=True),
                        reads=[K("bm"), K("ones_b")], writes=[pk])
                P.op("dve", Q.tensor_copy(
                    out=modc[l][:, cb * 4:(cb + 1) * 4, :],
                    in_=pt[:, 0:4 * (S + 1)].rearrange("p (a b) -> p a b", b=S + 1)),
                    reads=[pk], writes=[K("modc", l)])
    P.barrier()

    def make_AB(l, which, col, nkey):
        sh = 0 if which == 0 else 24
        P.op("dve", Q.scalar_tensor_tensor(
            out=AB[:, 0, :], in0=modc[l][:, sh + 8:sh + 16, col], scalar=1.0, in1=normc[l][:, nkey, :],
            op0=ALU.add, op1=ALU.mult), reads=[K("modc", l), K("normc", l)], writes=[K("AB")])
        P.op("dve", Q.tensor_copy(out=AB[:, 1, :], in_=modc[l][:, sh:sh + 8, col]),
             reads=[K("modc", l)], writes=[K("AB")])

    def make_gate(l, which, col):
        g0 = 16 if which == 0 else 40
        with Phase() as ph:
            dg = ph.enter_context(nc.sbuf_tensor(un("dg"), [128, 8, 128], F32))
            for kc in range(8):
                P.op("dve", Q.tensor_scalar(
                    out=dg[:, kc, :], in0=ident_f[:], scalar1=modc[l][:, g0 + kc, col:col + 1], scalar2=None,
                    op0=ALU.mult), reads=[K("ident_f"), K("modc", l)], writes=[K("dg", kc)])
            for h in range(2):
                for k4 in range(4):
                    kc = h * 4 + k4
                    P.op("pe", Q.matmul(
                        PB[h][:, k4 * 128:(k4 + 1) * 128], ones_f[:], dg[:, kc, :], start=True, stop=True),
                        reads=[K("ones_f"), K("dg", kc)], writes=[K("pb", h)])
                P.op("act", Q.activation(out=gate_b[:, h * 512:(h + 1) * 512], in_=PB[h][:], func=AF.Copy),
                     reads=[K("pb", h)], writes=[K("gate_b")])
        P.barrier()

    def rms_xs(src, c, xs_out, keys_r, keys_w, width=D):
        sl = c % 16
        sq = stat[:, 2 * sl:2 * sl + 1]
        rs = stat[:, 2 * sl + 1:2 * sl + 2]
        sk = K("stat", sl)
        P.op("act", Q.activation(out=junk[:, 0:width], in_=src, func=AF.Square),
             reads=keys_r, writes=[K("junk")])
        P.op("dve", Q.reduce_sum(out=sq, in_=junk[:, 0:width], axis=AX.X),
             reads=[K("junk")], writes=[sk])
        P.op("dve", Q.tensor_scalar(out=rs, in0=sq, scalar1=1.0 / width, scalar2=EPS, op0=ALU.mult, op1=ALU.add),
             reads=[sk], writes=[sk])
        P.op("act", Q.activation(out=rs, in_=rs, func=AF.Sqrt), reads=[sk], writes=[sk])
        P.op("dve", Q.reciprocal(out=rs, in_=rs), reads=[sk], writes=[sk])
        P.op("dve", Q.tensor_scalar(out=xs_out, in0=src, scalar1=rs, scalar2=None, op0=ALU.mult),
             reads=keys_r + [sk], writes=keys_w)

    def norm_T(hT, hT_key, ntok_chunks, src_chunk, src_keys, xsg, group_cb=None, xs_keep=None):
        ng = ntok_chunks // 4
        for g in range(ng):
            for j in range(4):
                c = g * 4 + j
                if xs_keep is not None:
                    xo = xs_keep[:, c, :]
                    kw = [K("xs2", c)]
                else:
                    xo = xsg[:, j, :]
                    kw = [K("xsg", j)]
                rms_xs(src_chunk(c), c, xo, src_keys(c), kw)
            for dc in range(8):
                pt = PBb[dc % 2]
                pk = K("pb", dc % 2)
                for j in range(4):
                    c = g * 4 + j
                    if xs_keep is not None:
                        xi = xs_keep[:, c, dc * 128:(dc + 1) * 128]
                        kr = [K("xs2", c)]
                    else:
                        xi = xsg[:, j, dc * 128:(dc + 1) * 128]
                        kr = [K("xsg", j)]
                    P.op("pe", Q.transpose(pt[:, j * 128:(j + 1) * 128], xi, ident_b[:]),
                         reads=kr + [K("ident_b")], writes=[pk])
                P.op("act", Q.activation(
                    out=hT[:, dc, g * 512:(g + 1) * 512], in_=pt[:, 0:512], func=AF.Identity,
                    scale=AB[:, 0, dc:dc + 1], bias=AB[:, 1, dc:dc + 1]),
                    reads=[pk, K("AB")], writes=[K(hT_key, g)])
            if group_cb is not None:
                group_cb(g)

    def out_proj_unit(l, u, yT, yT_key, wo):
        P.op("pool", Q.dma_start(out=wo[0][:], in_=L[l]["w_out"][u * 128:(u + 1) * 128, :]),
             writes=[K("wo_f")], dma=True)
        P.op("dve", Q.tensor_tensor(out=wo[1][:], in0=wo[0][:], in1=gate_b[:], op=ALU.mult),
             reads=[K("wo_f"), K("gate_b")], writes=[K("wo_b")])
        for c in range(NCH):
            for h in range(2):
                bk = 6 + h
                P.op("pe", Q.matmul(
                    PB[bk][:], yT[:, c * 128:(c + 1) * 128], wo[1][:, h * 512:(h + 1) * 512], start=True, stop=True),
                    reads=[K(yT_key), K("wo_b")], writes=[K("pb", bk)])
                P.op("dve", Q.tensor_tensor(
                    out=x[:, c, h * 512:(h + 1) * 512], in0=PB[bk][:], in1=x[:, c, h * 512:(h + 1) * 512], op=ALU.add),
                    reads=[K("pb", bk), K("x", c)], writes=[K("x", c)])

    def proj_T(dst, dst_keys, w, wkey, col0, hT, hT_key, ntok, evac="act", tok0=0, dst0=0, pbs=(0, 1)):
        nb = (ntok + 511) // 512
        for b in range(nb):
            n = min(512, ntok - b * 512)
            pt = PB[pbs[b % len(pbs)]]
            pk = K("pb", pbs[b % len(pbs)])
            for kc in range(8):
                P.op("pe", Q.matmul(
                    pt[:, 0:n], w[:, kc, col0:col0 + 128], hT[:, kc, tok0 + b * 512: tok0 + b * 512 + n],
                    start=(kc == 0), stop=(kc == 7)), reads=[wkey, K(hT_key, (tok0 + b * 512) // 512)], writes=[pk])
            o = dst[:, dst0 + b * 512: dst0 + b * 512 + n]
            if evac == "act":
                P.op("act", Q.activation(out=o, in_=pt[:, 0:n], func=AF.Copy),
                     reads=[pk], writes=dst_keys)
            else:
                evac(b, n, pt, pk, o)

    def tap(name, s):
        if name in tap_d and s == 0:
            P.op("sp", Q.dma_start(out=tap_d[name].rearrange("(c p) d -> p c d", p=128), in_=x[:]),
                 reads=[K("x", c) for c in range(NCH)], dma=True)

    def attn_block(pb, qb, qT, kT, chunks, v_fn, dv, eb_fn, Pt, Pt_key, etmp, out_cb, qkey, kkey, vkey):
        nk = len(chunks)
        ngr = (nk + 1) // 2
        for gi in range(ngr):
            bk = 2 + (gi % 3)
            pt = PB[bk]
            pk = K("pb", bk)
            sub = chunks[gi * 2: gi * 2 + 2]
            for j, kc in enumerate(sub):
                P.op("pe", Q.matmul(
                    pt[:, j * 256:(j + 1) * 256], kT[pb:pb + 64, kc * 128:(kc + 1) * 128],
                    qT[pb:pb + 64, qb * 256:(qb + 1) * 256], start=True, stop=True),
                    reads=[qkey, kkey], writes=[pk])
            w = 256 * len(sub)
            dst = Pt[:, gi * 2: gi * 2 + len(sub), :].rearrange("p a b -> p (a b)")
            eb = eb_fn(gi * 2, len(sub)) if eb_fn is not None else None
            if eb is None:
                P.op("act", Q.activation(out=dst, in_=pt[:, 0:w], func=AF.Exp, scale=0.125),
                     reads=[pk], writes=[K(Pt_key, gi)])
            else:
                et = etmp[gi % 2]
                ek = K("etmp", gi % 2)
                P.op("act", Q.activation(out=et[:, 0:w], in_=pt[:, 0:w], func=AF.Exp, scale=0.125),
                     reads=[pk], writes=[ek])
                P.op("dve", Q.tensor_tensor(out=dst, in0=et[:, 0:w], in1=eb, op=ALU.mult),
                     reads=[ek, K("big16")], writes=[K(Pt_key, gi)])
        for qi in range(2):
            qc = qb * 2 + qi
            pt = PB[5]
            pk = K("pb", 5)
            for i, kc in enumerate(chunks):
                P.op("pe", Q.matmul(
                    pt[:, 0:dv + 1], Pt[:, i, qi * 128:(qi + 1) * 128], v_fn(kc), start=(i == 0), stop=(i == nk - 1)),
                    reads=[K(Pt_key, i // 2), vkey], writes=[pk])
            out_cb(qc, pt, pk)

    try:
        XK = [K("x", c) for c in range(NCH)]
        for s in range(S):
            P.op("sp", Q.dma_start(out=x[:], in_=x_d[s].rearrange("(c p) d -> p c d", p=128)),
                 writes=XK, dma=True)
            stage(0)
            for l in range(2):
                make_gate(l, 0, s)
                with Phase() as ph:
                    def pa(name, shape, dt):
                        return ph.enter_context(nc.sbuf_tensor(un(name), list(shape), dt))
                    hT = pa("hT", [128, 8, SEQ], BF16)
                    wo = (pa("wo_f", [128, D], F32), pa("wo_b", [128, D], BF16))
                    yT = pa("yT", [128, SEQ], BF16)
                    if l == 0:
                        Pt = [pa(f"Pt{k}", [128, 18, 256], BF16) for k in range(2)]
                        xsg = Pt[0][:, 0:16, :].rearrange("p (a b) c -> p a (b c)", a=4)
                    else:
                        xsg = pa("xsg", [128, 4, D], BF16)
                    make_AB(l, 0, s, 0)
                    norm_T(hT, "hT", NCH, lambda c: x[:, c, :], lambda c: [K("x", c)], xsg)
                    if l == 0:
                        hcT = pa("hcT", [128, 8, CTX], BF16)
                        big16 = pa("big16", [128, 2, SEQ], F32)
                        cx = big16[:, 0, :].rearrange("p (a b) -> p a b", a=2)
                        P.op("sp", Q.dma_start(out=cx, in_=ctx_d[s].rearrange("(c p) d -> p c d", p=128)),
                             writes=[K("big16")], dma=True)
                        make_AB(l, 0, S, 0)
                        for j in range(2):
                            rms_xs(cx[:, j, :], j, xsg[:, j, :], [K("big16")], [K("xsg", j)])
                        for dc in range(8):
                            pt = PBb[dc % 2]
                            pk = K("pb", dc % 2)
                            for j in range(2):
                                P.op("pe", Q.transpose(
                                    pt[:, j * 128:(j + 1) * 128], xsg[:, j, dc * 128:(dc + 1) * 128], ident_b[:]),
                                    reads=[K("xsg", j), K("ident_b")], writes=[pk])
                            P.op("act", Q.activation(
                                out=hcT[:, dc, :], in_=pt[:, 0:256], func=AF.Identity,
                                scale=AB[:, 0, dc:dc + 1], bias=AB[:, 1, dc:dc + 1]),
                                reads=[pk, K("AB")], writes=[K("hcT", 0)])
                        P.barrier()
                        stage(1)
                        wu = pa("wu", [128, 8, 640], BF16)
                        qT = pa("qT", [128, SEQ], BF16)
                        kT = pa("kT", [128, SEQ + CTX], BF16)
                        vv = pa("vv", [128, 18, 130], BF16)
                        etmp = [pa(f"etmp{k}", [128, 512], F32) for k in range(2)]
                        EB = big16[:].rearrange("p a b -> p (a b)")[:, 0:14 * 256].rearrange("p (a b) -> p a b", a=14)
                        rope = big16
                        ytm = pa("ytm", [128, NCH, 128], BF16)
                        tmpA = pa("tmpA", [128, 512], F32)
                        tmpB = pa("tmpB", [128, 512], F32)
                        lamv = pa("lamv", [128, 4, 64], F32)
                        lam = pa("lam", [128, 4], F32)
                        subl = pa("subl", [128, 128], F32)
                        o0 = pa("o0", [128, 2, 129], F32)
                        P.op("sp", Q.dma_start(out=lamv[:].rearrange("p a b -> p (a b)"), in_=L[0]["lam"].partition_broadcast(128)),
                             writes=[K("lamv")], dma=True)
                        P.op("sp", Q.dma_start(out=subl[:], in_=L[0]["subln"].partition_broadcast(128)),
                             writes=[K("subl")], dma=True)
                        lam_init = 0.8 - 0.6 * math.exp(-0.3 * 0)
                        for j in range(2):
                            P.op("dve", Q.tensor_tensor(out=tmpA[:, 0:64], in0=lamv[:, 2 * j, :], in1=lamv[:, 2 * j + 1, :], op=ALU.mult),
                                 reads=[K("lamv")], writes=[K("tmpA")])
                            P.op("dve", Q.reduce_sum(out=lam[:, j:j + 1], in_=tmpA[:, 0:64], axis=AX.X),
                                 reads=[K("tmpA")], writes=[K("lam")])
                        P.op("act", Q.activation(out=lam[:, 0:2], in_=lam[:, 0:2], func=AF.Exp), reads=[K("lam")], writes=[K("lam")])
                        P.op("dve", Q.tensor_tensor(out=lam[:, 2:3], in0=lam[:, 0:1], in1=lam[:, 1:2], op=ALU.subtract),
                             reads=[K("lam")], writes=[K("lam")])
                        P.op("dve", Q.tensor_scalar(out=lam[:, 3:4], in0=lam[:, 2:3], scalar1=lam_init, scalar2=-1.0, op0=ALU.add, op1=ALU.mult),
                             reads=[K("lam")], writes=[K("lam")])
                        P.op("dve", Q.tensor_scalar(out=subl[:], in0=subl[:], scalar1=1.0 - lam_init, scalar2=None, op0=ALU.mult),
                             reads=[K("subl")], writes=[K("subl")])
                        blocks = _na_blocks()
                        for u in range(8):
                            is_na = u < 4
                            ncol = 384 if is_na else 640
                            wsrc = (L[0]["w_na"][u] if is_na else L[0]["w_df"][u - 4]).rearrange("(kc p) n -> p kc n", p=128)
                            P.op("pool", Q.dma_start(out=wu[:, :, 0:ncol], in_=wsrc),
                                 writes=[K("wu")], dma=True)
                            if is_na:
                                proj_T(qT, [K("qT")], wu, K("wu"), 0, hT, "hT", SEQ)
                                proj_T(kT, [K("kT")], wu, K("wu"), 128, hT, "hT", SEQ)
                                proj_T(kT, [K("kT")], wu, K("wu"), 128, hcT, "hcT", CTX, dst0=SEQ)
                                vcol, dv = 256, 64
                            else:
                                if u == 4:
                                    P.op("sp", Q.dma_start(out=rope[:], in_=rope_d.rearrange("a p t -> p a t")),
                                         writes=[K("big16")], dma=True)
                                for (dst, dkey, c1, c2) in ((qT, K("qT"), 0, 128), (kT, K("kT"), 256, 384)):
                                    for b in range(4):
                                        for (pi, cc) in ((0, c1), (1, c2)):
                                            for kc in range(8):
                                                P.op("pe", Q.matmul(
                                                    PB[pi][:], wu[:, kc, cc:cc + 128], hT[:, kc, b * 512:(b + 1) * 512],
                                                    start=(kc == 0), stop=(kc == 7)),
                                                    reads=[K("wu"), K("hT", b)], writes=[K("pb", pi)])
                                        P.op("dve", Q.tensor_tensor(out=tmpA[:], in0=PB[0][:], in1=rope[:, 0, b * 512:(b + 1) * 512], op=ALU.mult),
                                             reads=[K("pb", 0), K("big16")], writes=[K("tmpA")])
                                        P.op("dve", Q.tensor_tensor(out=tmpB[:], in0=PB[1][:], in1=rope[:, 1, b * 512:(b + 1) * 512], op=ALU.mult),
                                             reads=[K("pb", 1), K("big16")], writes=[K("tmpB")])
                                        P.op("dve", Q.tensor_tensor(out=dst[:, b * 512:(b + 1) * 512], in0=tmpA[:], in1=tmpB[:], op=ALU.add),
                                             reads=[K("tmpA"), K("tmpB")], writes=[dkey])
                                proj_T(kT, [K("kT")], wu, K("wu"), 256, hcT, "hcT", CTX, dst0=SEQ)
                                vcol, dv = 512, 128
                            for c in range(18):
                                src = hT[:, :, c * 128:(c + 1) * 128] if c < 16 else hcT[:, :, (c - 16) * 128:(c - 15) * 128]
                                srck = K("hT", c // 4) if c < 16 else K("hcT", 0)
                                pt = PB[c % 2]
                                pk = K("pb", c % 2)
                                for kc in range(8):
                                    P.op("pe", Q.matmul(
                                        pt[:, 0:128], src[:, kc, :], wu[:, kc, vcol:vcol + 128], start=(kc == 0), stop=(kc == 7)),
                                        reads=[K("wu"), srck], writes=[pk])
                                if is_na:
                                    P.op("act", Q.activation(
                                        out=vv[:, c, :].rearrange("p (h d) -> p h d", h=2)[:, :, 0:64],
                                        in_=pt[:, 0:128].rearrange("p (h d) -> p h d", h=2), func=AF.Copy),
                                        reads=[pk], writes=[K("vv")])
                                else:
                                    P.op("act", Q.activation(out=vv[:, c, 0:128], in_=pt[:, 0:128], func=AF.Copy),
                                         reads=[pk], writes=[K("vv")])
                            if is_na:
                                P.op("dve", Q.memset(vv[:].rearrange("p c (h d) -> p c h d", h=2)[:, :, :, 64:65], 1.0),
                                     writes=[K("vv")])
                            else:
                                P.op("dve", Q.memset(vv[:, :, 128:129], 1.0), writes=[K("vv")])
                            if is_na:
                                for hh in range(2):
                                    head = 2 * u + hh
                                    P.op("sp", Q.dma_start(
                                        out=EB.rearrange("p a b -> p (a b)"), in_=L[0]["na_bias"][head]),
                                        writes=[K("big16")], dma=True)
                                    for a in range(2):
                                        P.op("act", Q.activation(out=EB[:, a * 7:(a + 1) * 7, :], in_=EB[:, a * 7:(a + 1) * 7, :], func=AF.Exp),
                                             reads=[K("big16")], writes=[K("big16")])

                                    def ocb(qc, pt, pk, hh=hh):
                                        P.op("dve", Q.reciprocal(out=small[:, 2:3], in_=pt[:, 64:65]),
                                             reads=[pk], writes=[K("small")])
                                        P.op("dve", Q.tensor_scalar(out=ytm[:, qc, hh * 64:(hh + 1) * 64], in0=pt[:, 0:64],
                                                                              scalar1=small[:, 2:3], scalar2=None, op0=ALU.mult),
                                             reads=[pk, K("small")], writes=[K("ytm", qc)])

                                    for qb in range(8):
                                        nb = len(blocks[qb])
                                        off = NA_CLS_OFF[_na_class(qb)]

                                        def ebf(i0, n, nb=nb, off=off):
                                            if i0 >= nb:
                                                return None
                                            assert i0 + n <= nb
                                            return EB[:, off + i0:off + i0 + n, :].rearrange("p a b -> p (a b)")

                                        attn_block(64 * hh, qb, qT, kT, blocks[qb] + [16, 17],
                                                   lambda kc, hh=hh: vv[:, kc, hh * 65:(hh + 1) * 65], 64, ebf,
                                                   Pt[hh], ("Pt", hh), etmp, ocb, K("qT"), K("kT"), K("vv"))
                            else:
                                def ocb0(qc, pt, pk):
                                    P.op("act", Q.activation(out=o0[:, qc % 2, :], in_=pt[:, 0:129], func=AF.Copy),
                                         reads=[pk], writes=[K("o0", qc % 2)])

                                def ocb1(qc, pt, pk):
                                    r0, r1, ssq, rs = small[:, 4:5], small[:, 5:6], small[:, 6:7], small[:, 7:8]
                                    ok = K("o0", qc % 2)
                                    P.op("dve", Q.reciprocal(out=r0, in_=o0[:, qc % 2, 128:129]), reads=[ok], writes=[K("small")])
                                    P.op("dve", Q.reciprocal(out=r1, in_=pt[:, 128:129]), reads=[pk], writes=[K("small")])
                                    P.op("dve", Q.tensor_tensor(out=r1, in0=r1, in1=lam[:, 3:4], op=ALU.mult),
                                         reads=[K("small"), K("lam")], writes=[K("small")])
                                    P.op("dve", Q.tensor_scalar(out=tmpA[:, 0:128], in0=o0[:, qc % 2, 0:128], scalar1=r0, scalar2=None, op0=ALU.mult),
                                         reads=[ok, K("small")], writes=[K("tmpA")])
                                    P.op("dve", Q.scalar_tensor_tensor(out=tmpA[:, 0:128], in0=pt[:, 0:128], scalar=r1, in1=tmpA[:, 0:128],
                                                                                 op0=ALU.mult, op1=ALU.add),
                                         reads=[pk, K("small"), K("tmpA")], writes=[K("tmpA")])
                                    P.op("act", Q.activation(out=tmpB[:, 0:128], in_=tmpA[:, 0:128], func=AF.Square),
                                         reads=[K("tmpA")], writes=[K("tmpB")])
                                    P.op("dve", Q.reduce_sum(out=ssq, in_=tmpB[:, 0:128], axis=AX.X), reads=[K("tmpB")], writes=[K("small")])
                                    P.op("dve", Q.tensor_scalar(out=rs, in0=ssq, scalar1=1.0 / 128, scalar2=EPS, op0=ALU.mult, op1=ALU.add),
                                         reads=[K("small")], writes=[K("small")])
                                    P.op("act", Q.activation(out=rs, in_=rs, func=AF.Sqrt), reads=[K("small")], writes=[K("small")])
                                    P.op("dve", Q.reciprocal(out=rs, in_=rs), reads=[K("small")], writes=[K("small")])
                                    P.op("dve", Q.scalar_tensor_tensor(out=ytm[:, qc, :], in0=tmpA[:, 0:128], scalar=rs, in1=subl[:],
                                                                                 op0=ALU.mult, op1=ALU.mult),
                                         reads=[K("tmpA"), K("small"), K("subl")], writes=[K("ytm", qc)])

                                for qb in range(8):
                                    for m in range(2):
                                        attn_block(64 * m, qb, qT, kT, list(range(18)), lambda kc: vv[:, kc, 0:129], 128, None,
                                                   Pt[m], ("Pt", m), etmp, ocb0 if m == 0 else ocb1, K("qT"), K("kT"), K("vv"))
                            for g in range(4):
                                pt = PBb[g % 2]
                                pk = K("pb", g % 2)
                                for j in range(4):
                                    c = g * 4 + j
                                    P.op("pe", Q.transpose(pt[:, j * 128:(j + 1) * 128], ytm[:, c, :], ident_b[:]),
                                         reads=[K("ytm", c), K("ident_b")], writes=[pk])
                                P.op("act", Q.activation(out=yT[:, g * 512:(g + 1) * 512], in_=pt[:, 0:512], func=AF.Copy),
                                     reads=[pk], writes=[K("yT")])
                            out_proj_unit(l, u, yT, "yT", wo)
                            stage(20 + u)
                    else:
                        P.barrier()
                        wu = pa("wu", [128, 8, 384], BF16)
                        scw = pa("scw", [128, 4, 3], F32)
                        cfw = pa("cfw", [128, 4, 31], F32)
                        cfv = pa("cfv", [128, 3, 4], F32)
                        P.op("sp", Q.dma_start(out=scw[:], in_=L[1]["sc_w"]), writes=[K("scw")], dma=True)
                        P.op("sp", Q.dma_start(out=cfw[:], in_=L[1]["cf_w"]), writes=[K("cfw")], dma=True)
                        P.op("sp", Q.dma_start(out=cfv[:], in_=L[1]["cf_vec"]), writes=[K("cfv")], dma=True)
                        va = pa("va", [128, SEQ + 32], F32)
                        vb = pa("vb", [128, SEQ], F32)
                        vc = pa("vc", [128, SEQ], F32)
                        ud = pa("ud", [128, 4, SEQ], F32)
                        P.op("dve", Q.memset(va[:], 0.0), writes=[K("va")])
                        for j in range(4):
                            P.op("pool", Q.dma_start(out=wu[:, :, 0:384], in_=L[1]["w_c"][j].rearrange("(kc p) n -> p kc n", p=128)),
                                 writes=[K("wu")], dma=True)
                            proj_T(vb, [K("vb")], wu, K("wu"), 0, hT, "hT", SEQ)

                            def ev_gc(b, n, pt, pk, o):
                                P.op("dve", Q.tensor_tensor(out=o, in0=pt[:, 0:n], in1=vb[:, b * 512:b * 512 + n], op=ALU.mult),
                                     reads=[pk, K("vb")], writes=[K("va")])
                            proj_T(va, [K("va")], wu, K("wu"), 256, hT, "hT", SEQ, evac=ev_gc, dst0=1)
                            proj_T(vc, [K("vc")], wu, K("wu"), 128, hT, "hT", SEQ)
                            P.op("dve", Q.tensor_scalar(out=vb[:], in0=va[:, 0:SEQ], scalar1=scw[:, j, 0:1], scalar2=None, op0=ALU.mult),
                                 reads=[K("va"), K("scw")], writes=[K("vb")])
                            for k in (1, 2):
                                P.op("dve", Q.scalar_tensor_tensor(out=vb[:], in0=va[:, k:k + SEQ], scalar=scw[:, j, k:k + 1], in1=vb[:],
                                                                                       op0=ALU.mult, op1=ALU.add),
                                     reads=[K("va"), K("scw"), K("vb")], writes=[K("vb")])
                            P.op("dve", Q.tensor_tensor(out=yT[:], in0=vb[:], in1=vc[:], op=ALU.mult),
                                 reads=[K("vb"), K("vc")], writes=[K("yT")])
                            out_proj_unit(l, j, yT, "yT", wo)
                        P.op("dve", Q.memset(va[:], 0.0), reads=[K("va")], writes=[K("va")])
                        for j in range(4):
                            P.op("pool", Q.dma_start(out=wu[:, :, 0:256], in_=L[1]["w_d"][j].rearrange("(kc p) n -> p kc n", p=128)),
                                 writes=[K("wu")], dma=True)

                            def ev_sig(b, n, pt, pk, o):
                                P.op("act", Q.activation(out=o, in_=pt[:, 0:n], func=AF.Sigmoid), reads=[pk], writes=[K("vb")])
                            proj_T(vb, [K("vb")], wu, K("wu"), 128, hT, "hT", SEQ, evac=ev_sig)

                            def ev_glu(b, n, pt, pk, o):
                                P.op("dve", Q.tensor_tensor(out=o, in0=pt[:, 0:n], in1=vb[:, b * 512:b * 512 + n], op=ALU.mult),
                                     reads=[pk, K("vb")], writes=[K("va")])
                            proj_T(va, [K("va")], wu, K("wu"), 0, hT, "hT", SEQ, evac=ev_glu, dst0=15)
                            P.op("dve", Q.tensor_scalar(out=ud[:, j, :], in0=va[:, 0:SEQ], scalar1=cfw[:, j, 0:1], scalar2=cfv[:, 0, j:j + 1],
                                                                       op0=ALU.mult, op1=ALU.add),
                                 reads=[K("va"), K("cfw"), K("cfv")], writes=[K("ud", j)])
                            for k in range(1, 31):
                                P.op("dve", Q.scalar_tensor_tensor(out=ud[:, j, :], in0=va[:, k:k + SEQ], scalar=cfw[:, j, k:k + 1], in1=ud[:, j, :],
                                                                                       op0=ALU.mult, op1=ALU.add),
                                     reads=[K("va"), K("cfw"), K("ud", j)], writes=[K("ud", j)])
                        for b in range(4):
                            sl = slice(b * 512, (b + 1) * 512)
                            for j in range(4):
                                P.op("pe", Q.matmul(PB[2][:], ones_f[:], ud[:, j, sl], start=(j == 0), stop=(j == 3)),
                                     reads=[K("ones_f"), K("ud", j)], writes=[K("pb", 2)])
                            for j in range(4):
                                P.op("act", Q.activation(out=vb[:, j * 512:(j + 1) * 512], in_=ud[:, j, sl], func=AF.Square),
                                     reads=[K("ud", j)], writes=[K("vb")])
                            for j in range(4):
                                P.op("pe", Q.matmul(PB[3][:], ones_f[:], vb[:, j * 512:(j + 1) * 512], start=(j == 0), stop=(j == 3)),
                                     reads=[K("ones_f"), K("vb")], writes=[K("pb", 3)])
                            mean, rstd, t0, t1 = vc[:, 0:512], vc[:, 512:1024], vc[:, 1024:1536], vc[:, 1536:2048]
                            P.op("dve", Q.tensor_scalar(out=mean, in0=PB[2][:], scalar1=1.0 / 512, scalar2=None, op0=ALU.mult),
                                 reads=[K("pb", 2)], writes=[K("vc")])
                            P.op("dve", Q.tensor_tensor(out=t0, in0=mean, in1=mean, op=ALU.mult), reads=[K("vc")], writes=[K("vc")])
                            P.op("dve", Q.scalar_tensor_tensor(out=rstd, in0=PB[3][:], scalar=1.0 / 512, in1=t0, op0=ALU.mult, op1=ALU.subtract),
                                 reads=[K("pb", 3), K("vc")], writes=[K("vc")])
                            P.op("dve", Q.tensor_scalar(out=rstd, in0=rstd, scalar1=EPS, scalar2=None, op0=ALU.add), reads=[K("vc")], writes=[K("vc")])
                            P.op("act", Q.activation(out=rstd, in_=rstd, func=AF.Sqrt), reads=[K("vc")], writes=[K("vc")])
                            P.op("dve", Q.reciprocal(out=rstd, in_=rstd), reads=[K("vc")], writes=[K("vc")])
                            for j in range(4):
                                P.op("dve", Q.tensor_tensor(out=t1, in0=ud[:, j, sl], in1=mean, op=ALU.subtract),
                                     reads=[K("ud", j), K("vc")], writes=[K("vc")])
                                P.op("dve", Q.tensor_tensor(out=t1, in0=t1, in1=rstd, op=ALU.mult), reads=[K("vc")], writes=[K("vc")])
                                P.op("act", Q.activation(out=t1, in_=t1, func=AF.Identity, scale=cfv[:, 1, j:j + 1], bias=cfv[:, 2, j:j + 1]),
                                     reads=[K("vc"), K("cfv")], writes=[K("vc")])
                                P.op("act", Q.activation(out=vb[:, 0:512], in_=t1, func=AF.Sigmoid), reads=[K("vc")], writes=[K("vb")])
                                P.op("dve", Q.tensor_tensor(out=ud[:, j, sl], in0=t1, in1=vb[:, 0:512], op=ALU.mult),
                                     reads=[K("vc"), K("vb")], writes=[K("ud", j)])
                        for j in range(4):
                            P.op("act", Q.activation(out=yT[:], in_=ud[:, j, :], func=AF.Copy), reads=[K("ud", j)], writes=[K("yT")])
                            out_proj_unit(l, 4 + j, yT, "yT", wo)
                P.barrier()
                tap(f"mix{l}", s)
                stage(3 if l == 0 else 6)

                make_gate(l, 1, s)
                with Phase() as ph:
                    def pa(name, shape, dt):
                        return ph.enter_context(nc.sbuf_tensor(un(name), list(shape), dt))
                    xs2 = pa("xs2", [128, NCH, D], BF16)
                    wr = pa("wr", [128, 8, 16], BF16)
                    aff_tm = pa("aff_tm", [128, NCH, 16], F32)
                    mask_f = pa("mask_f", [128, NCH, 16], F32)
                    mask_b = pa("mask_b", [128, NCH, 16], BF16)
                    pos_tm = pa("pos_tm", [128, NCH, 16], F32)
                    R = pa("R", [128, NCH, 16, 4], BF16)
                    hi_f = pa("hi_f", [128, NCH, 16], F32)
                    P.op("pool", Q.dma_start(out=wr[:], in_=L[l]["w_router"].rearrange("(kc p) n -> p kc n", p=128)),
                         writes=[K("wr")], dma=True)
                    make_AB(l, 1, s, 1)
                    with Phase() as ph2:
                        def pb2(name, shape, dt):
                            return ph2.enter_context(nc.sbuf_tensor(un(name), list(shape), dt))
                        hTg = pb2("hTg", [128, 8, 512], BF16)
                        expT = pb2("expT", [16, SEQ], F32)
                        affT = pb2("affT", [16, SEQ], F32)
                        work = pb2("work", [16, SEQ], F32)
                        maskT = pb2("maskT", [16, SEQ], F32)
                        m8 = pb2("m8", [16, 8], F32)
                        thr = pb2("thr", [16, 1], F32)
                        for g in range(4):
                            for j in range(4):
                                c = g * 4 + j
                                rms_xs(x[:, c, :], c, xs2[:, c, :], [K("x", c)], [K("xs2", c)])
                            for dc in range(8):
                                pt = PBb[dc % 2]
                                pk = K("pb", dc % 2)
                                for j in range(4):
                                    c = g * 4 + j
                                    P.op("pe", Q.transpose(pt[:, j * 128:(j + 1) * 128], xs2[:, c, dc * 128:(dc + 1) * 128], ident_b[:]),
                                         reads=[K("xs2", c), K("ident_b")], writes=[pk])
                                P.op("act", Q.activation(out=hTg[:, dc, :], in_=pt[:, 0:512], func=AF.Identity,
                                                                                 scale=AB[:, 0, dc:dc + 1], bias=AB[:, 1, dc:dc + 1]),
                                     reads=[pk, K("AB")], writes=[K("hTg")])
                            for kc in range(8):
                                P.op("pe", Q.matmul(PB[2][0:16, :], wr[:, kc, :], hTg[:, kc, :], start=(kc == 0), stop=(kc == 7)),
                                     reads=[K("wr"), K("hTg")], writes=[K("pb", 2)])
                            P.op("act", Q.activation(out=expT[:, g * 512:(g + 1) * 512], in_=PB[2][0:16, :], func=AF.Exp),
                                 reads=[K("pb", 2)], writes=[K("expT")])
                            for j in range(4):
                                for kc in range(8):
                                    P.op("pe", Q.matmul(PB[3][:, j * 16:(j + 1) * 16], hTg[:, kc, j * 128:(j + 1) * 128], wr[:, kc, :],
                                                                              start=(kc == 0), stop=(kc == 7)),
                                         reads=[K("wr"), K("hTg")], writes=[K("pb", 3)])
                            P.op("act", Q.activation(out=aff_tm[:, g * 4:(g + 1) * 4, :].rearrange("p a b -> p (a b)"), in_=PB[3][:, 0:64], func=AF.Exp),
                                 reads=[K("pb", 3)], writes=[K("aff_tm")])
                        stage(41 if l == 0 else 410)
                        for b in range(4):
                            P.op("pe", Q.matmul(PB[2][0:16, :], ones_f[0:16, 0:16], expT[:, b * 512:(b + 1) * 512], start=True, stop=True),
                                 reads=[K("ones_f"), K("expT")], writes=[K("pb", 2)])
                            P.op("dve", Q.reciprocal(out=work[:, b * 512:(b + 1) * 512], in_=PB[2][0:16, :]),
                                 reads=[K("pb", 2)], writes=[K("work")])
                        P.op("dve", Q.tensor_tensor(out=affT[:], in0=expT[:], in1=work[:], op=ALU.mult),
                             reads=[K("expT"), K("work")], writes=[K("affT")])
                        stage(42 if l == 0 else 420)
                        P.op("dve", Q.tensor_copy(out=work[:], in_=affT[:]), reads=[K("affT")], writes=[K("work")])
                        for r in range(CAP // 8):
                            P.op("dve", Q.max(out=m8[:], in_=work[:]), reads=[K("work")], writes=[K("m8")])
                            if r < CAP // 8 - 1:
                                P.op("dve", Q.match_replace(out=work[:], in_to_replace=m8[:], in_values=work[:], imm_value=-1.0),
                                     reads=[K("m8"), K("work")], writes=[K("work")])
                        P.op("dve", Q.tensor_reduce(out=thr[:], in_=m8[:], axis=AX.X, op=ALU.min), reads=[K("m8")], writes=[K("thr")])
                        P.op("dve", Q.tensor_scalar(out=maskT[:], in0=affT[:], scalar1=thr[:, 0:1], scalar2=None, op0=ALU.is_ge),
                             reads=[K("affT"), K("thr")], writes=[K("maskT")])
                        stage(43 if l == 0 else 430)
                        for c in range(NCH):
                            P.op("pe", Q.matmul(PB[0][:, c * 16:(c + 1) * 16], maskT[:, c * 128:(c + 1) * 128], ident_f[0:16, 0:16], start=True, stop=True),
                                 reads=[K("maskT"), K("ident_f")], writes=[K("pb", 0)])
                        stage(44)
                        P.op("act", Q.activation(out=mask_f[:].rearrange("p a b -> p (a b)"), in_=PB[0][:, 0:256], func=AF.Copy),
                             reads=[K("pb", 0)], writes=[K("mask_f")])
                        stage(45)
                        P.op("dve", Q.tensor_copy(out=mask_b[:], in_=mask_f[:]),
                             reads=[K("mask_f")], writes=[K("mask_b")])
                        stage(46)
                    P.barrier()
                    stage(4 if l == 0 else 40)
                    for c in range(NCH):
                        for c2 in range(c + 1):
                            lt = ltri_b if c2 == c else ones_b
                            P.op("pe", Q.matmul(PB[1][:, c * 16:(c + 1) * 16], lt[:], mask_b[:, c2, :], start=(c2 == 0), stop=(c2 == c)),
                                 reads=[K("ltri_b"), K("ones_b"), K("mask_b")], writes=[K("pb", 1)])
                    P.op("act", Q.activation(out=pos_tm[:].rearrange("p a b -> p (a b)"), in_=PB[1][:, 0:256], func=AF.Copy),
                         reads=[K("pb", 1)], writes=[K("pos_tm")])
                    P.op("dve", Q.reduce_sum(out=hi_f[:, :, 0], in_=aff_tm[:], axis=AX.X), reads=[K("aff_tm")], writes=[K("hi_f")])
                    P.op("dve", Q.reciprocal(out=hi_f[:, :, 1], in_=hi_f[:, :, 0]), reads=[K("hi_f")], writes=[K("hi_f")])
                    for c in range(NCH):
                        P.op("dve", Q.tensor_scalar(out=aff_tm[:, c, :], in0=aff_tm[:, c, :], scalar1=hi_f[:, c, 1:2], scalar2=None, op0=ALU.mult),
                             reads=[K("aff_tm"), K("hi_f")], writes=[K("aff_tm")])
                    P.op("dve", Q.tensor_copy(out=R[:, :, :, 0], in_=aff_tm[:]), reads=[K("aff_tm")], writes=[K("R")])
                    P.op("dve", Q.tensor_copy(out=hi_f[:], in_=R[:, :, :, 0]), reads=[K("R"), K("aff_tm")], writes=[K("hi_f")])
                    P.op("dve", Q.tensor_tensor(out=R[:, :, :, 1], in0=aff_tm[:], in1=hi_f[:], op=ALU.subtract),
                         reads=[K("aff_tm"), K("hi_f")], writes=[K("R")])
                    for q in range(2):
                        P.op("dve", Q.tensor_copy(out=R[:, :, :, 2 + q], in_=cidx[:, :, q:q + 1].to_broadcast([128, NCH, 16])),
                             reads=[K("cidx")], writes=[K("R")])

                    Se = pa("Se", [128, NCH, CAP], BF16)
                    SeT = pa("SeT", [128, 2, SEQ], BF16)
                    gi = pa("gi", [128, 2, 4], F32)
                    xinT = pa("xinT", [128, 8, CAP], BF16)
                    hidT = pa("hidT", [128, NFC, CAP], BF16)
                    sgt = pa("sgt", [128, CAP], F32)
                    yg = pa("yg", [128, 2, D], BF16)
                    w13 = [pa(f"w13_{k}", [128, 2, 8, 256], BF16) for k in range(2)]
                    w2b = [pa(f"w2_{k}", [128, 2, D], BF16) for k in range(2)]
                    wcount = [0, 0]
                    for ex in range(E):
                        for c in range(NCH):
                            P.op("dve", Q.tensor_scalar(out=Se[:, c, :], in0=iota_t[:, 0:CAP], scalar1=pos_tm[:, c, ex:ex + 1],
                                                                              scalar2=mask_f[:, c, ex:ex + 1], op0=ALU.is_equal, op1=ALU.mult),
                                 reads=[K("iota_t"), K("pos_tm"), K("mask_f")], writes=[K("Se", c)])
                        for jc in range(2):
                            for c in range(NCH):
                                P.op("pe", Q.matmul(PB[0][:, jc * 4:(jc + 1) * 4], Se[:, c, jc * 128:(jc + 1) * 128], R[:, c, ex, :],
                                                                                start=(c == 0), stop=(c == NCH - 1)),
                                     reads=[K("Se", c), K("R")], writes=[K("pb", 0)])
                        P.op("act", Q.activation(out=gi[:].rearrange("p a b -> p (a b)"), in_=PB[0][:, 0:8], func=AF.Copy),
                             reads=[K("pb", 0)], writes=[K("gi")])
                        P.op("dve", Q.tensor_tensor(out=gi[:, :, 0], in0=gi[:, :, 0], in1=gi[:, :, 1], op=ALU.add), reads=[K("gi")], writes=[K("gi")])
                        P.op("dve", Q.scalar_tensor_tensor(out=gi[:, :, 2], in0=gi[:, :, 2], scalar=128.0, in1=gi[:, :, 3], op0=ALU.mult, op1=ALU.add),
                             reads=[K("gi")], writes=[K("gi")])
                        for jc in range(2):
                            P.op("dve", Q.tensor_scalar(out=SeT[:, jc, :], in0=iota_t[:], scalar1=gi[:, jc, 2:3], scalar2=None, op0=ALU.is_equal),
                                 reads=[K("iota_t"), K("gi")], writes=[K("SeT", jc)])
                        for dc in range(8):
                            bk = dc % 2
                            for c in range(NCH):
                                P.op("pe", Q.matmul(PB[bk][:, 0:CAP], xs2[:, c, dc * 128:(dc + 1) * 128], Se[:, c, :],
                                                                                start=(c == 0), stop=(c == NCH - 1)),
                                     reads=[K("xs2", c), K("Se", c)], writes=[K("pb", bk)])
                            P.op("act", Q.activation(out=xinT[:, dc, :], in_=PB[bk][:, 0:CAP], func=AF.Identity,
                                                                             scale=AB[:, 0, dc:dc + 1], bias=AB[:, 1, dc:dc + 1]),
                                 reads=[K("pb", bk), K("AB")], writes=[K("xinT")])
                        for fg in range(NFC // 2):
                            slot = wcount[0] % 2
                            wcount[0] += 1
                            wt = w13[slot]
                            for wi, nm in enumerate(("w1", "w3")):
                                src = L[l][nm][ex][:, fg * 256:(fg + 1) * 256].rearrange("(kc p) n -> p kc n", p=128)
                                P.op("pool", Q.dma_start(out=wt[:, wi], in_=src), writes=[K("w13", slot, wi)], dma=True)
                            for f2 in range(2):
                                fc = fg * 2 + f2
                                bk = 2 + (fc % 2)
                                for wi in range(2):
                                    for kc in range(8):
                                        P.op("pe", Q.matmul(
                                            PB[bk][:, wi * CAP:(wi + 1) * CAP], wt[:, wi, kc, f2 * 128:(f2 + 1) * 128], xinT[:, kc, :],
                                            start=(kc == 0), stop=(kc == 7)),
                                            reads=[K("w13", slot, wi), K("xinT")], writes=[K("pb", bk)])
                                P.op("act", Q.activation(out=sgt[:], in_=PB[bk][:, 0:CAP], func=AF.Sigmoid), reads=[K("pb", bk)], writes=[K("sgt")])
                                P.op("dve", Q.tensor_tensor(out=sgt[:], in0=PB[bk][:, 0:CAP], in1=sgt[:], op=ALU.mult),
                                     reads=[K("pb", bk), K("sgt")], writes=[K("sgt")])
                                P.op("dve", Q.tensor_tensor(out=hidT[:, fc, :], in0=PB[bk][:, CAP:2 * CAP], in1=sgt[:], op=ALU.mult),
                                     reads=[K("pb", bk), K("sgt")], writes=[K("hidT")])
                        for fg in range(NFC // 2):
                            slot = wcount[1] % 2
                            wcount[1] += 1
                            wt = w2b[slot]
                            src = L[l]["w2"][ex][fg * 256:(fg + 1) * 256, :].rearrange("(a p) n -> p a n", p=128)
                            P.op("pool", Q.dma_start(out=wt[:], in_=src), writes=[K("w2b", slot)], dma=True)
                            for f2 in range(2):
                                fc = fg * 2 + f2
                                for jc in range(2):
                                    for h in range(2):
                                        bk = 4 + jc * 2 + h
                                        P.op("pe", Q.matmul(
                                            PB[bk][:], hidT[:, fc, jc * 128:(jc + 1) * 128], wt[:, f2, h * 512:(h + 1) * 512],
                                            start=(fc == 0), stop=(fc == NFC - 1)),
                                            reads=[K("hidT"), K("w2b", slot)], writes=[K("pb", bk)])
                        for jc in range(2):
                            for h in range(2):
                                bk = 4 + jc * 2 + h
                                P.op("dve", Q.scalar_tensor_tensor(
                                    out=yg[:, jc, h * 512:(h + 1) * 512], in0=PB[bk][:], scalar=gi[:, jc, 0:1], in1=gate_b[:, h * 512:(h + 1) * 512],
                                    op0=ALU.mult, op1=ALU.mult), reads=[K("pb", bk), K("gi"), K("gate_b")], writes=[K("yg")])
                        for c in range(NCH):
                            for h in range(2):
                                bk = (c * 2 + h) % 2
                                for jc in range(2):
                                    P.op("pe", Q.matmul(
                                        PB[bk][:], SeT[:, jc, c * 128:(c + 1) * 128], yg[:, jc, h * 512:(h + 1) * 512], start=(jc == 0), stop=(jc == 1)),
                                        reads=[K("SeT", jc), K("yg")], writes=[K("pb", bk)])
                                P.op("dve", Q.tensor_tensor(
                                    out=x[:, c, h * 512:(h + 1) * 512], in0=PB[bk][:], in1=x[:, c, h * 512:(h + 1) * 512], op=ALU.add),
                                    reads=[K("pb", bk), K("x", c)], writes=[K("x", c)])
                P.barrier()
                tap(f"moe{l}", s)
                stage(5 if l == 0 else 7)

            with Phase() as ph:
                ob = [ph.enter_context(nc.sbuf_tensor(un(f"ob{k}"), [128, D], F32)) for k in range(2)]
                for c in range(NCH):
                    o = ob[c % 2]
                    ok = K("ob", c % 2)
                    sl = c % 16
                    sq, rs = stat[:, 2 * sl:2 * sl + 1], stat[:, 2 * sl + 1:2 * sl + 2]
                    sk = K("stat", sl)
                    P.op("act", Q.activation(out=o[:], in_=x[:, c, :], func=AF.Square),
                         reads=[K("x", c)], writes=[ok])
                    P.op("dve", Q.reduce_sum(out=sq, in_=o[:], axis=AX.X), reads=[ok], writes=[sk])
                    P.op("dve", Q.tensor_scalar(out=rs, in0=sq, scalar1=1.0 / D, scalar2=EPS, op0=ALU.mult, op1=ALU.add),
                         reads=[sk], writes=[sk])
                    P.op("act", Q.activation(out=rs, in_=rs, func=AF.Sqrt), reads=[sk], writes=[sk])
                    P.op("dve", Q.reciprocal(out=rs, in_=rs), reads=[sk], writes=[sk])
                    P.op("dve", Q.scalar_tensor_tensor(out=o[:], in0=x[:, c, :], scalar=rs, in1=fnorm_b[:], op0=ALU.mult, op1=ALU.mult),
                         reads=[K("x", c), sk, K("fnorm_b")], writes=[ok])
                    P.op("sp", Q.dma_start(out=out_d[s, c * 128:(c + 1) * 128, :], in_=o[:]), reads=[ok], dma=True)
            P.barrier()
    except _Stop:
        P.barrier()
        P.op("sp", Q.dma_start(out=out_d[0].rearrange("(c p) d -> p c d", p=128), in_=x[:]), reads=[K("x", c) for c in range(NCH)], dma=True)
        P.barrier()

    P.emit(st)
    st.close()
    return nc


def _col(v):
    return np.ascontiguousarray(v.reshape(-1, 128).T)


def prep_shared(inp, E):
    f = np.float32
    sh = {}
    perm = _rope_perm()
    for l in range(2):
        p = f"l{l}_"
        sh[p + "normcols"] = np.ascontiguousarray(np.stack([_col(inp[p + "norm1"]), _col(inp[p + "norm2"])], 1)).astype(f)
        sh[p + "w_mod"] = np.ascontiguousarray(inp[p + "w_mod"])
        sh[p + "b_mod"] = np.ascontiguousarray(inp[p + "b_mod"].reshape(1, -1))
        sh[p + "w_out"] = np.ascontiguousarray(inp[p + "w_out"])
        sh[p + "w_router"] = np.ascontiguousarray(inp[p + "w_router"])
        sh[p + "w1"] = np.ascontiguousarray(inp[p + "w1"][:E])
        sh[p + "w3"] = np.ascontiguousarray(inp[p + "w3"][:E])
        sh[p + "w2"] = np.ascontiguousarray(inp[p + "w2"][:E])
    w = inp["l0_w_in"]
    aq, bq, ak, bk, av, bv = (w[:, i * 512:(i + 1) * 512] for i in range(6))
    na = [np.concatenate([aq[:, i * 128:(i + 1) * 128], ak[:, i * 128:(i + 1) * 128], av[:, i * 128:(i + 1) * 128]], 1) for i in range(4)]
    sh["l0_w_na"] = np.ascontiguousarray(np.stack(na, 0))
    pidx = np.concatenate([perm, 64 + perm])
    df = []
    for h in range(4):
        q = bq[:, h * 128:(h + 1) * 128]
        k = bk[:, h * 128:(h + 1) * 128]
        df.append(np.concatenate([q, q[:, pidx], k, k[:, pidx], bv[:, h * 128:(h + 1) * 128]], 1))
    sh["l0_w_df"] = np.ascontiguousarray(np.stack(df, 0))
    sh["l0_na_bias"] = np.ascontiguousarray(_na_bias_tables(inp["l0_rpb"]).reshape(8, 128, 14 * 256))
    sh["l0_lam"] = np.ascontiguousarray(np.concatenate([inp["l0_lam_q1"], inp["l0_lam_k1"], inp["l0_lam_q2"], inp["l0_lam_k2"]], 0).reshape(1, 256))
    sh["l0_subln"] = np.ascontiguousarray(inp["l0_subln"].reshape(1, 128))
    w = inp["l1_w_in"]
    xc, gb, gc, ga, gg = (w[:, i * 512:(i + 1) * 512] for i in range(5))
    sh["l1_w_c"] = np.ascontiguousarray(np.stack([np.concatenate([xc[:, j * 128:(j + 1) * 128], gb[:, j * 128:(j + 1) * 128], gc[:, j * 128:(j + 1) * 128]], 1) for j in range(4)], 0))
    sh["l1_w_d"] = np.ascontiguousarray(np.stack([np.concatenate([ga[:, j * 128:(j + 1) * 128], gg[:, j * 128:(j + 1) * 128]], 1) for j in range(4)], 0))
    sh["l1_sc_w"] = np.ascontiguousarray(inp["l1_sc_w"].T.reshape(4, 128, 3).transpose(1, 0, 2))
    sh["l1_cf_w"] = np.ascontiguousarray(inp["l1_cf_w"].T.reshape(4, 128, 31).transpose(1, 0, 2))
    sh["l1_cf_vec"] = np.ascontiguousarray(np.stack([_col(inp["l1_cf_b"]), _col(inp["l1_ln_g"]), _col(inp["l1_ln_b"])], 1))
    sh["final_norm"] = np.ascontiguousarray(inp["final_norm"].reshape(1, -1))
    cos, sin = _rope_tables()
    sh["rope"] = np.ascontiguousarray(np.stack([cos, sin], 0))
    import ml_dtypes
    ci = np.zeros((128, 16, 2), np.float32)
    ci[:, :, 0] = np.arange(16)[None, :]
    ci[:, :, 1] = np.arange(128)[:, None]
    sh["cidx"] = ci.astype(ml_dtypes.bfloat16)
    sh["ident"] = np.eye(128, dtype=f)
    sh["ltri"] = np.triu(np.ones((128, 128), f), 1)
    sh["iota"] = np.arange(SEQ, dtype=f).reshape(1, SEQ)
    return sh


def core_inputs(inp, sh, samples):
    S = len(samples)
    m = dict(sh)
    m["x"] = np.ascontiguousarray(inp["x"][samples])
    m["ctx"] = np.ascontiguousarray(inp["ctx"][samples])
    cc = np.concatenate([inp["c"][samples], inp["c_ctx"][None, :]], 0)
    m["cT"] = np.ascontiguousarray(cc.reshape(S + 1, 8, 128).transpose(2, 1, 0))
    return m


NCORES = 8
_cache = {}


def kernel(**inputs):
    inp = {k: np.asarray(v) for k, v in inputs.items()}
    B = inp["x"].shape[0]
    S = B // NCORES
    E = 16
    key = (S, E)
    if key not in _cache:
        _cache[key] = build(S, E)
    nc = _cache[key]
    sh = prep_shared(inp, E)
    in_maps = [core_inputs(inp, sh, list(range(c * S, (c + 1) * S))) for c in range(NCORES)]
    res = run_bass_kernel_spmd(nc, in_maps, core_ids=list(range(NCORES)))
    out = np.concatenate([res.results[c]["out"] for c in range(NCORES)], 0)
    return out.astype(np.float32)
```

```python
import math
from contextlib import ExitStack
import numpy as np
import concourse.bass as bass
import concourse.mybir as mybir
from concourse.bass_utils import run_bass_kernel_spmd

F32 = mybir.dt.float32
BF16 = mybir.dt.bfloat16
AF = mybir.ActivationFunctionType
ALU = mybir.AluOpType
AX = mybir.AxisListType

D = 1024
SEQ = 2048
NCH = 16
CTX = 256
DE = 2816
NFC = 22
CAP = 256
EPS = 1e-6
GRID_W = 64
NEG = -30000.0


class _Rec:
    def __getattr__(self, name):
        def mk(*a, **k):
            return lambda e: getattr(e, name)(*a, **k)
        return mk


Q = _Rec()


class Prog:
    ENGS = ("pe", "act", "dve", "pool", "sp")
    INORDER = ("pe", "act", "dve")

    def __init__(self, nc):
        self.nc = nc
        self.ops = {e: [] for e in self.ENGS}
        self.last_w = {}
        self.readers = {}
        self.dma_since_barrier = []

    def op(self, eng, fn, reads=(), writes=(), dma=False):
        idx = len(self.ops[eng])
        deps = set()
        for k in reads:
            w = self.last_w.get(k)
            if w is not None:
                deps.add(w)
        for k in writes:
            w = self.last_w.get(k)
            if w is not None:
                deps.add(w)
            for r in self.readers.get(k, ()):
                deps.add(r)
        if eng == "pe":
            deps = {d for d in deps if not (d[0] == "pe" and not self.ops["pe"][d[1]]["dma"])}
        self.ops[eng].append(dict(fn=fn, deps=deps, inc=False, dma=dma))
        me = (eng, idx)
        for k in reads:
            lst = self.readers.setdefault(k, [])
            if not dma and eng in self.INORDER:
                lst[:] = [r for r in lst if not (r[0] == eng and not self.ops[eng][r[1]]["dma"])]
            lst.append(me)
        for k in writes:
            self.last_w[k] = me
            self.readers[k] = []
        if dma:
            self.dma_since_barrier.append(me)
        return me

    def barrier(self):
        targets = set(self.dma_since_barrier)
        for e in self.ENGS:
            for i in range(len(self.ops[e]) - 1, -1, -1):
                if not self.ops[e][i]["dma"] and self.ops[e][i]["fn"] is not None:
                    targets.add((e, i))
                    break
        for e in self.ENGS:
            self.ops[e].append(dict(fn=None, deps=set(targets), inc=False, dma=False))
        self.dma_since_barrier = []

    def emit(self, stack):
        nc = self.nc
        for e in self.ENGS:
            for o in self.ops[e]:
                for (de, di) in o["deps"]:
                    t = self.ops[de][di]
                    if not t["dma"]:
                        t["inc"] = True
        EPOCH = 30000
        ev = {}
        for e in self.ENGS:
            cnt = 0
            sem = None
            for i, o in enumerate(self.ops[e]):
                if o["dma"] or not o["inc"]:
                    continue
                if sem is None or cnt >= EPOCH:
                    sem = stack.enter_context(nc.semaphore(f"s_{e}_{i}"))
                    cnt = 0
                cnt += 1
                ev[(e, i)] = (sem, cnt)
                o["sem"] = sem
        NDS = 12
        for e in self.ENGS:
            if not any(o["dma"] for o in self.ops[e]):
                continue
            pool = [stack.enter_context(nc.semaphore(f"d_{e}_{k}")) for k in range(NDS)]
            vals = [0] * NDS
            prev = [None] * NDS
            k = 0
            for i, o in enumerate(self.ops[e]):
                if not o["dma"]:
                    continue
                slot = k % NDS
                k += 1
                vals[slot] += 16
                ev[(e, i)] = (pool[slot], vals[slot])
                o["sem"] = pool[slot]
                if prev[slot] is not None:
                    o["deps"].add(prev[slot])
                prev[slot] = (e, i)
        engobj = dict(pe=nc.tensor, act=nc.scalar, dve=nc.vector, pool=nc.gpsimd, sp=nc.sync)
        ops = self.ops

        def run(ename):
            def body(eng):
                waited = {}
                for o in ops[ename]:
                    for d in sorted(o["deps"]):
                        sem, val = ev[d]
                        key = id(sem)
                        if waited.get(key, 0) >= val:
                            continue
                        waited[key] = val
                        eng.wait_ge(sem, val)
                    if o["fn"] is None:
                        continue
                    ins = o["fn"](eng)
                    if o["dma"]:
                        ins.then_inc(o["sem"], 16)
                    elif o["inc"]:
                        ins.then_inc(o["sem"], 1)
            return body

        with nc.Block() as block:
            block.tensor(run("pe"))
            block.scalar(run("act"))
            block.vector(run("dve"))
            block.gpsimd(run("pool"))
            block.sync(run("sp"))


def _rope_tables():
    n_freq = 16
    freqs = (np.float32(10000.0) ** (-np.arange(n_freq, dtype=np.float32) / n_freq)).astype(np.float32)
    t = np.arange(SEQ)
    row = (t // GRID_W).astype(np.float32)
    col = (t % GRID_W).astype(np.float32)
    cos = np.zeros((64, SEQ), np.float32)
    sin = np.zeros((64, SEQ), np.float32)
    for f in range(64):
        pos = row if f < 32 else col
        i = f % 32
        ang = (pos * freqs[i % 16]).astype(np.float32)
        cos[f] = np.cos(ang)
        sin[f] = np.sin(ang) * (-1.0 if i < 16 else 1.0)
    return np.concatenate([cos, cos], 0), np.concatenate([sin, sin], 0)


def _rope_perm():
    p = np.zeros(64, np.int64)
    for f in range(64):
        base = 0 if f < 32 else 32
        i = f % 32
        p[f] = base + (i + 16 if i < 16 else i - 16)
    return p


def _na_blocks():
    out = []
    for b in range(8):
        if b == 0:
            out.append(list(range(0, 4)))
        elif b == 7:
            out.append(list(range(12, 16)))
        else:
            out.append(list(range(2 * b - 2, 2 * b + 4)))
    return out


def _na_class(b):
    return 0 if b == 0 else (2 if b == 7 else 1)


NA_CLS_OFF = [0, 4, 10]
NA_CLS_B = [0, 1, 7]


def _na_bias_tables(rpb):
    H = rpb.shape[0]
    tab = np.full((H, 128, 14, 256), NEG, np.float32)
    blocks = _na_blocks()
    kp = np.arange(128)
    kr_l = kp // 64
    kc = kp % 64
    qq = np.arange(256)
    qr_l = qq // 64
    qc = qq % 64
    cs = np.clip(qc - 8, 0, 48)
    for cls in range(3):
        b = NA_CLS_B[cls]
        for si, c in enumerate(blocks[b]):
            kr = 2 * c + kr_l
            r = 4 * b + qr_l
            rs = np.clip(r - 4, 0, 24)
            inrow = (kr[:, None] >= rs[None, :]) & (kr[:, None] <= rs[None, :] + 7)
            incol = (kc[:, None] >= cs[None, :]) & (kc[:, None] < cs[None, :] + 16)
            ok = inrow & incol
            dr = np.clip(kr[:, None] - r[None, :] + 7, 0, 14)
            dc = np.clip(kc[:, None] - qc[None, :] + 15, 0, 30)
            g = rpb[:, dr, dc]
            tab[:, :, NA_CLS_OFF[cls] + si, :] = np.where(ok[None], g, np.float32(NEG))
    return tab


class _Stop(Exception):
    pass


class Phase(ExitStack):
    def __exit__(self, et, ev, tb):
        super().__exit__(None, None, None)
        return False


def build(S, E, taps=(), stop=None):
    nc = bass.Bass("TRN2", target_bir_lowering=False)
    P = Prog(nc)
    st = ExitStack()

    def din(name, shape, dt=F32):
        return nc.dram_tensor(name, list(shape), dt, kind="ExternalInput").ap()

    x_d = din("x", [S, SEQ, D])
    c_d = din("cT", [128, 8, S + 1])
    ctx_d = din("ctx", [S, CTX, D])
    out_d = nc.dram_tensor("out", [S, SEQ, D], F32, kind="ExternalOutput").ap()
    tap_d = {t: nc.dram_tensor(f"tap_{t}", [SEQ, D], F32, kind="ExternalOutput").ap() for t in taps}
    L = []
    for l in range(2):
        p = {}
        p["normcols"] = din(f"l{l}_normcols", [128, 2, 8])
        p["w_mod"] = din(f"l{l}_w_mod", [D, 6 * D])
        p["b_mod"] = din(f"l{l}_b_mod", [1, 6 * D])
        p["w_out"] = din(f"l{l}_w_out", [D, D])
        p["w_router"] = din(f"l{l}_w_router", [D, 16])
        p["w1"] = din(f"l{l}_w1", [E, D, DE])
        p["w3"] = din(f"l{l}_w3", [E, D, DE])
        p["w2"] = din(f"l{l}_w2", [E, DE, D])
        if l == 0:
            p["w_na"] = din("l0_w_na", [4, D, 384])
            p["w_df"] = din("l0_w_df", [4, D, 640])
            p["na_bias"] = din("l0_na_bias", [8, 128, 14 * 256])
            p["lam"] = din("l0_lam", [1, 256])
            p["subln"] = din("l0_subln", [1, 128])
        else:
            p["w_c"] = din("l1_w_c", [4, D, 384])
            p["w_d"] = din("l1_w_d", [4, D, 256])
            p["sc_w"] = din("l1_sc_w", [128, 4, 3])
            p["cf_w"] = din("l1_cf_w", [128, 4, 31])
            p["cf_vec"] = din("l1_cf_vec", [128, 3, 4])
        L.append(p)
    fnorm_d = din("final_norm", [1, D])
    rope_d = din("rope", [2, 128, SEQ])
    cidx_d = din("cidx", [128, 16, 2], BF16)
    ident_d = din("ident", [128, 128])
    ltri_d = din("ltri", [128, 128])
    iota_d = din("iota", [1, SEQ])

    uid = [0]

    def un(name):
        uid[0] += 1
        return f"t{uid[0]}_{name}"

    def sb(name, shape, dt):
        return st.enter_context(nc.sbuf_tensor(un(name), list(shape), dt))

    def ps(name, shape, dt=F32):
        return st.enter_context(nc.psum_tensor(name, list(shape), dt))

    x = sb("x", [128, NCH, D], F32)
    ident_f = sb("ident_f", [128, 128], F32)
    ident_b = sb("ident_b", [128, 128], BF16)
    ones_f = sb("ones_f", [128, 128], F32)
    ltri_b = sb("ltri_b", [128, 128], BF16)
    ones_b = sb("ones_b", [128, 128], BF16)
    iota_t = sb("iota_t", [128, SEQ], F32)
    cidx = sb("cidx_sb", [128, 16, 2], BF16)
    modc = [sb(f"modc{l}", [128, 48, S + 1], F32) for l in range(2)]
    normc = [sb(f"normc{l}", [128, 2, 8], F32) for l in range(2)]
    siluc = sb("siluc", [128, 8, S + 1], BF16)
    AB = sb("AB", [128, 2, 8], F32)
    gate_b = sb("gate_b", [128, D], F32)
    small = sb("small", [128, 64], F32)
    stat = sb("stat", [128, 32], F32)
    junk = sb("junk", [128, D], F32)

    PB = [ps(f"pb{k}", [128, 512], F32) for k in range(8)]
    PBb = [t[:].bitcast(BF16) for t in PB]

    def K(*a):
        return a

    def stage(n):
        if stop is not None and n == stop:
            raise _Stop()

    P.op("sp", Q.dma_start(out=ident_f[:], in_=ident_d), writes=[K("ident_f")], dma=True)
    P.op("pool", Q.dma_start(out=ident_b[:], in_=ident_d), writes=[K("ident_b")], dma=True)
    P.op("pool", Q.dma_start(out=ltri_b[:], in_=ltri_d), writes=[K("ltri_b")], dma=True)
    P.op("sp", Q.dma_start(out=iota_t[:], in_=iota_d.partition_broadcast(128)), writes=[K("iota_t")], dma=True)
    P.op("sp", Q.dma_start(out=cidx[:], in_=cidx_d), writes=[K("cidx")], dma=True)
    P.op("dve", Q.memset(ones_f[:], 1.0), writes=[K("ones_f")])
    P.op("dve", Q.memset(ones_b[:], 1.0), writes=[K("ones_b")])
    for l in range(2):
        P.op("sp", Q.dma_start(out=normc[l][:], in_=L[l]["normcols"]), writes=[K("normc", l)], dma=True)

    with Phase() as ph:
        cT = ph.enter_context(nc.sbuf_tensor(un("cT_sb"), [128, 8, S + 1], F32))
        sg = ph.enter_context(nc.sbuf_tensor(un("sg_sb"), [128, 8, S + 1], F32))
        bm = ph.enter_context(nc.sbuf_tensor(un("bm_sb"), [1, 6 * D], BF16))
        wm = [ph.enter_context(nc.sbuf_tensor(un(f"wm{k}"), [128, 8, 512], BF16)) for k in range(2)]
        P.op("sp", Q.dma_start(out=cT[:], in_=c_d), writes=[K("cT")], dma=True)
        P.op("act", Q.activation(out=sg[:], in_=cT[:], func=AF.Sigmoid), reads=[K("cT")], writes=[K("sg")])
        P.op("dve", Q.tensor_tensor(out=siluc[:], in0=cT[:], in1=sg[:], op=ALU.mult),
             reads=[K("cT"), K("sg")], writes=[K("siluc")])
        blk = 0
        for l in range(2):
            P.op("pool", Q.dma_start(out=bm[:], in_=L[l]["b_mod"]), writes=[K("bm")], dma=True)
            for cb in range(12):
                w = wm[blk % 2]
                wk = K("wm", blk % 2)
                blk += 1
                src = L[l]["w_mod"][:, cb * 512:(cb + 1) * 512].rearrange("(kc p) n -> p kc n", p=128)
                P.op("pool", Q.dma_start(out=w[:], in_=src), writes=[wk], dma=True)
                pt = PB[cb % 2]
                pk = K("pb", cb % 2)
                for fcl in range(4):
                    o = pt[:, fcl * (S + 1):(fcl + 1) * (S + 1)]
                    for kc in range(8):
                        P.op("pe", Q.matmul(
                            o, w[:, kc, fcl * 128:(fcl + 1) * 128], siluc[:, kc, :], start=(kc == 0), stop=False),
                            reads=[wk, K("siluc")], writes=[pk])
                    f0 = cb * 512 + fcl * 128
                    P.op("pe", Q.matmul(
                        o, bm[0:1, f0:f0 + 128], ones_b[0:1, 0:S + 1], start=False, stop=True),
                        reads=[K("bm"), K("ones_b")], writes=[pk])
                P.op("dve", Q.tensor_copy(
                    out=modc[l][:, cb * 4:(cb + 1) * 4, :],
                    in_=pt[:, 0:4 * (S + 1)].rearrange("p (a b) -> p a b", b=S + 1)),
                    reads=[pk], writes=[K("modc", l)])
    P.barrier()

    def make_AB(l, which, col, nkey):
        sh = 0 if which == 0 else 24
        P.op("dve", Q.scalar_tensor_tensor(
            out=AB[:, 0, :], in0=modc[l][:, sh + 8:sh + 16, col], scalar=1.0, in1=normc[l][:, nkey, :],
            op0=ALU.add, op1=ALU.mult), reads=[K("modc", l), K("normc", l)], writes=[K("AB")])
        P.op("dve", Q.tensor_copy(out=AB[:, 1, :], in_=modc[l][:, sh:sh + 8, col]),
             reads=[K("modc", l)], writes=[K("AB")])

    def make_gate(l, which, col):
        g0 = 16 if which == 0 else 40
        with Phase() as ph:
            dg = ph.enter_context(nc.sbuf_tensor(un("dg"), [128, 8, 128], F32))
            for kc in range(8):
                P.op("dve", Q.tensor_scalar(
                    out=dg[:, kc, :], in0=ident_f[:], scalar1=modc[l][:, g0 + kc, col:col + 1], scalar2=None,
                    op0=ALU.mult), reads=[K("ident_f"), K("modc", l)], writes=[K("dg", kc)])
            for h in range(2):
                for k4 in range(4):
                    kc = h * 4 + k4
                    P.op("pe", Q.matmul(
                        PB[h][:, k4 * 128:(k4 + 1) * 128], ones_f[:], dg[:, kc, :], start=True, stop=True),
                        reads=[K("ones_f"), K("dg", kc)], writes=[K("pb", h)])
                P.op("act", Q.activation(out=gate_b[:, h * 512:(h + 1) * 512], in_=PB[h][:], func=AF.Copy),
                     reads=[K("pb", h)], writes=[K("gate_b")])
        P.barrier()

    def rms_xs(src, c, xs_out, keys_r, keys_w, width=D):
        sl = c % 16
        sq = stat[:, 2 * sl:2 * sl + 1]
        rs = stat[:, 2 * sl + 1:2 * sl + 2]
        sk = K("stat", sl)
        P.op("act", Q.activation(out=junk[:, 0:width], in_=src, func=AF.Square),
             reads=keys_r, writes=[K("junk")])
        P.op("dve", Q.reduce_sum(out=sq, in_=junk[:, 0:width], axis=AX.X),
             reads=[K("junk")], writes=[sk])
        P.op("dve", Q.tensor_scalar(out=rs, in0=sq, scalar1=1.0 / width, scalar2=EPS, op0=ALU.mult, op1=ALU.add),
             reads=[sk], writes=[sk])
        P.op("act", Q.activation(out=rs, in_=rs, func=AF.Sqrt), reads=[sk], writes=[sk])
        P.op("dve", Q.reciprocal(out=rs, in_=rs), reads=[sk], writes=[sk])
        P.op("dve", Q.tensor_scalar(out=xs_out, in0=src, scalar1=rs, scalar2=None, op0=ALU.mult),
             reads=keys_r + [sk], writes=keys_w)

    def norm_T(hT, hT_key, ntok_chunks, src_chunk, src_keys, xsg, group_cb=None, xs_keep=None):
        ng = ntok_chunks // 4
        for g in range(ng):
            for j in range(4):
                c = g * 4 + j
                if xs_keep is not None:
                    xo = xs_keep[:, c, :]
                    kw = [K("xs2", c)]
                else:
                    xo = xsg[:, j, :]
                    kw = [K("xsg", j)]
                rms_xs(src_chunk(c), c, xo, src_keys(c), kw)
            for dc in range(8):
                pt = PBb[dc % 2]
                pk = K("pb", dc % 2)
                for j in range(4):
                    c = g * 4 + j
                    if xs_keep is not None:
                        xi = xs_keep[:, c, dc * 128:(dc + 1) * 128]
                        kr = [K("xs2", c)]
                    else:
                        xi = xsg[:, j, dc * 128:(dc + 1) * 128]
                        kr = [K("xsg", j)]
                    P.op("pe", Q.transpose(pt[:, j * 128:(j + 1) * 128], xi, ident_b[:]),
                         reads=kr + [K("ident_b")], writes=[pk])
                P.op("act", Q.activation(
                    out=hT[:, dc, g * 512:(g + 1) * 512], in_=pt[:, 0:512], func=AF.Identity,
                    scale=AB[:, 0, dc:dc + 1], bias=AB[:, 1, dc:dc + 1]),
                    reads=[pk, K("AB")], writes=[K(hT_key, g)])
            if group_cb is not None:
                group_cb(g)

    def out_proj_unit(l, u, yT, yT_key, wo):
        P.op("pool", Q.dma_start(out=wo[0][:], in_=L[l]["w_out"][u * 128:(u + 1) * 128, :]),
             writes=[K("wo_f")], dma=True)
        P.op("dve", Q.tensor_tensor(out=wo[1][:], in0=wo[0][:], in1=gate_b[:], op=ALU.mult),
             reads=[K("wo_f"), K("gate_b")], writes=[K("wo_b")])
        for c in range(NCH):
            for h in range(2):
                bk = 6 + h
                P.op("pe", Q.matmul(
                    PB[bk][:], yT[:, c * 128:(c + 1) * 128], wo[1][:, h * 512:(h + 1) * 512], start=True, stop=True),
                    reads=[K(yT_key), K("wo_b")], writes=[K("pb", bk)])
                P.op("dve", Q.tensor_tensor(
                    out=x[:, c, h * 512:(h + 1) * 512], in0=PB[bk][:], in1=x[:, c, h * 512:(h + 1) * 512], op=ALU.add),
                    reads=[K("pb", bk), K("x", c)], writes=[K("x", c)])

    def proj_T(dst, dst_keys, w, wkey, col0, hT, hT_key, ntok, evac="act", tok0=0, dst0=0, pbs=(0, 1)):
        nb = (ntok + 511) // 512
        for b in range(nb):
            n = min(512, ntok - b * 512)
            pt = PB[pbs[b % len(pbs)]]
            pk = K("pb", pbs[b % len(pbs)])
            for kc in range(8):
                P.op("pe", Q.matmul(
                    pt[:, 0:n], w[:, kc, col0:col0 + 128], hT[:, kc, tok0 + b * 512: tok0 + b * 512 + n],
                    start=(kc == 0), stop=(kc == 7)), reads=[wkey, K(hT_key, (tok0 + b * 512) // 512)], writes=[pk])
            o = dst[:, dst0 + b * 512: dst0 + b * 512 + n]
            if evac == "act":
                P.op("act", Q.activation(out=o, in_=pt[:, 0:n], func=AF.Copy),
                     reads=[pk], writes=dst_keys)
            else:
                evac(b, n, pt, pk, o)

    def tap(name, s):
        if name in tap_d and s == 0:
            P.op("sp", Q.dma_start(out=tap_d[name].rearrange("(c p) d -> p c d", p=128), in_=x[:]),
                 reads=[K("x", c) for c in range(NCH)], dma=True)

    SC_BANKS = (2, 3, 4, 0, 1)
    sc_rot = [0]

    def attn_scores(pb, qb, qT, kT, chunks, eb_fn, Pt, Pt_key, etmp, qkey, kkey):
        nk = len(chunks)
        ngr = (nk + 1) // 2
        for gi in range(ngr):
            bk = SC_BANKS[sc_rot[0] % len(SC_BANKS)]
            sc_rot[0] += 1
            pt = PB[bk]
            pk = K("pb", bk)
            sub = chunks[gi * 2: gi * 2 + 2]
            for j, kc in enumerate(sub):
                P.op("pe", Q.matmul(
                    pt[:, j * 256:(j + 1) * 256], kT[pb:pb + 64, kc * 128:(kc + 1) * 128],
                    qT[pb:pb + 64, qb * 256:(qb + 1) * 256], start=True, stop=True),
                    reads=[qkey, kkey], writes=[pk])
            w = 256 * len(sub)
            dst = Pt[:, gi * 2: gi * 2 + len(sub), :].rearrange("p a b -> p (a b)")
            eb = eb_fn(gi * 2, len(sub)) if eb_fn is not None else None
            if eb is None:
                P.op("act", Q.activation(out=dst, in_=pt[:, 0:w], func=AF.Exp, scale=0.125),
                     reads=[pk], writes=[K(Pt_key, gi)])
            else:
                et = etmp[gi % 2]
                ek = K("etmp", gi % 2)
                P.op("act", Q.activation(out=et[:, 0:w], in_=pt[:, 0:w], func=AF.Exp, scale=0.125),
                     reads=[pk], writes=[ek])
                P.op("dve", Q.tensor_tensor(out=dst, in0=et[:, 0:w], in1=eb, op=ALU.mult),
                     reads=[ek, K("big16")], writes=[K(Pt_key, gi)])

    def attn_pv(qb, chunks, v_fn, dv, Pt, Pt_key, out_cb, vkey):
        nk = len(chunks)
        for qi in range(2):
            qc = qb * 2 + qi
            pt = PB[5]
            pk = K("pb", 5)
            for i, kc in enumerate(chunks):
                P.op("pe", Q.matmul(
                    pt[:, 0:dv + 1], Pt[:, i, qi * 128:(qi + 1) * 128], v_fn(kc), start=(i == 0), stop=(i == nk - 1)),
                    reads=[K(Pt_key, i // 2), vkey], writes=[pk])
            out_cb(qc, pt, pk)

    def attn_pipeline(blocks_list):
        pend = None
        for sa, pa_ in blocks_list:
            attn_scores(*sa)
            if pend is not None:
                attn_pv(*pend)
            pend = pa_
        if pend is not None:
            attn_pv(*pend)

    try:
        XK = [K("x", c) for c in range(NCH)]
        for s in range(S):
            P.op("sp", Q.dma_start(out=x[:], in_=x_d[s].rearrange("(c p) d -> p c d", p=128)),
                 writes=XK, dma=True)
            stage(0)
            for l in range(2):
                make_gate(l, 0, s)
                with Phase() as ph:
                    def pa(name, shape, dt):
                        return ph.enter_context(nc.sbuf_tensor(un(name), list(shape), dt))
                    hT = pa("hT", [128, 8, SEQ], BF16)
                    wo = (pa("wo_f", [128, D], F32), pa("wo_b", [128, D], BF16))
                    yT = pa("yT", [128, SEQ], BF16)
                    if l == 0:
                        Pt = [pa(f"Pt{k}", [128, 18, 256], BF16) for k in range(2)]
                        xsg = Pt[0][:, 0:16, :].rearrange("p (a b) c -> p a (b c)", a=4)
                    else:
                        xsg = pa("xsg", [128, 4, D], BF16)
                    make_AB(l, 0, s, 0)
                    norm_T(hT, "hT", NCH, lambda c: x[:, c, :], lambda c: [K("x", c)], xsg)
                    if l == 0:
                        hcT = pa("hcT", [128, 8, CTX], BF16)
                        big16 = pa("big16", [128, 2, SEQ], F32)
                        cx = big16[:, 0, :].rearrange("p (a b) -> p a b", a=2)
                        P.op("sp", Q.dma_start(out=cx, in_=ctx_d[s].rearrange("(c p) d -> p c d", p=128)),
                             writes=[K("big16")], dma=True)
                        make_AB(l, 0, S, 0)
                        for j in range(2):
                            rms_xs(cx[:, j, :], j, xsg[:, j, :], [K("big16")], [K("xsg", j)])
                        for dc in range(8):
                            pt = PBb[dc % 2]
                            pk = K("pb", dc % 2)
                            for j in range(2):
                                P.op("pe", Q.transpose(
                                    pt[:, j * 128:(j + 1) * 128], xsg[:, j, dc * 128:(dc + 1) * 128], ident_b[:]),
                                    reads=[K("xsg", j), K("ident_b")], writes=[pk])
                            P.op("act", Q.activation(
                                out=hcT[:, dc, :], in_=pt[:, 0:256], func=AF.Identity,
                                scale=AB[:, 0, dc:dc + 1], bias=AB[:, 1, dc:dc + 1]),
                                reads=[pk, K("AB")], writes=[K("hcT", 0)])
                        P.barrier()
                        stage(1)
                        wu = pa("wu", [128, 8, 640], BF16)
                        qT = pa("qT", [128, SEQ], BF16)
                        kT = pa("kT", [128, SEQ + CTX], BF16)
                        vv = pa("vv", [128, 18, 130], BF16)
                        etmp = [pa(f"etmp{k}", [128, 512], F32) for k in range(2)]
                        EB = big16[:].rearrange("p a b -> p (a b)")[:, 0:14 * 256].rearrange("p (a b) -> p a b", a=14)
                        rope = big16
                        ytm = pa("ytm", [128, NCH, 128], BF16)
                        tmpA = pa("tmpA", [128, 512], F32)
                        tmpB = pa("tmpB", [128, 512], F32)
                        lamv = pa("lamv", [128, 4, 64], F32)
                        lam = pa("lam", [128, 4], F32)
                        subl = pa("subl", [128, 128], F32)
                        o0 = pa("o0", [128, 2, 129], F32)
                        P.op("sp", Q.dma_start(out=lamv[:].rearrange("p a b -> p (a b)"), in_=L[0]["lam"].partition_broadcast(128)),
                             writes=[K("lamv")], dma=True)
                        P.op("sp", Q.dma_start(out=subl[:], in_=L[0]["subln"].partition_broadcast(128)),
                             writes=[K("subl")], dma=True)
                        lam_init = 0.8 - 0.6 * math.exp(-0.3 * 0)
                        for j in range(2):
                            P.op("dve", Q.tensor_tensor(out=tmpA[:, 0:64], in0=lamv[:, 2 * j, :], in1=lamv[:, 2 * j + 1, :], op=ALU.mult),
                                 reads=[K("lamv")], writes=[K("tmpA")])
                            P.op("dve", Q.reduce_sum(out=lam[:, j:j + 1], in_=tmpA[:, 0:64], axis=AX.X),
                                 reads=[K("tmpA")], writes=[K("lam")])
                        P.op("act", Q.activation(out=lam[:, 0:2], in_=lam[:, 0:2], func=AF.Exp), reads=[K("lam")], writes=[K("lam")])
                        P.op("dve", Q.tensor_tensor(out=lam[:, 2:3], in0=lam[:, 0:1], in1=lam[:, 1:2], op=ALU.subtract),
                             reads=[K("lam")], writes=[K("lam")])
                        P.op("dve", Q.tensor_scalar(out=lam[:, 3:4], in0=lam[:, 2:3], scalar1=lam_init, scalar2=-1.0, op0=ALU.add, op1=ALU.mult),
                             reads=[K("lam")], writes=[K("lam")])
                        P.op("dve", Q.tensor_scalar(out=subl[:], in0=subl[:], scalar1=1.0 - lam_init, scalar2=None, op0=ALU.mult),
                             reads=[K("subl")], writes=[K("subl")])
                        blocks = _na_blocks()
                        for u in range(8):
                            is_na = u < 4
                            ncol = 384 if is_na else 640
                            wsrc = (L[0]["w_na"][u] if is_na else L[0]["w_df"][u - 4]).rearrange("(kc p) n -> p kc n", p=128)
                            P.op("pool", Q.dma_start(out=wu[:, :, 0:ncol], in_=wsrc),
                                 writes=[K("wu")], dma=True)
                            if is_na:
                                proj_T(qT, [K("qT")], wu, K("wu"), 0, hT, "hT", SEQ)
                                proj_T(kT, [K("kT")], wu, K("wu"), 128, hT, "hT", SEQ)
                                proj_T(kT, [K("kT")], wu, K("wu"), 128, hcT, "hcT", CTX, dst0=SEQ)
                                vcol, dv = 256, 64
                            else:
                                if u == 4:
                                    P.op("sp", Q.dma_start(out=rope[:], in_=rope_d.rearrange("a p t -> p a t")),
                                         writes=[K("big16")], dma=True)
                                for (dst, dkey, c1, c2) in ((qT, K("qT"), 0, 128), (kT, K("kT"), 256, 384)):
                                    for b in range(4):
                                        for (pi, cc) in ((0, c1), (1, c2)):
                                            for kc in range(8):
                                                P.op("pe", Q.matmul(
                                                    PB[pi][:], wu[:, kc, cc:cc + 128], hT[:, kc, b * 512:(b + 1) * 512],
                                                    start=(kc == 0), stop=(kc == 7)),
                                                    reads=[K("wu"), K("hT", b)], writes=[K("pb", pi)])
                                        P.op("dve", Q.tensor_tensor(out=tmpA[:], in0=PB[0][:], in1=rope[:, 0, b * 512:(b + 1) * 512], op=ALU.mult),
                                             reads=[K("pb", 0), K("big16")], writes=[K("tmpA")])
                                        P.op("dve", Q.tensor_tensor(out=tmpB[:], in0=PB[1][:], in1=rope[:, 1, b * 512:(b + 1) * 512], op=ALU.mult),
                                             reads=[K("pb", 1), K("big16")], writes=[K("tmpB")])
                                        P.op("dve", Q.tensor_tensor(out=dst[:, b * 512:(b + 1) * 512], in0=tmpA[:], in1=tmpB[:], op=ALU.add),
                                             reads=[K("tmpA"), K("tmpB")], writes=[dkey])
                                proj_T(kT, [K("kT")], wu, K("wu"), 256, hcT, "hcT", CTX, dst0=SEQ)
                                vcol, dv = 512, 128
                            for c in range(18):
                                src = hT[:, :, c * 128:(c + 1) * 128] if c < 16 else hcT[:, :, (c - 16) * 128:(c - 15) * 128]
                                srck = K("hT", c // 4) if c < 16 else K("hcT", 0)
                                pt = PB[c % 2]
                                pk = K("pb", c % 2)
                                for kc in range(8):
                                    P.op("pe", Q.matmul(
                                        pt[:, 0:128], src[:, kc, :], wu[:, kc, vcol:vcol + 128], start=(kc == 0), stop=(kc == 7)),
                                        reads=[K("wu"), srck], writes=[pk])
                                if is_na:
                                    P.op("act", Q.activation(
                                        out=vv[:, c, :].rearrange("p (h d) -> p h d", h=2)[:, :, 0:64],
                                        in_=pt[:, 0:128].rearrange("p (h d) -> p h d", h=2), func=AF.Copy),
                                        reads=[pk], writes=[K("vv")])
                                else:
                                    P.op("act", Q.activation(out=vv[:, c, 0:128], in_=pt[:, 0:128], func=AF.Copy),
                                         reads=[pk], writes=[K("vv")])
                            if is_na:
                                P.op("dve", Q.memset(vv[:].rearrange("p c (h d) -> p c h d", h=2)[:, :, :, 64:65], 1.0),
                                     writes=[K("vv")])
                            else:
                                P.op("dve", Q.memset(vv[:, :, 128:129], 1.0), writes=[K("vv")])
                            if is_na:
                                for hh in range(2):
                                    head = 2 * u + hh
                                    P.op("sp", Q.dma_start(
                                        out=EB.rearrange("p a b -> p (a b)"), in_=L[0]["na_bias"][head]),
                                        writes=[K("big16")], dma=True)
                                    for a in range(2):
                                        P.op("act", Q.activation(out=EB[:, a * 7:(a + 1) * 7, :], in_=EB[:, a * 7:(a + 1) * 7, :], func=AF.Exp),
                                             reads=[K("big16")], writes=[K("big16")])

                                    def ocb(qc, pt, pk, hh=hh):
                                        P.op("dve", Q.reciprocal(out=small[:, 2:3], in_=pt[:, 64:65]),
                                             reads=[pk], writes=[K("small")])
                                        P.op("dve", Q.tensor_scalar(out=ytm[:, qc, hh * 64:(hh + 1) * 64], in0=pt[:, 0:64],
                                                                              scalar1=small[:, 2:3], scalar2=None, op0=ALU.mult),
                                             reads=[pk, K("small")], writes=[K("ytm", qc)])

                                    bl = []
                                    for qb in range(8):
                                        nb = len(blocks[qb])
                                        off = NA_CLS_OFF[_na_class(qb)]

                                        def ebf(i0, n, nb=nb, off=off):
                                            if i0 >= nb:
                                                return None
                                            assert i0 + n <= nb
                                            return EB[:, off + i0:off + i0 + n, :].rearrange("p a b -> p (a b)")

                                        ch = blocks[qb] + [16, 17]
                                        pi = qb % 2
                                        vf = (lambda kc, hh=hh: vv[:, kc, hh * 65:(hh + 1) * 65])
                                        bl.append(((64 * hh, qb, qT, kT, ch, ebf, Pt[pi], ("Pt", pi), etmp, K("qT"), K("kT")),
                                                   (qb, ch, vf, 64, Pt[pi], ("Pt", pi), ocb, K("vv"))))
                                    attn_pipeline(bl)
                            else:
                                def ocb0(qc, pt, pk):
                                    P.op("act", Q.activation(out=o0[:, qc % 2, :], in_=pt[:, 0:129], func=AF.Copy),
                                         reads=[pk], writes=[K("o0", qc % 2)])

                                def ocb1(qc, pt, pk):
                                    r0, r1, ssq, rs = small[:, 4:5], small[:, 5:6], small[:, 6:7], small[:, 7:8]
                                    ok = K("o0", qc % 2)
                                    P.op("dve", Q.reciprocal(out=r0, in_=o0[:, qc % 2, 128:129]), reads=[ok], writes=[K("small")])
                                    P.op("dve", Q.reciprocal(out=r1, in_=pt[:, 128:129]), reads=[pk], writes=[K("small")])
                                    P.op("dve", Q.tensor_tensor(out=r1, in0=r1, in1=lam[:, 3:4], op=ALU.mult),
                                         reads=[K("small"), K("lam")], writes=[K("small")])
                                    P.op("dve", Q.tensor_scalar(out=tmpA[:, 0:128], in0=o0[:, qc % 2, 0:128], scalar1=r0, scalar2=None, op0=ALU.mult),
                                         reads=[ok, K("small")], writes=[K("tmpA")])
                                    P.op("dve", Q.scalar_tensor_tensor(out=tmpA[:, 0:128], in0=pt[:, 0:128], scalar=r1, in1=tmpA[:, 0:128],
                                                                                 op0=ALU.mult, op1=ALU.add),
                                         reads=[pk, K("small"), K("tmpA")], writes=[K("tmpA")])
                                    P.op("act", Q.activation(out=tmpB[:, 0:128], in_=tmpA[:, 0:128], func=AF.Square),
                                         reads=[K("tmpA")], writes=[K("tmpB")])
                                    P.op("dve", Q.reduce_sum(out=ssq, in_=tmpB[:, 0:128], axis=AX.X), reads=[K("tmpB")], writes=[K("small")])
                                    P.op("dve", Q.tensor_scalar(out=rs, in0=ssq, scalar1=1.0 / 128, scalar2=EPS, op0=ALU.mult, op1=ALU.add),
                                         reads=[K("small")], writes=[K("small")])
                                    P.op("act", Q.activation(out=rs, in_=rs, func=AF.Sqrt), reads=[K("small")], writes=[K("small")])
                                    P.op("dve", Q.reciprocal(out=rs, in_=rs), reads=[K("small")], writes=[K("small")])
                                    P.op("dve", Q.scalar_tensor_tensor(out=ytm[:, qc, :], in0=tmpA[:, 0:128], scalar=rs, in1=subl[:],
                                                                                 op0=ALU.mult, op1=ALU.mult),
                                         reads=[K("tmpA"), K("small"), K("subl")], writes=[K("ytm", qc)])

                                bl = []
                                ch = list(range(18))
                                vf = (lambda kc: vv[:, kc, 0:129])
                                for qb in range(8):
                                    for m in range(2):
                                        bl.append(((64 * m, qb, qT, kT, ch, None, Pt[m], ("Pt", m), etmp, K("qT"), K("kT")),
                                                   (qb, ch, vf, 128, Pt[m], ("Pt", m), ocb0 if m == 0 else ocb1, K("vv"))))
                                attn_pipeline(bl)
                            for g in range(4):
                                pt = PBb[g % 2]
                                pk = K("pb", g % 2)
                                for j in range(4):
                                    c = g * 4 + j
                                    P.op("pe", Q.transpose(pt[:, j * 128:(j + 1) * 128], ytm[:, c, :], ident_b[:]),
                                         reads=[K("ytm", c), K("ident_b")], writes=[pk])
                                P.op("act", Q.activation(out=yT[:, g * 512:(g + 1) * 512], in_=pt[:, 0:512], func=AF.Copy),
                                     reads=[pk], writes=[K("yT")])
                            out_proj_unit(l, u, yT, "yT", wo)
                            stage(20 + u)
                    else:
                        P.barrier()
                        wu = pa("wu", [128, 8, 384], BF16)
                        scw = pa("scw", [128, 4, 3], F32)
                        cfw = pa("cfw", [128, 4, 31], F32)
                        cfv = pa("cfv", [128, 3, 4], F32)
                        P.op("sp", Q.dma_start(out=scw[:], in_=L[1]["sc_w"]), writes=[K("scw")], dma=True)
                        P.op("sp", Q.dma_start(out=cfw[:], in_=L[1]["cf_w"]), writes=[K("cfw")], dma=True)
                        P.op("sp", Q.dma_start(out=cfv[:], in_=L[1]["cf_vec"]), writes=[K("cfv")], dma=True)
                        va = pa("va", [128, SEQ + 32], F32)
                        vb = pa("vb", [128, SEQ], F32)
                        vc = pa("vc", [128, SEQ], F32)
                        ud = pa("ud", [128, 4, SEQ], F32)
                        P.op("dve", Q.memset(va[:], 0.0), writes=[K("va")])
                        for j in range(4):
                            P.op("pool", Q.dma_start(out=wu[:, :, 0:384], in_=L[1]["w_c"][j].rearrange("(kc p) n -> p kc n", p=128)),
                                 writes=[K("wu")], dma=True)
                            proj_T(vb, [K("vb")], wu, K("wu"), 0, hT, "hT", SEQ)

                            def ev_gc(b, n, pt, pk, o):
                                P.op("dve", Q.tensor_tensor(out=o, in0=pt[:, 0:n], in1=vb[:, b * 512:b * 512 + n], op=ALU.mult),
                                     reads=[pk, K("vb")], writes=[K("va")])
                            proj_T(va, [K("va")], wu, K("wu"), 256, hT, "hT", SEQ, evac=ev_gc, dst0=1)
                            proj_T(vc, [K("vc")], wu, K("wu"), 128, hT, "hT", SEQ)
                            P.op("dve", Q.tensor_scalar(out=vb[:], in0=va[:, 0:SEQ], scalar1=scw[:, j, 0:1], scalar2=None, op0=ALU.mult),
                                 reads=[K("va"), K("scw")], writes=[K("vb")])
                            for k in (1, 2):
                                P.op("dve", Q.scalar_tensor_tensor(out=vb[:], in0=va[:, k:k + SEQ], scalar=scw[:, j, k:k + 1], in1=vb[:],
                                                                                       op0=ALU.mult, op1=ALU.add),
                                     reads=[K("va"), K("scw"), K("vb")], writes=[K("vb")])
                            P.op("dve", Q.tensor_tensor(out=yT[:], in0=vb[:], in1=vc[:], op=ALU.mult),
                                 reads=[K("vb"), K("vc")], writes=[K("yT")])
                            out_proj_unit(l, j, yT, "yT", wo)
                        P.op("dve", Q.memset(va[:], 0.0), reads=[K("va")], writes=[K("va")])
                        for j in range(4):
                            P.op("pool", Q.dma_start(out=wu[:, :, 0:256], in_=L[1]["w_d"][j].rearrange("(kc p) n -> p kc n", p=128)),
                                 writes=[K("wu")], dma=True)

                            def ev_sig(b, n, pt, pk, o):
                                P.op("act", Q.activation(out=o, in_=pt[:, 0:n], func=AF.Sigmoid), reads=[pk], writes=[K("vb")])
                            proj_T(vb, [K("vb")], wu, K("wu"), 128, hT, "hT", SEQ, evac=ev_sig)

                            def ev_glu(b, n, pt, pk, o):
                                P.op("dve", Q.tensor_tensor(out=o, in0=pt[:, 0:n], in1=vb[:, b * 512:b * 512 + n], op=ALU.mult),
                                     reads=[pk, K("vb")], writes=[K("va")])
                            proj_T(va, [K("va")], wu, K("wu"), 0, hT, "hT", SEQ, evac=ev_glu, dst0=15)
                            P.op("dve", Q.tensor_scalar(out=ud[:, j, :], in0=va[:, 0:SEQ], scalar1=cfw[:, j, 0:1], scalar2=cfv[:, 0, j:j + 1],
                                                                       op0=ALU.mult, op1=ALU.add),
                                 reads=[K("va"), K("cfw"), K("cfv")], writes=[K("ud", j)])
                            for k in range(1, 31):
                                P.op("dve", Q.scalar_tensor_tensor(out=ud[:, j, :], in0=va[:, k:k + SEQ], scalar=cfw[:, j, k:k + 1], in1=ud[:, j, :],
                                                                                       op0=ALU.mult, op1=ALU.add),
                                     reads=[K("va"), K("cfw"), K("ud", j)], writes=[K("ud", j)])
                        for b in range(4):
                            sl = slice(b * 512, (b + 1) * 512)
                            for j in range(4):
                                P.op("pe", Q.matmul(PB[2][:], ones_f[:], ud[:, j, sl], start=(j == 0), stop=(j == 3)),
                                     reads=[K("ones_f"), K("ud", j)], writes=[K("pb", 2)])
                            for j in range(4):
                                P.op("act", Q.activation(out=vb[:, j * 512:(j + 1) * 512], in_=ud[:, j, sl], func=AF.Square),
                                     reads=[K("ud", j)], writes=[K("vb")])
                            for j in range(4):
                                P.op("pe", Q.matmul(PB[3][:], ones_f[:], vb[:, j * 512:(j + 1) * 512], start=(j == 0), stop=(j == 3)),
                                     reads=[K("ones_f"), K("vb")], writes=[K("pb", 3)])
                            mean, rstd, t0, t1 = vc[:, 0:512], vc[:, 512:1024], vc[:, 1024:1536], vc[:, 1536:2048]
                            P.op("dve", Q.tensor_scalar(out=mean, in0=PB[2][:], scalar1=1.0 / 512, scalar2=None, op0=ALU.mult),
                                 reads=[K("pb", 2)], writes=[K("vc")])
                            P.op("dve", Q.tensor_tensor(out=t0, in0=mean, in1=mean, op=ALU.mult), reads=[K("vc")], writes=[K("vc")])
                            P.op("dve", Q.scalar_tensor_tensor(out=rstd, in0=PB[3][:], scalar=1.0 / 512, in1=t0, op0=ALU.mult, op1=ALU.subtract),
                                 reads=[K("pb", 3), K("vc")], writes=[K("vc")])
                            P.op("dve", Q.tensor_scalar(out=rstd, in0=rstd, scalar1=EPS, scalar2=None, op0=ALU.add), reads=[K("vc")], writes=[K("vc")])
                            P.op("act", Q.activation(out=rstd, in_=rstd, func=AF.Sqrt), reads=[K("vc")], writes=[K("vc")])
                            P.op("dve", Q.reciprocal(out=rstd, in_=rstd), reads=[K("vc")], writes=[K("vc")])
                            for j in range(4):
                                P.op("dve", Q.tensor_tensor(out=t1, in0=ud[:, j, sl], in1=mean, op=ALU.subtract),
                                     reads=[K("ud", j), K("vc")], writes=[K("vc")])
                                P.op("dve", Q.tensor_tensor(out=t1, in0=t1, in1=rstd, op=ALU.mult), reads=[K("vc")], writes=[K("vc")])
                                P.op("act", Q.activation(out=t1, in_=t1, func=AF.Identity, scale=cfv[:, 1, j:j + 1], bias=cfv[:, 2, j:j + 1]),
                                     reads=[K("vc"), K("cfv")], writes=[K("vc")])
                                P.op("act", Q.activation(out=vb[:, 0:512], in_=t1, func=AF.Sigmoid), reads=[K("vc")], writes=[K("vb")])
                                P.op("dve", Q.tensor_tensor(out=ud[:, j, sl], in0=t1, in1=vb[:, 0:512], op=ALU.mult),
                                     reads=[K("vc"), K("vb")], writes=[K("ud", j)])
                        for j in range(4):
                            P.op("act", Q.activation(out=yT[:], in_=ud[:, j, :], func=AF.Copy), reads=[K("ud", j)], writes=[K("yT")])
                            out_proj_unit(l, 4 + j, yT, "yT", wo)
                P.barrier()
                tap(f"mix{l}", s)
                stage(3 if l == 0 else 6)

                make_gate(l, 1, s)
                with Phase() as ph:
                    def pa(name, shape, dt):
                        return ph.enter_context(nc.sbuf_tensor(un(name), list(shape), dt))
                    xs2 = pa("xs2", [128, NCH, D], BF16)
                    wr = pa("wr", [128, 8, 16], BF16)
                    aff_tm = pa("aff_tm", [128, NCH, 16], F32)
                    mask_f = pa("mask_f", [128, NCH, 16], F32)
                    mask_b = pa("mask_b", [128, NCH, 16], BF16)
                    pos_tm = pa("pos_tm", [128, NCH, 16], F32)
                    R = pa("R", [128, NCH, 16, 4], BF16)
                    hi_f = pa("hi_f", [128, NCH, 16], F32)
                    P.op("pool", Q.dma_start(out=wr[:], in_=L[l]["w_router"].rearrange("(kc p) n -> p kc n", p=128)),
                         writes=[K("wr")], dma=True)
                    make_AB(l, 1, s, 1)
                    with Phase() as ph2:
                        def pb2(name, shape, dt):
                            return ph2.enter_context(nc.sbuf_tensor(un(name), list(shape), dt))
                        hTg = pb2("hTg", [128, 8, 512], BF16)
                        expT = pb2("expT", [16, SEQ], F32)
                        affT = pb2("affT", [16, SEQ], F32)
                        work = pb2("work", [16, SEQ], F32)
                        maskT = pb2("maskT", [16, SEQ], F32)
                        m8 = pb2("m8", [16, 8], F32)
                        thr = pb2("thr", [16, 1], F32)
                        for g in range(4):
                            for j in range(4):
                                c = g * 4 + j
                                rms_xs(x[:, c, :], c, xs2[:, c, :], [K("x", c)], [K("xs2", c)])
                            for dc in range(8):
                                pt = PBb[dc % 2]
                                pk = K("pb", dc % 2)
                                for j in range(4):
                                    c = g * 4 + j
                                    P.op("pe", Q.transpose(pt[:, j * 128:(j + 1) * 128], xs2[:, c, dc * 128:(dc + 1) * 128], ident_b[:]),
                                         reads=[K("xs2", c), K("ident_b")], writes=[pk])
                                P.op("act", Q.activation(out=hTg[:, dc, :], in_=pt[:, 0:512], func=AF.Identity,
                                                                                 scale=AB[:, 0, dc:dc + 1], bias=AB[:, 1, dc:dc + 1]),
                                     reads=[pk, K("AB")], writes=[K("hTg")])
                            for kc in range(8):
                                P.op("pe", Q.matmul(PB[2][0:16, :], wr[:, kc, :], hTg[:, kc, :], start=(kc == 0), stop=(kc == 7)),
                                     reads=[K("wr"), K("hTg")], writes=[K("pb", 2)])
                            P.op("act", Q.activation(out=expT[:, g * 512:(g + 1) * 512], in_=PB[2][0:16, :], func=AF.Exp),
                                 reads=[K("pb", 2)], writes=[K("expT")])
                            for j in range(4):
                                for kc in range(8):
                                    P.op("pe", Q.matmul(PB[3][:, j * 16:(j + 1) * 16], hTg[:, kc, j * 128:(j + 1) * 128], wr[:, kc, :],
                                                                              start=(kc == 0), stop=(kc == 7)),
                                         reads=[K("wr"), K("hTg")], writes=[K("pb", 3)])
                            P.op("act", Q.activation(out=aff_tm[:, g * 4:(g + 1) * 4, :].rearrange("p a b -> p (a b)"), in_=PB[3][:, 0:64], func=AF.Exp),
                                 reads=[K("pb", 3)], writes=[K("aff_tm")])
                        stage(41 if l == 0 else 410)
                        for b in range(4):
                            P.op("pe", Q.matmul(PB[2][0:16, :], ones_f[0:16, 0:16], expT[:, b * 512:(b + 1) * 512], start=True, stop=True),
                                 reads=[K("ones_f"), K("expT")], writes=[K("pb", 2)])
                            P.op("dve", Q.reciprocal(out=work[:, b * 512:(b + 1) * 512], in_=PB[2][0:16, :]),
                                 reads=[K("pb", 2)], writes=[K("work")])
                        P.op("dve", Q.tensor_tensor(out=affT[:], in0=expT[:], in1=work[:], op=ALU.mult),
                             reads=[K("expT"), K("work")], writes=[K("affT")])
                        stage(42 if l == 0 else 420)
                        P.op("dve", Q.tensor_copy(out=work[:], in_=affT[:]), reads=[K("affT")], writes=[K("work")])
                        for r in range(CAP // 8):
                            P.op("dve", Q.max(out=m8[:], in_=work[:]), reads=[K("work")], writes=[K("m8")])
                            if r < CAP // 8 - 1:
                                P.op("dve", Q.match_replace(out=work[:], in_to_replace=m8[:], in_values=work[:], imm_value=-1.0),
                                     reads=[K("m8"), K("work")], writes=[K("work")])
                        P.op("dve", Q.tensor_reduce(out=thr[:], in_=m8[:], axis=AX.X, op=ALU.min), reads=[K("m8")], writes=[K("thr")])
                        P.op("dve", Q.tensor_scalar(out=maskT[:], in0=affT[:], scalar1=thr[:, 0:1], scalar2=None, op0=ALU.is_ge),
                             reads=[K("affT"), K("thr")], writes=[K("maskT")])
                        stage(43 if l == 0 else 430)
                        for c in range(NCH):
                            P.op("pe", Q.matmul(PB[0][:, c * 16:(c + 1) * 16], maskT[:, c * 128:(c + 1) * 128], ident_f[0:16, 0:16], start=True, stop=True),
                                 reads=[K("maskT"), K("ident_f")], writes=[K("pb", 0)])
                        stage(44)
                        P.op("act", Q.activation(out=mask_f[:].rearrange("p a b -> p (a b)"), in_=PB[0][:, 0:256], func=AF.Copy),
                             reads=[K("pb", 0)], writes=[K("mask_f")])
                        stage(45)
                        P.op("dve", Q.tensor_copy(out=mask_b[:], in_=mask_f[:]),
                             reads=[K("mask_f")], writes=[K("mask_b")])
                        stage(46)
                    P.barrier()
                    stage(4 if l == 0 else 40)
                    for c in range(NCH):
                        for c2 in range(c + 1):
                            lt = ltri_b if c2 == c else ones_b
                            P.op("pe", Q.matmul(PB[1][:, c * 16:(c + 1) * 16], lt[:], mask_b[:, c2, :], start=(c2 == 0), stop=(c2 == c)),
                                 reads=[K("ltri_b"), K("ones_b"), K("mask_b")], writes=[K("pb", 1)])
                    P.op("act", Q.activation(out=pos_tm[:].rearrange("p a b -> p (a b)"), in_=PB[1][:, 0:256], func=AF.Copy),
                         reads=[K("pb", 1)], writes=[K("pos_tm")])
                    P.op("dve", Q.reduce_sum(out=hi_f[:, :, 0], in_=aff_tm[:], axis=AX.X), reads=[K("aff_tm")], writes=[K("hi_f")])
                    P.op("dve", Q.reciprocal(out=hi_f[:, :, 1], in_=hi_f[:, :, 0]), reads=[K("hi_f")], writes=[K("hi_f")])
                    for c in range(NCH):
                        P.op("dve", Q.tensor_scalar(out=aff_tm[:, c, :], in0=aff_tm[:, c, :], scalar1=hi_f[:, c, 1:2], scalar2=None, op0=ALU.mult),
                             reads=[K("aff_tm"), K("hi_f")], writes=[K("aff_tm")])
                    P.op("dve", Q.tensor_copy(out=R[:, :, :, 0], in_=aff_tm[:]), reads=[K("aff_tm")], writes=[K("R")])
                    P.op("dve", Q.tensor_copy(out=hi_f[:], in_=R[:, :, :, 0]), reads=[K("R"), K("aff_tm")], writes=[K("hi_f")])
                    P.op("dve", Q.tensor_tensor(out=R[:, :, :, 1], in0=aff_tm[:], in1=hi_f[:], op=ALU.subtract),
                         reads=[K("aff_tm"), K("hi_f")], writes=[K("R")])
                    for q in range(2):
                        P.op("dve", Q.tensor_copy(out=R[:, :, :, 2 + q], in_=cidx[:, :, q:q + 1].to_broadcast([128, NCH, 16])),
                             reads=[K("cidx")], writes=[K("R")])

                    Se = pa("Se", [128, NCH, CAP], BF16)
                    SeT = pa("SeT", [128, 2, SEQ], BF16)
                    gi = pa("gi", [128, 2, 4], F32)
                    xinT = pa("xinT", [128, 8, CAP], BF16)
                    hidT = pa("hidT", [128, NFC, CAP], BF16)
                    sgt = pa("sgt", [128, CAP], F32)
                    yg = pa("yg", [128, 2, D], BF16)
                    w13 = [pa(f"w13_{k}", [128, 2, 8, 256], BF16) for k in range(4)]
                    w2b = [pa(f"w2_{k}", [128, 2, D], BF16) for k in range(3)]
                    wcount = [0, 0]
                    for ex in range(E):
                        for c in range(NCH):
                            P.op("dve", Q.tensor_scalar(out=Se[:, c, :], in0=iota_t[:, 0:CAP], scalar1=pos_tm[:, c, ex:ex + 1],
                                                                              scalar2=mask_f[:, c, ex:ex + 1], op0=ALU.is_equal, op1=ALU.mult),
                                 reads=[K("iota_t"), K("pos_tm"), K("mask_f")], writes=[K("Se", c)])
                        for jc in range(2):
                            for c in range(NCH):
                                P.op("pe", Q.matmul(PB[0][:, jc * 4:(jc + 1) * 4], Se[:, c, jc * 128:(jc + 1) * 128], R[:, c, ex, :],
                                                                                start=(c == 0), stop=(c == NCH - 1)),
                                     reads=[K("Se", c), K("R")], writes=[K("pb", 0)])
                        P.op("act", Q.activation(out=gi[:].rearrange("p a b -> p (a b)"), in_=PB[0][:, 0:8], func=AF.Copy),
                             reads=[K("pb", 0)], writes=[K("gi")])
                        P.op("dve", Q.tensor_tensor(out=gi[:, :, 0], in0=gi[:, :, 0], in1=gi[:, :, 1], op=ALU.add), reads=[K("gi")], writes=[K("gi")])
                        P.op("dve", Q.scalar_tensor_tensor(out=gi[:, :, 2], in0=gi[:, :, 2], scalar=128.0, in1=gi[:, :, 3], op0=ALU.mult, op1=ALU.add),
                             reads=[K("gi")], writes=[K("gi")])
                        for jc in range(2):
                            P.op("dve", Q.tensor_scalar(out=SeT[:, jc, :], in0=iota_t[:], scalar1=gi[:, jc, 2:3], scalar2=None, op0=ALU.is_equal),
                                 reads=[K("iota_t"), K("gi")], writes=[K("SeT", jc)])
                        for dc in range(8):
                            bk = dc % 2
                            for c in range(NCH):
                                P.op("pe", Q.matmul(PB[bk][:, 0:CAP], xs2[:, c, dc * 128:(dc + 1) * 128], Se[:, c, :],
                                                                                start=(c == 0), stop=(c == NCH - 1)),
                                     reads=[K("xs2", c), K("Se", c)], writes=[K("pb", bk)])
                            P.op("act", Q.activation(out=xinT[:, dc, :], in_=PB[bk][:, 0:CAP], func=AF.Identity,
                                                                             scale=AB[:, 0, dc:dc + 1], bias=AB[:, 1, dc:dc + 1]),
                                 reads=[K("pb", bk), K("AB")], writes=[K("xinT")])
                        for fg in range(NFC // 2):
                            slot = wcount[0] % len(w13)
                            wcount[0] += 1
                            wt = w13[slot]
                            for wi, nm in enumerate(("w1", "w3")):
                                src = L[l][nm][ex][:, fg * 256:(fg + 1) * 256].rearrange("(kc p) n -> p kc n", p=128)
                                P.op("pool", Q.dma_start(out=wt[:, wi], in_=src), writes=[K("w13", slot, wi)], dma=True)
                            for f2 in range(2):
                                fc = fg * 2 + f2
                                bk = 2 + (fc % 2)
                                for wi in range(2):
                                    for kc in range(8):
                                        P.op("pe", Q.matmul(
                                            PB[bk][:, wi * CAP:(wi + 1) * CAP], wt[:, wi, kc, f2 * 128:(f2 + 1) * 128], xinT[:, kc, :],
                                            start=(kc == 0), stop=(kc == 7)),
                                            reads=[K("w13", slot, wi), K("xinT")], writes=[K("pb", bk)])
                                P.op("act", Q.activation(out=sgt[:], in_=PB[bk][:, 0:CAP], func=AF.Sigmoid), reads=[K("pb", bk)], writes=[K("sgt")])
                                P.op("dve", Q.tensor_tensor(out=sgt[:], in0=PB[bk][:, 0:CAP], in1=sgt[:], op=ALU.mult),
                                     reads=[K("pb", bk), K("sgt")], writes=[K("sgt")])
                                P.op("dve", Q.tensor_tensor(out=hidT[:, fc, :], in0=PB[bk][:, CAP:2 * CAP], in1=sgt[:], op=ALU.mult),
                                     reads=[K("pb", bk), K("sgt")], writes=[K("hidT")])
                        for fg in range(NFC // 2):
                            slot = wcount[1] % len(w2b)
                            wcount[1] += 1
                            wt = w2b[slot]
                            src = L[l]["w2"][ex][fg * 256:(fg + 1) * 256, :].rearrange("(a p) n -> p a n", p=128)
                            P.op("pool", Q.dma_start(out=wt[:], in_=src), writes=[K("w2b", slot)], dma=True)
                            for f2 in range(2):
                                fc = fg * 2 + f2
                                for jc in range(2):
                                    for h in range(2):
                                        bk = 4 + jc * 2 + h
                                        P.op("pe", Q.matmul(
                                            PB[bk][:], hidT[:, fc, jc * 128:(jc + 1) * 128], wt[:, f2, h * 512:(h + 1) * 512],
                                            start=(fc == 0), stop=(fc == NFC - 1)),
                                            reads=[K("hidT"), K("w2b", slot)], writes=[K("pb", bk)])
                        for jc in range(2):
                            for h in range(2):
                                bk = 4 + jc * 2 + h
                                P.op("dve", Q.scalar_tensor_tensor(
                                    out=yg[:, jc, h * 512:(h + 1) * 512], in0=PB[bk][:], scalar=gi[:, jc, 0:1], in1=gate_b[:, h * 512:(h + 1) * 512],
                                    op0=ALU.mult, op1=ALU.mult), reads=[K("pb", bk), K("gi"), K("gate_b")], writes=[K("yg")])
                        for c in range(NCH):
                            for h in range(2):
                                bk = (c * 2 + h) % 2
                                for jc in range(2):
                                    P.op("pe", Q.matmul(
                                        PB[bk][:], SeT[:, jc, c * 128:(c + 1) * 128], yg[:, jc, h * 512:(h + 1) * 512], start=(jc == 0), stop=(jc == 1)),
                                        reads=[K("SeT", jc), K("yg")], writes=[K("pb", bk)])
                                P.op("dve", Q.tensor_tensor(
                                    out=x[:, c, h * 512:(h + 1) * 512], in0=PB[bk][:], in1=x[:, c, h * 512:(h + 1) * 512], op=ALU.add),
                                    reads=[K("pb", bk), K("x", c)], writes=[K("x", c)])
                P.barrier()
                tap(f"moe{l}", s)
                stage(5 if l == 0 else 7)

            with Phase() as ph:
                ob = [ph.enter_context(nc.sbuf_tensor(un(f"ob{k}"), [128, D], F32)) for k in range(2)]
                fnorm_b = ph.enter_context(nc.sbuf_tensor(un("fnorm_b"), [128, D], F32))
                P.op("sp", Q.dma_start(out=fnorm_b[:], in_=fnorm_d.partition_broadcast(128)), writes=[K("fnorm_b")], dma=True)
                for c in range(NCH):
                    o = ob[c % 2]
                    ok = K("ob", c % 2)
                    sl = c % 16
                    sq, rs = stat[:, 2 * sl:2 * sl + 1], stat[:, 2 * sl + 1:2 * sl + 2]
                    sk = K("stat", sl)
                    P.op("act", Q.activation(out=o[:], in_=x[:, c, :], func=AF.Square),
                         reads=[K("x", c)], writes=[ok])
                    P.op("dve", Q.reduce_sum(out=sq, in_=o[:], axis=AX.X), reads=[ok], writes=[sk])
                    P.op("dve", Q.tensor_scalar(out=rs, in0=sq, scalar1=1.0 / D, scalar2=EPS, op0=ALU.mult, op1=ALU.add),
                         reads=[sk], writes=[sk])
                    P.op("act", Q.activation(out=rs, in_=rs, func=AF.Sqrt), reads=[sk], writes=[sk])
                    P.op("dve", Q.reciprocal(out=rs, in_=rs), reads=[sk], writes=[sk])
                    P.op("dve", Q.scalar_tensor_tensor(out=o[:], in0=x[:, c, :], scalar=rs, in1=fnorm_b[:], op0=ALU.mult, op1=ALU.mult),
                         reads=[K("x", c), sk, K("fnorm_b")], writes=[ok])
                    P.op("sp", Q.dma_start(out=out_d[s, c * 128:(c + 1) * 128, :], in_=o[:]), reads=[ok], dma=True)
            P.barrier()
    except _Stop:
        P.barrier()
        P.op("sp", Q.dma_start(out=out_d[0].rearrange("(c p) d -> p c d", p=128), in_=x[:]), reads=[K("x", c) for c in range(NCH)], dma=True)
        P.barrier()

    P.emit(st)
    st.close()
    return nc


def _col(v):
    return np.ascontiguousarray(v.reshape(-1, 128).T)


def prep_shared(inp, E):
    f = np.float32
    sh = {}
    perm = _rope_perm()
    for l in range(2):
        p = f"l{l}_"
        sh[p + "normcols"] = np.ascontiguousarray(np.stack([_col(inp[p + "norm1"]), _col(inp[p + "norm2"])], 1)).astype(f)
        sh[p + "w_mod"] = np.ascontiguousarray(inp[p + "w_mod"])
        sh[p + "b_mod"] = np.ascontiguousarray(inp[p + "b_mod"].reshape(1, -1))
        sh[p + "w_out"] = np.ascontiguousarray(inp[p + "w_out"])
        sh[p + "w_router"] = np.ascontiguousarray(inp[p + "w_router"])
        sh[p + "w1"] = np.ascontiguousarray(inp[p + "w1"][:E])
        sh[p + "w3"] = np.ascontiguousarray(inp[p + "w3"][:E])
        sh[p + "w2"] = np.ascontiguousarray(inp[p + "w2"][:E])
    w = inp["l0_w_in"]
    aq, bq, ak, bk, av, bv = (w[:, i * 512:(i + 1) * 512] for i in range(6))
    na = [np.concatenate([aq[:, i * 128:(i + 1) * 128], ak[:, i * 128:(i + 1) * 128], av[:, i * 128:(i + 1) * 128]], 1) for i in range(4)]
    sh["l0_w_na"] = np.ascontiguousarray(np.stack(na, 0))
    pidx = np.concatenate([perm, 64 + perm])
    df = []
    for h in range(4):
        q = bq[:, h * 128:(h + 1) * 128]
        k = bk[:, h * 128:(h + 1) * 128]
        df.append(np.concatenate([q, q[:, pidx], k, k[:, pidx], bv[:, h * 128:(h + 1) * 128]], 1))
    sh["l0_w_df"] = np.ascontiguousarray(np.stack(df, 0))
    sh["l0_na_bias"] = np.ascontiguousarray(_na_bias_tables(inp["l0_rpb"]).reshape(8, 128, 14 * 256))
    sh["l0_lam"] = np.ascontiguousarray(np.concatenate([inp["l0_lam_q1"], inp["l0_lam_k1"], inp["l0_lam_q2"], inp["l0_lam_k2"]], 0).reshape(1, 256))
    sh["l0_subln"] = np.ascontiguousarray(inp["l0_subln"].reshape(1, 128))
    w = inp["l1_w_in"]
    xc, gb, gc, ga, gg = (w[:, i * 512:(i + 1) * 512] for i in range(5))
    sh["l1_w_c"] = np.ascontiguousarray(np.stack([np.concatenate([xc[:, j * 128:(j + 1) * 128], gb[:, j * 128:(j + 1) * 128], gc[:, j * 128:(j + 1) * 128]], 1) for j in range(4)], 0))
    sh["l1_w_d"] = np.ascontiguousarray(np.stack([np.concatenate([ga[:, j * 128:(j + 1) * 128], gg[:, j * 128:(j + 1) * 128]], 1) for j in range(4)], 0))
    sh["l1_sc_w"] = np.ascontiguousarray(inp["l1_sc_w"].T.reshape(4, 128, 3).transpose(1, 0, 2))
    sh["l1_cf_w"] = np.ascontiguousarray(inp["l1_cf_w"].T.reshape(4, 128, 31).transpose(1, 0, 2))
    sh["l1_cf_vec"] = np.ascontiguousarray(np.stack([_col(inp["l1_cf_b"]), _col(inp["l1_ln_g"]), _col(inp["l1_ln_b"])], 1))
    sh["final_norm"] = np.ascontiguousarray(inp["final_norm"].reshape(1, -1))
    cos, sin = _rope_tables()
    sh["rope"] = np.ascontiguousarray(np.stack([cos, sin], 0))
    import ml_dtypes
    ci = np.zeros((128, 16, 2), np.float32)
    ci[:, :, 0] = np.arange(16)[None, :]
    ci[:, :, 1] = np.arange(128)[:, None]
    sh["cidx"] = ci.astype(ml_dtypes.bfloat16)
    sh["ident"] = np.eye(128, dtype=f)
    sh["ltri"] = np.triu(np.ones((128, 128), f), 1)
    sh["iota"] = np.arange(SEQ, dtype=f).reshape(1, SEQ)
    return sh


def core_inputs(inp, sh, samples):
    S = len(samples)
    m = dict(sh)
    m["x"] = np.ascontiguousarray(inp["x"][samples])
    m["ctx"] = np.ascontiguousarray(inp["ctx"][samples])
    cc = np.concatenate([inp["c"][samples], inp["c_ctx"][None, :]], 0)
    m["cT"] = np.ascontiguousarray(cc.reshape(S + 1, 8, 128).transpose(2, 1, 0))
    return m


NCORES = 8
_cache = {}


def kernel(**inputs):
    inp = {k: np.asarray(v) for k, v in inputs.items()}
    B = inp["x"].shape[0]
    S = B // NCORES
    E = 16
    key = (S, E)
    if key not in _cache:
        _cache[key] = build(S, E)
    nc = _cache[key]
    sh = prep_shared(inp, E)
    in_maps = [core_inputs(inp, sh, list(range(c * S, (c + 1) * S))) for c in range(NCORES)]
    res = run_bass_kernel_spmd(nc, in_maps, core_ids=list(range(NCORES)))
    out = np.concatenate([res.results[c]["out"] for c in range(NCORES)], 0)
    return out.astype(np.float32)
```

```python
import math
from contextlib import ExitStack
import numpy as np
import concourse.bass as bass
import concourse.mybir as mybir
from concourse.bass_utils import run_bass_kernel_spmd

F32 = mybir.dt.float32
BF16 = mybir.dt.bfloat16
AF = mybir.ActivationFunctionType
ALU = mybir.AluOpType
AX = mybir.AxisListType

D = 1024
SEQ = 2048
NCH = 16
CTX = 256
DE = 2816
NFC = 22
CAP = 256
EPS = 1e-6
GRID_W = 64
NEG = -30000.0


class _Rec:
    def __getattr__(self, name):
        def mk(*a, **k):
            return lambda e: getattr(e, name)(*a, **k)
        return mk


Q = _Rec()


class Prog:
    ENGS = ("pe", "act", "dve", "pool", "sp")
    INORDER = ("pe", "act", "dve")

    def __init__(self, nc):
        self.nc = nc
        self.ops = {e: [] for e in self.ENGS}
        self.last_w = {}
        self.readers = {}
        self.dma_since_barrier = []

    def op(self, eng, fn, reads=(), writes=(), dma=False):
        idx = len(self.ops[eng])
        deps = set()
        for k in reads:
            w = self.last_w.get(k)
            if w is not None:
                deps.add(w)
        for k in writes:
            w = self.last_w.get(k)
            if w is not None:
                deps.add(w)
            for r in self.readers.get(k, ()):
                deps.add(r)
        if eng == "pe":
            deps = {d for d in deps if not (d[0] == "pe" and not self.ops["pe"][d[1]]["dma"])}
        self.ops[eng].append(dict(fn=fn, deps=deps, inc=False, dma=dma))
        me = (eng, idx)
        for k in reads:
            lst = self.readers.setdefault(k, [])
            if not dma and eng in self.INORDER:
                lst[:] = [r for r in lst if not (r[0] == eng and not self.ops[eng][r[1]]["dma"])]
            lst.append(me)
        for k in writes:
            self.last_w[k] = me
            self.readers[k] = []
        if dma:
            self.dma_since_barrier.append(me)
        return me

    def barrier(self):
        targets = set(self.dma_since_barrier)
        for e in self.ENGS:
            for i in range(len(self.ops[e]) - 1, -1, -1):
                if not self.ops[e][i]["dma"] and self.ops[e][i]["fn"] is not None:
                    targets.add((e, i))
                    break
        for e in self.ENGS:
            self.ops[e].append(dict(fn=None, deps=set(targets), inc=False, dma=False))
        self.dma_since_barrier = []

    def emit(self, stack):
        nc = self.nc
        for e in self.ENGS:
            for o in self.ops[e]:
                for (de, di) in o["deps"]:
                    t = self.ops[de][di]
                    if not t["dma"]:
                        t["inc"] = True
        EPOCH = 30000
        ev = {}
        for e in self.ENGS:
            cnt = 0
            sem = None
            for i, o in enumerate(self.ops[e]):
                if o["dma"] or not o["inc"]:
                    continue
                if sem is None or cnt >= EPOCH:
                    sem = stack.enter_context(nc.semaphore(f"s_{e}_{i}"))
                    cnt = 0
                cnt += 1
                ev[(e, i)] = (sem, cnt)
                o["sem"] = sem
        NDS = 12
        for e in self.ENGS:
            if not any(o["dma"] for o in self.ops[e]):
                continue
            pool = [stack.enter_context(nc.semaphore(f"d_{e}_{k}")) for k in range(NDS)]
            vals = [0] * NDS
            prev = [None] * NDS
            k = 0
            for i, o in enumerate(self.ops[e]):
                if not o["dma"]:
                    continue
                slot = k % NDS
                k += 1
                vals[slot] += 16
                ev[(e, i)] = (pool[slot], vals[slot])
                o["sem"] = pool[slot]
                if prev[slot] is not None:
                    o["deps"].add(prev[slot])
                prev[slot] = (e, i)
        engobj = dict(pe=nc.tensor, act=nc.scalar, dve=nc.vector, pool=nc.gpsimd, sp=nc.sync)
        ops = self.ops

        def run(ename):
            def body(eng):
                waited = {}
                for o in ops[ename]:
                    for d in sorted(o["deps"]):
                        sem, val = ev[d]
                        key = id(sem)
                        if waited.get(key, 0) >= val:
                            continue
                        waited[key] = val
                        eng.wait_ge(sem, val)
                    if o["fn"] is None:
                        continue
                    ins = o["fn"](eng)
                    if o["dma"]:
                        ins.then_inc(o["sem"], 16)
                    elif o["inc"]:
                        ins.then_inc(o["sem"], 1)
            return body

        with nc.Block() as block:
            block.tensor(run("pe"))
            block.scalar(run("act"))
            block.vector(run("dve"))
            block.gpsimd(run("pool"))
            block.sync(run("sp"))


def _rope_tables():
    n_freq = 16
    freqs = (np.float32(10000.0) ** (-np.arange(n_freq, dtype=np.float32) / n_freq)).astype(np.float32)
    t = np.arange(SEQ)
    row = (t // GRID_W).astype(np.float32)
    col = (t % GRID_W).astype(np.float32)
    cos = np.zeros((64, SEQ), np.float32)
    sin = np.zeros((64, SEQ), np.float32)
    for f in range(64):
        pos = row if f < 32 else col
        i = f % 32
        ang = (pos * freqs[i % 16]).astype(np.float32)
        cos[f] = np.cos(ang)
        sin[f] = np.sin(ang) * (-1.0 if i < 16 else 1.0)
    return np.concatenate([cos, cos], 0), np.concatenate([sin, sin], 0)


def _rope_perm():
    p = np.zeros(64, np.int64)
    for f in range(64):
        base = 0 if f < 32 else 32
        i = f % 32
        p[f] = base + (i + 16 if i < 16 else i - 16)
    return p


def _na_blocks():
    out = []
    for b in range(8):
        if b == 0:
            out.append(list(range(0, 4)))
        elif b == 7:
            out.append(list(range(12, 16)))
        else:
            out.append(list(range(2 * b - 2, 2 * b + 4)))
    return out


def _na_class(b):
    return 0 if b == 0 else (2 if b == 7 else 1)


NA_CLS_OFF = [0, 4, 10]
NA_CLS_B = [0, 1, 7]


def _na_bias_tables(rpb):
    H = rpb.shape[0]
    tab = np.full((H, 128, 14, 256), NEG, np.float32)
    blocks = _na_blocks()
    kp = np.arange(128)
    kr_l = kp // 64
    kc = kp % 64
    qq = np.arange(256)
    qr_l = qq // 64
    qc = qq % 64
    cs = np.clip(qc - 8, 0, 48)
    for cls in range(3):
        b = NA_CLS_B[cls]
        for si, c in enumerate(blocks[b]):
            kr = 2 * c + kr_l
            r = 4 * b + qr_l
            rs = np.clip(r - 4, 0, 24)
            inrow = (kr[:, None] >= rs[None, :]) & (kr[:, None] <= rs[None, :] + 7)
            incol = (kc[:, None] >= cs[None, :]) & (kc[:, None] < cs[None, :] + 16)
            ok = inrow & incol
            dr = np.clip(kr[:, None] - r[None, :] + 7, 0, 14)
            dc = np.clip(kc[:, None] - qc[None, :] + 15, 0, 30)
            g = rpb[:, dr, dc]
            tab[:, :, NA_CLS_OFF[cls] + si, :] = np.where(ok[None], g, np.float32(NEG))
    return tab


class _Stop(Exception):
    pass


class Phase(ExitStack):
    def __exit__(self, et, ev, tb):
        super().__exit__(None, None, None)
        return False


def build(S, E, taps=(), stop=None):
    nc = bass.Bass("TRN2", target_bir_lowering=False)
    P = Prog(nc)
    st = ExitStack()

    def din(name, shape, dt=F32):
        return nc.dram_tensor(name, list(shape), dt, kind="ExternalInput").ap()

    x_d = din("x", [S, SEQ, D])
    c_d = din("cT", [128, 8, S + 1])
    ctx_d = din("ctx", [S, CTX, D])
    out_d = nc.dram_tensor("out", [S, SEQ, D], F32, kind="ExternalOutput").ap()
    tap_d = {t: nc.dram_tensor(f"tap_{t}", [SEQ, D], F32, kind="ExternalOutput").ap() for t in taps}
    L = []
    for l in range(2):
        p = {}
        p["normcols"] = din(f"l{l}_normcols", [128, 2, 8])
        p["w_mod"] = din(f"l{l}_w_mod", [D, 6 * D])
        p["b_mod"] = din(f"l{l}_b_mod", [1, 6 * D])
        p["w_out"] = din(f"l{l}_w_out", [D, D])
        p["w_router"] = din(f"l{l}_w_router", [D, 16])
        p["w1"] = din(f"l{l}_w1", [E, D, DE])
        p["w3"] = din(f"l{l}_w3", [E, D, DE])
        p["w2"] = din(f"l{l}_w2", [E, DE, D])
        if l == 0:
            p["w_na"] = din("l0_w_na", [4, D, 384])
            p["w_df"] = din("l0_w_df", [4, D, 640])
            p["na_bias"] = din("l0_na_bias", [8, 128, 14 * 256])
            p["lam"] = din("l0_lam", [1, 256])
            p["subln"] = din("l0_subln", [1, 128])
        else:
            p["w_c"] = din("l1_w_c", [4, D, 384])
            p["w_d"] = din("l1_w_d", [4, D, 256])
            p["sc_w"] = din("l1_sc_w", [128, 4, 3])
            p["cf_w"] = din("l1_cf_w", [128, 4, 31])
            p["cf_vec"] = din("l1_cf_vec", [128, 3, 4])
        L.append(p)
    fnorm_d = din("final_norm", [1, D])
    rope_d = din("rope", [2, 128, SEQ])
    cidx_d = din("cidx", [128, 16, 2], BF16)
    ident_d = din("ident", [128, 128])
    ltri_d = din("ltri", [128, 128])
    iota_d = din("iota", [1, SEQ])

    uid = [0]

    def un(name):
        uid[0] += 1
        return f"t{uid[0]}_{name}"

    def sb(name, shape, dt):
        return st.enter_context(nc.sbuf_tensor(un(name), list(shape), dt))

    def ps(name, shape, dt=F32):
        return st.enter_context(nc.psum_tensor(name, list(shape), dt))

    x = sb("x", [128, NCH, D], F32)
    ident_f = sb("ident_f", [128, 128], F32)
    ident_b = sb("ident_b", [128, 128], BF16)
    ones_f = sb("ones_f", [128, 128], F32)
    ltri_b = sb("ltri_b", [128, 128], BF16)
    ones_b = sb("ones_b", [128, 128], BF16)
    iota_t = sb("iota_t", [128, SEQ], F32)
    cidx = sb("cidx_sb", [128, 16, 2], BF16)
    modc = [sb(f"modc{l}", [128, 48, S + 1], F32) for l in range(2)]
    normc = [sb(f"normc{l}", [128, 2, 8], F32) for l in range(2)]
    siluc = sb("siluc", [128, 8, S + 1], BF16)
    AB = sb("AB", [128, 2, 8], F32)
    gate_b = sb("gate_b", [128, D], F32)
    small = sb("small", [128, 64], F32)
    stat = sb("stat", [128, 32], F32)
    junk = sb("junk", [128, D], F32)

    PB = [ps(f"pb{k}", [128, 512], F32) for k in range(8)]
    PBb = [t[:].bitcast(BF16) for t in PB]

    def K(*a):
        return a

    def stage(n):
        if stop is not None and n == stop:
            raise _Stop()

    P.op("sp", Q.dma_start(out=ident_f[:], in_=ident_d), writes=[K("ident_f")], dma=True)
    P.op("pool", Q.dma_start(out=ident_b[:], in_=ident_d), writes=[K("ident_b")], dma=True)
    P.op("pool", Q.dma_start(out=ltri_b[:], in_=ltri_d), writes=[K("ltri_b")], dma=True)
    P.op("sp", Q.dma_start(out=iota_t[:], in_=iota_d.partition_broadcast(128)), writes=[K("iota_t")], dma=True)
    P.op("sp", Q.dma_start(out=cidx[:], in_=cidx_d), writes=[K("cidx")], dma=True)
    P.op("dve", Q.memset(ones_f[:], 1.0), writes=[K("ones_f")])
    P.op("dve", Q.memset(ones_b[:], 1.0), writes=[K("ones_b")])
    for l in range(2):
        P.op("sp", Q.dma_start(out=normc[l][:], in_=L[l]["normcols"]), writes=[K("normc", l)], dma=True)

    with Phase() as ph:
        cT = ph.enter_context(nc.sbuf_tensor(un("cT_sb"), [128, 8, S + 1], F32))
        sg = ph.enter_context(nc.sbuf_tensor(un("sg_sb"), [128, 8, S + 1], F32))
        bm = ph.enter_context(nc.sbuf_tensor(un("bm_sb"), [1, 6 * D], BF16))
        wm = [ph.enter_context(nc.sbuf_tensor(un(f"wm{k}"), [128, 8, 512], BF16)) for k in range(2)]
        P.op("sp", Q.dma_start(out=cT[:], in_=c_d), writes=[K("cT")], dma=True)
        P.op("act", Q.activation(out=sg[:], in_=cT[:], func=AF.Sigmoid), reads=[K("cT")], writes=[K("sg")])
        P.op("dve", Q.tensor_tensor(out=siluc[:], in0=cT[:], in1=sg[:], op=ALU.mult),
             reads=[K("cT"), K("sg")], writes=[K("siluc")])
        blk = 0
        for l in range(2):
            P.op("pool", Q.dma_start(out=bm[:], in_=L[l]["b_mod"]), writes=[K("bm")], dma=True)
            for cb in range(12):
                w = wm[blk % 2]
                wk = K("wm", blk % 2)
                blk += 1
                src = L[l]["w_mod"][:, cb * 512:(cb + 1) * 512].rearrange("(kc p) n -> p kc n", p=128)
                P.op("pool", Q.dma_start(out=w[:], in_=src), writes=[wk], dma=True)
                pt = PB[cb % 2]
                pk = K("pb", cb % 2)
                for fcl in range(4):
                    o = pt[:, fcl * (S + 1):(fcl + 1) * (S + 1)]
                    for kc in range(8):
                        P.op("pe", Q.matmul(
                            o, w[:, kc, fcl * 128:(fcl + 1) * 128], siluc[:, kc, :], start=(kc == 0), stop=False),
                            reads=[wk, K("siluc")], writes=[pk])
                    f0 = cb * 512 + fcl * 128
                    P.op("pe", Q.matmul(
                        o, bm[0:1, f0:f0 + 128], ones_b[0:1, 0:S + 1], start=False, stop=True),
                        reads=[K("bm"), K("ones_b")], writes=[pk])
                P.op("dve", Q.tensor_copy(
                    out=modc[l][:, cb * 4:(cb + 1) * 4, :],
                    in_=pt[:, 0:4 * (S + 1)].rearrange("p (a b) -> p a b", b=S + 1)),
                    reads=[pk], writes=[K("modc", l)])
    P.barrier()

    def make_AB(l, which, col, nkey):
        sh = 0 if which == 0 else 24
        P.op("dve", Q.scalar_tensor_tensor(
            out=AB[:, 0, :], in0=modc[l][:, sh + 8:sh + 16, col], scalar=1.0, in1=normc[l][:, nkey, :],
            op0=ALU.add, op1=ALU.mult), reads=[K("modc", l), K("normc", l)], writes=[K("AB")])
        P.op("dve", Q.tensor_copy(out=AB[:, 1, :], in_=modc[l][:, sh:sh + 8, col]),
             reads=[K("modc", l)], writes=[K("AB")])

    def make_gate(l, which, col):
        g0 = 16 if which == 0 else 40
        with Phase() as ph:
            dg = ph.enter_context(nc.sbuf_tensor(un("dg"), [128, 8, 128], F32))
            for kc in range(8):
                P.op("dve", Q.tensor_scalar(
                    out=dg[:, kc, :], in0=ident_f[:], scalar1=modc[l][:, g0 + kc, col:col + 1], scalar2=None,
                    op0=ALU.mult), reads=[K("ident_f"), K("modc", l)], writes=[K("dg", kc)])
            for h in range(2):
                for k4 in range(4):
                    kc = h * 4 + k4
                    P.op("pe", Q.matmul(
                        PB[h][:, k4 * 128:(k4 + 1) * 128], ones_f[:], dg[:, kc, :], start=True, stop=True),
                        reads=[K("ones_f"), K("dg", kc)], writes=[K("pb", h)])
                P.op("act", Q.activation(out=gate_b[:, h * 512:(h + 1) * 512], in_=PB[h][:], func=AF.Copy),
                     reads=[K("pb", h)], writes=[K("gate_b")])
        P.barrier()

    def rms_xs(src, c, xs_out, keys_r, keys_w, width=D):
        sl = c % 16
        sq = stat[:, 2 * sl:2 * sl + 1]
        rs = stat[:, 2 * sl + 1:2 * sl + 2]
        sk = K("stat", sl)
        P.op("act", Q.activation(out=junk[:, 0:width], in_=src, func=AF.Square),
             reads=keys_r, writes=[K("junk")])
        P.op("dve", Q.reduce_sum(out=sq, in_=junk[:, 0:width], axis=AX.X),
             reads=[K("junk")], writes=[sk])
        P.op("dve", Q.tensor_scalar(out=rs, in0=sq, scalar1=1.0 / width, scalar2=EPS, op0=ALU.mult, op1=ALU.add),
             reads=[sk], writes=[sk])
        P.op("act", Q.activation(out=rs, in_=rs, func=AF.Sqrt), reads=[sk], writes=[sk])
        P.op("dve", Q.reciprocal(out=rs, in_=rs), reads=[sk], writes=[sk])
        P.op("dve", Q.tensor_scalar(out=xs_out, in0=src, scalar1=rs, scalar2=None, op0=ALU.mult),
             reads=keys_r + [sk], writes=keys_w)

    def norm_T(hT, hT_key, ntok_chunks, src_chunk, src_keys, xsg, group_cb=None, xs_keep=None):
        ng = ntok_chunks // 4
        for g in range(ng):
            for j in range(4):
                c = g * 4 + j
                if xs_keep is not None:
                    xo = xs_keep[:, c, :]
                    kw = [K("xs2", c)]
                else:
                    xo = xsg[:, j, :]
                    kw = [K("xsg", j)]
                rms_xs(src_chunk(c), c, xo, src_keys(c), kw)
            for dc in range(8):
                pt = PBb[dc % 2]
                pk = K("pb", dc % 2)
                for j in range(4):
                    c = g * 4 + j
                    if xs_keep is not None:
                        xi = xs_keep[:, c, dc * 128:(dc + 1) * 128]
                        kr = [K("xs2", c)]
                    else:
                        xi = xsg[:, j, dc * 128:(dc + 1) * 128]
                        kr = [K("xsg", j)]
                    P.op("pe", Q.transpose(pt[:, j * 128:(j + 1) * 128], xi, ident_b[:]),
                         reads=kr + [K("ident_b")], writes=[pk])
                P.op("act", Q.activation(
                    out=hT[:, dc, g * 512:(g + 1) * 512], in_=pt[:, 0:512], func=AF.Identity,
                    scale=AB[:, 0, dc:dc + 1], bias=AB[:, 1, dc:dc + 1]),
                    reads=[pk, K("AB")], writes=[K(hT_key, g)])
            if group_cb is not None:
                group_cb(g)

    def out_proj_unit(l, u, yT, yT_key, wo):
        P.op("pool", Q.dma_start(out=wo[0][:], in_=L[l]["w_out"][u * 128:(u + 1) * 128, :]),
             writes=[K("wo_f")], dma=True)
        P.op("dve", Q.tensor_tensor(out=wo[1][:], in0=wo[0][:], in1=gate_b[:], op=ALU.mult),
             reads=[K("wo_f"), K("gate_b")], writes=[K("wo_b")])
        for c in range(NCH):
            for h in range(2):
                bk = 6 + h
                P.op("pe", Q.matmul(
                    PB[bk][:], yT[:, c * 128:(c + 1) * 128], wo[1][:, h * 512:(h + 1) * 512], start=True, stop=True),
                    reads=[K(yT_key), K("wo_b")], writes=[K("pb", bk)])
                P.op("dve", Q.tensor_tensor(
                    out=x[:, c, h * 512:(h + 1) * 512], in0=PB[bk][:], in1=x[:, c, h * 512:(h + 1) * 512], op=ALU.add),
                    reads=[K("pb", bk), K("x", c)], writes=[K("x", c)])

    def proj_T(dst, dst_keys, w, wkey, col0, hT, hT_key, ntok, evac="act", tok0=0, dst0=0, pbs=(0, 1)):
        nb = (ntok + 511) // 512
        for b in range(nb):
            n = min(512, ntok - b * 512)
            pt = PB[pbs[b % len(pbs)]]
            pk = K("pb", pbs[b % len(pbs)])
            for kc in range(8):
                P.op("pe", Q.matmul(
                    pt[:, 0:n], w[:, kc, col0:col0 + 128], hT[:, kc, tok0 + b * 512: tok0 + b * 512 + n],
                    start=(kc == 0), stop=(kc == 7)), reads=[wkey, K(hT_key, (tok0 + b * 512) // 512)], writes=[pk])
            o = dst[:, dst0 + b * 512: dst0 + b * 512 + n]
            if evac == "act":
                P.op("act", Q.activation(out=o, in_=pt[:, 0:n], func=AF.Copy),
                     reads=[pk], writes=dst_keys)
            else:
                evac(b, n, pt, pk, o)

    def tap(name, s):
        if name in tap_d and s == 0:
            P.op("sp", Q.dma_start(out=tap_d[name].rearrange("(c p) d -> p c d", p=128), in_=x[:]),
                 reads=[K("x", c) for c in range(NCH)], dma=True)

    SC_BANKS = (2, 3, 4, 0, 1)
    sc_rot = [0]

    def attn_scores(pb, qb, qT, kT, chunks, eb_fn, Pt, Pt_key, etmp, qkey, kkey):
        nk = len(chunks)
        ngr = (nk + 1) // 2
        for gi in range(ngr):
            bk = SC_BANKS[sc_rot[0] % len(SC_BANKS)]
            sc_rot[0] += 1
            pt = PB[bk]
            pk = K("pb", bk)
            sub = chunks[gi * 2: gi * 2 + 2]
            for j, kc in enumerate(sub):
                P.op("pe", Q.matmul(
                    pt[:, j * 256:(j + 1) * 256], kT[pb:pb + 64, kc * 128:(kc + 1) * 128],
                    qT[pb:pb + 64, qb * 256:(qb + 1) * 256], start=True, stop=True),
                    reads=[qkey, kkey], writes=[pk])
            w = 256 * len(sub)
            dst = Pt[:, gi * 2: gi * 2 + len(sub), :].rearrange("p a b -> p (a b)")
            eb = eb_fn(gi * 2, len(sub)) if eb_fn is not None else None
            if eb is None:
                P.op("act", Q.activation(out=dst, in_=pt[:, 0:w], func=AF.Exp, scale=0.125),
                     reads=[pk], writes=[K(Pt_key, gi)])
            else:
                et = etmp[gi % 2]
                ek = K("etmp", gi % 2)
                P.op("act", Q.activation(out=et[:, 0:w], in_=pt[:, 0:w], func=AF.Exp, scale=0.125),
                     reads=[pk], writes=[ek])
                P.op("dve", Q.tensor_tensor(out=dst, in0=et[:, 0:w], in1=eb, op=ALU.mult),
                     reads=[ek, K("big16")], writes=[K(Pt_key, gi)])

    def attn_pv(qb, chunks, v_fn, dv, Pt, Pt_key, out_cb, vkey):
        nk = len(chunks)
        for qi in range(2):
            qc = qb * 2 + qi
            pt = PB[5]
            pk = K("pb", 5)
            for i, kc in enumerate(chunks):
                P.op("pe", Q.matmul(
                    pt[:, 0:dv + 1], Pt[:, i, qi * 128:(qi + 1) * 128], v_fn(kc), start=(i == 0), stop=(i == nk - 1)),
                    reads=[K(Pt_key, i // 2), vkey], writes=[pk])
            out_cb(qc, pt, pk)

    def attn_pipeline(blocks_list):
        pend = None
        for sa, pa_ in blocks_list:
            attn_scores(*sa)
            if pend is not None:
                attn_pv(*pend)
            pend = pa_
        if pend is not None:
            attn_pv(*pend)

    try:
        XK = [K("x", c) for c in range(NCH)]
        for s in range(S):
            P.op("sp", Q.dma_start(out=x[:], in_=x_d[s].rearrange("(c p) d -> p c d", p=128)),
                 writes=XK, dma=True)
            stage(0)
            for l in range(2):
                make_gate(l, 0, s)
                with Phase() as ph:
                    def pa(name, shape, dt):
                        return ph.enter_context(nc.sbuf_tensor(un(name), list(shape), dt))
                    hT = pa("hT", [128, 8, SEQ], BF16)
                    wo = (pa("wo_f", [128, D], F32), pa("wo_b", [128, D], BF16))
                    yT = pa("yT", [128, SEQ], BF16)
                    if l == 0:
                        Pt = [pa(f"Pt{k}", [128, 18, 256], BF16) for k in range(2)]
                        xsg = Pt[0][:, 0:16, :].rearrange("p (a b) c -> p a (b c)", a=4)
                    else:
                        xsg = pa("xsg", [128, 4, D], BF16)
                    make_AB(l, 0, s, 0)
                    norm_T(hT, "hT", NCH, lambda c: x[:, c, :], lambda c: [K("x", c)], xsg)
                    if l == 0:
                        hcT = pa("hcT", [128, 8, CTX], BF16)
                        big16 = pa("big16", [128, 2, SEQ], F32)
                        cx = big16[:, 0, :].rearrange("p (a b) -> p a b", a=2)
                        P.op("sp", Q.dma_start(out=cx, in_=ctx_d[s].rearrange("(c p) d -> p c d", p=128)),
                             writes=[K("big16")], dma=True)
                        make_AB(l, 0, S, 0)
                        for j in range(2):
                            rms_xs(cx[:, j, :], j, xsg[:, j, :], [K("big16")], [K("xsg", j)])
                        for dc in range(8):
                            pt = PBb[dc % 2]
                            pk = K("pb", dc % 2)
                            for j in range(2):
                                P.op("pe", Q.transpose(
                                    pt[:, j * 128:(j + 1) * 128], xsg[:, j, dc * 128:(dc + 1) * 128], ident_b[:]),
                                    reads=[K("xsg", j), K("ident_b")], writes=[pk])
                            P.op("act", Q.activation(
                                out=hcT[:, dc, :], in_=pt[:, 0:256], func=AF.Identity,
                                scale=AB[:, 0, dc:dc + 1], bias=AB[:, 1, dc:dc + 1]),
                                reads=[pk, K("AB")], writes=[K("hcT", 0)])
                        P.barrier()
                        stage(1)
                        wu = pa("wu", [128, 8, 640], BF16)
                        qT = pa("qT", [128, SEQ], BF16)
                        kT = pa("kT", [128, SEQ + CTX], BF16)
                        vv = pa("vv", [128, 18, 130], BF16)
                        etmp = [pa(f"etmp{k}", [128, 512], F32) for k in range(2)]
                        EB = big16[:].rearrange("p a b -> p (a b)")[:, 0:14 * 256].rearrange("p (a b) -> p a b", a=14)
                        rope = big16
                        ytm = pa("ytm", [128, NCH, 128], BF16)
                        tmpA = pa("tmpA", [128, 512], F32)
                        tmpB = pa("tmpB", [128, 512], F32)
                        lamv = pa("lamv", [128, 4, 64], F32)
                        lam = pa("lam", [128, 4], F32)
                        subl = pa("subl", [128, 128], F32)
                        o0 = pa("o0", [128, 2, 129], F32)
                        P.op("sp", Q.dma_start(out=lamv[:].rearrange("p a b -> p (a b)"), in_=L[0]["lam"].partition_broadcast(128)),
                             writes=[K("lamv")], dma=True)
                        P.op("sp", Q.dma_start(out=subl[:], in_=L[0]["subln"].partition_broadcast(128)),
                             writes=[K("subl")], dma=True)
                        lam_init = 0.8 - 0.6 * math.exp(-0.3 * 0)
                        for j in range(2):
                            P.op("dve", Q.tensor_tensor(out=tmpA[:, 0:64], in0=lamv[:, 2 * j, :], in1=lamv[:, 2 * j + 1, :], op=ALU.mult),
                                 reads=[K("lamv")], writes=[K("tmpA")])
                            P.op("dve", Q.reduce_sum(out=lam[:, j:j + 1], in_=tmpA[:, 0:64], axis=AX.X),
                                 reads=[K("tmpA")], writes=[K("lam")])
                        P.op("act", Q.activation(out=lam[:, 0:2], in_=lam[:, 0:2], func=AF.Exp), reads=[K("lam")], writes=[K("lam")])
                        P.op("dve", Q.tensor_tensor(out=lam[:, 2:3], in0=lam[:, 0:1], in1=lam[:, 1:2], op=ALU.subtract),
                             reads=[K("lam")], writes=[K("lam")])
                        P.op("dve", Q.tensor_scalar(out=lam[:, 3:4], in0=lam[:, 2:3], scalar1=lam_init, scalar2=-1.0, op0=ALU.add, op1=ALU.mult),
                             reads=[K("lam")], writes=[K("lam")])
                        P.op("dve", Q.tensor_scalar(out=subl[:], in0=subl[:], scalar1=1.0 - lam_init, scalar2=None, op0=ALU.mult),
                             reads=[K("subl")], writes=[K("subl")])
                        blocks = _na_blocks()
                        for u in range(8):
                            is_na = u < 4
                            ncol = 384 if is_na else 640
                            wsrc = (L[0]["w_na"][u] if is_na else L[0]["w_df"][u - 4]).rearrange("(kc p) n -> p kc n", p=128)
                            P.op("pool", Q.dma_start(out=wu[:, :, 0:ncol], in_=wsrc),
                                 writes=[K("wu")], dma=True)
                            if is_na:
                                proj_T(qT, [K("qT")], wu, K("wu"), 0, hT, "hT", SEQ)
                                proj_T(kT, [K("kT")], wu, K("wu"), 128, hT, "hT", SEQ)
                                proj_T(kT, [K("kT")], wu, K("wu"), 128, hcT, "hcT", CTX, dst0=SEQ)
                                vcol, dv = 256, 64
                            else:
                                if u == 4:
                                    P.op("sp", Q.dma_start(out=rope[:], in_=rope_d.rearrange("a p t -> p a t")),
                                         writes=[K("big16")], dma=True)
                                for (dst, dkey, c1, c2) in ((qT, K("qT"), 0, 128), (kT, K("kT"), 256, 384)):
                                    for b in range(4):
                                        for (pi, cc) in ((0, c1), (1, c2)):
                                            for kc in range(8):
                                                P.op("pe", Q.matmul(
                                                    PB[pi][:], wu[:, kc, cc:cc + 128], hT[:, kc, b * 512:(b + 1) * 512],
                                                    start=(kc == 0), stop=(kc == 7)),
                                                    reads=[K("wu"), K("hT", b)], writes=[K("pb", pi)])
                                        P.op("dve", Q.tensor_tensor(out=tmpA[:], in0=PB[0][:], in1=rope[:, 0, b * 512:(b + 1) * 512], op=ALU.mult),
                                             reads=[K("pb", 0), K("big16")], writes=[K("tmpA")])
                                        P.op("dve", Q.tensor_tensor(out=tmpB[:], in0=PB[1][:], in1=rope[:, 1, b * 512:(b + 1) * 512], op=ALU.mult),
                                             reads=[K("pb", 1), K("big16")], writes=[K("tmpB")])
                                        P.op("dve", Q.tensor_tensor(out=dst[:, b * 512:(b + 1) * 512], in0=tmpA[:], in1=tmpB[:], op=ALU.add),
                                             reads=[K("tmpA"), K("tmpB")], writes=[dkey])
                                proj_T(kT, [K("kT")], wu, K("wu"), 256, hcT, "hcT", CTX, dst0=SEQ)
                                vcol, dv = 512, 128
                            for c in range(18):
                                src = hT[:, :, c * 128:(c + 1) * 128] if c < 16 else hcT[:, :, (c - 16) * 128:(c - 15) * 128]
                                srck = K("hT", c // 4) if c < 16 else K("hcT", 0)
                                pt = PB[c % 2]
                                pk = K("pb", c % 2)
                                for kc in range(8):
                                    P.op("pe", Q.matmul(
                                        pt[:, 0:128], src[:, kc, :], wu[:, kc, vcol:vcol + 128], start=(kc == 0), stop=(kc == 7)),
                                        reads=[K("wu"), srck], writes=[pk])
                                if is_na:
                                    P.op("act", Q.activation(
                                        out=vv[:, c, :].rearrange("p (h d) -> p h d", h=2)[:, :, 0:64],
                                        in_=pt[:, 0:128].rearrange("p (h d) -> p h d", h=2), func=AF.Copy),
                                        reads=[pk], writes=[K("vv")])
                                else:
                                    P.op("act", Q.activation(out=vv[:, c, 0:128], in_=pt[:, 0:128], func=AF.Copy),
                                         reads=[pk], writes=[K("vv")])
                            if is_na:
                                P.op("dve", Q.memset(vv[:].rearrange("p c (h d) -> p c h d", h=2)[:, :, :, 64:65], 1.0),
                                     writes=[K("vv")])
                            else:
                                P.op("dve", Q.memset(vv[:, :, 128:129], 1.0), writes=[K("vv")])
                            if is_na:
                                for hh in range(2):
                                    head = 2 * u + hh
                                    P.op("sp", Q.dma_start(
                                        out=EB.rearrange("p a b -> p (a b)"), in_=L[0]["na_bias"][head]),
                                        writes=[K("big16")], dma=True)
                                    for a in range(2):
                                        P.op("act", Q.activation(out=EB[:, a * 7:(a + 1) * 7, :], in_=EB[:, a * 7:(a + 1) * 7, :], func=AF.Exp),
                                             reads=[K("big16")], writes=[K("big16")])

                                    def ocb(qc, pt, pk, hh=hh):
                                        P.op("dve", Q.reciprocal(out=small[:, 2:3], in_=pt[:, 64:65]),
                                             reads=[pk], writes=[K("small")])
                                        P.op("dve", Q.tensor_scalar(out=ytm[:, qc, hh * 64:(hh + 1) * 64], in0=pt[:, 0:64],
                                                                              scalar1=small[:, 2:3], scalar2=None, op0=ALU.mult),
                                             reads=[pk, K("small")], writes=[K("ytm", qc)])

                                    bl = []
                                    for qb in range(8):
                                        nb = len(blocks[qb])
                                        off = NA_CLS_OFF[_na_class(qb)]

                                        def ebf(i0, n, nb=nb, off=off):
                                            if i0 >= nb:
                                                return None
                                            assert i0 + n <= nb
                                            return EB[:, off + i0:off + i0 + n, :].rearrange("p a b -> p (a b)")

                                        ch = blocks[qb] + [16, 17]
                                        pi = qb % 2
                                        vf = (lambda kc, hh=hh: vv[:, kc, hh * 65:(hh + 1) * 65])
                                        bl.append(((64 * hh, qb, qT, kT, ch, ebf, Pt[pi], ("Pt", pi), etmp, K("qT"), K("kT")),
                                                   (qb, ch, vf, 64, Pt[pi], ("Pt", pi), ocb, K("vv"))))
                                    attn_pipeline(bl)
                            else:
                                def ocb0(qc, pt, pk):
                                    P.op("act", Q.activation(out=o0[:, qc % 2, :], in_=pt[:, 0:129], func=AF.Copy),
                                         reads=[pk], writes=[K("o0", qc % 2)])

                                def ocb1(qc, pt, pk):
                                    r0, r1, ssq, rs = small[:, 4:5], small[:, 5:6], small[:, 6:7], small[:, 7:8]
                                    ok = K("o0", qc % 2)
                                    P.op("dve", Q.reciprocal(out=r0, in_=o0[:, qc % 2, 128:129]), reads=[ok], writes=[K("small")])
                                    P.op("dve", Q.reciprocal(out=r1, in_=pt[:, 128:129]), reads=[pk], writes=[K("small")])
                                    P.op("dve", Q.tensor_tensor(out=r1, in0=r1, in1=lam[:, 3:4], op=ALU.mult),
                                         reads=[K("small"), K("lam")], writes=[K("small")])
                                    P.op("dve", Q.tensor_scalar(out=tmpA[:, 0:128], in0=o0[:, qc % 2, 0:128], scalar1=r0, scalar2=None, op0=ALU.mult),
                                         reads=[ok, K("small")], writes=[K("tmpA")])
                                    P.op("dve", Q.scalar_tensor_tensor(out=tmpA[:, 0:128], in0=pt[:, 0:128], scalar=r1, in1=tmpA[:, 0:128],
                                                                                 op0=ALU.mult, op1=ALU.add),
                                         reads=[pk, K("small"), K("tmpA")], writes=[K("tmpA")])
                                    P.op("act", Q.activation(out=tmpB[:, 0:128], in_=tmpA[:, 0:128], func=AF.Square),
                                         reads=[K("tmpA")], writes=[K("tmpB")])
                                    P.op("dve", Q.reduce_sum(out=ssq, in_=tmpB[:, 0:128], axis=AX.X), reads=[K("tmpB")], writes=[K("small")])
                                    P.op("dve", Q.tensor_scalar(out=rs, in0=ssq, scalar1=1.0 / 128, scalar2=EPS, op0=ALU.mult, op1=ALU.add),
                                         reads=[K("small")], writes=[K("small")])
                                    P.op("act", Q.activation(out=rs, in_=rs, func=AF.Sqrt), reads=[K("small")], writes=[K("small")])
                                    P.op("dve", Q.reciprocal(out=rs, in_=rs), reads=[K("small")], writes=[K("small")])
                                    P.op("dve", Q.scalar_tensor_tensor(out=ytm[:, qc, :], in0=tmpA[:, 0:128], scalar=rs, in1=subl[:],
                                                                                 op0=ALU.mult, op1=ALU.mult),
                                         reads=[K("tmpA"), K("small"), K("subl")], writes=[K("ytm", qc)])

                                bl = []
                                ch = list(range(18))
                                vf = (lambda kc: vv[:, kc, 0:129])
                                for qb in range(8):
                                    for m in range(2):
                                        bl.append(((64 * m, qb, qT, kT, ch, None, Pt[m], ("Pt", m), etmp, K("qT"), K("kT")),
                                                   (qb, ch, vf, 128, Pt[m], ("Pt", m), ocb0 if m == 0 else ocb1, K("vv"))))
                                attn_pipeline(bl)
                            for g in range(4):
                                pt = PBb[g % 2]
                                pk = K("pb", g % 2)
                                for j in range(4):
                                    c = g * 4 + j
                                    P.op("pe", Q.transpose(pt[:, j * 128:(j + 1) * 128], ytm[:, c, :], ident_b[:]),
                                         reads=[K("ytm", c), K("ident_b")], writes=[pk])
                                P.op("act", Q.activation(out=yT[:, g * 512:(g + 1) * 512], in_=pt[:, 0:512], func=AF.Copy),
                                     reads=[pk], writes=[K("yT")])
                            out_proj_unit(l, u, yT, "yT", wo)
                            stage(20 + u)
                    else:
                        P.barrier()
                        wu = pa("wu", [128, 8, 384], BF16)
                        scw = pa("scw", [128, 4, 3], F32)
                        cfw = pa("cfw", [128, 4, 31], F32)
                        cfv = pa("cfv", [128, 3, 4], F32)
                        P.op("sp", Q.dma_start(out=scw[:], in_=L[1]["sc_w"]), writes=[K("scw")], dma=True)
                        P.op("sp", Q.dma_start(out=cfw[:], in_=L[1]["cf_w"]), writes=[K("cfw")], dma=True)
                        P.op("sp", Q.dma_start(out=cfv[:], in_=L[1]["cf_vec"]), writes=[K("cfv")], dma=True)
                        va = pa("va", [128, SEQ + 32], F32)
                        vb = pa("vb", [128, SEQ], F32)
                        vc = pa("vc", [128, SEQ], F32)
                        ud = pa("ud", [128, 4, SEQ], F32)
                        P.op("dve", Q.memset(va[:], 0.0), writes=[K("va")])
                        for j in range(4):
                            P.op("pool", Q.dma_start(out=wu[:, :, 0:384], in_=L[1]["w_c"][j].rearrange("(kc p) n -> p kc n", p=128)),
                                 writes=[K("wu")], dma=True)
                            proj_T(vb, [K("vb")], wu, K("wu"), 0, hT, "hT", SEQ)

                            def ev_gc(b, n, pt, pk, o):
                                P.op("dve", Q.tensor_tensor(out=o, in0=pt[:, 0:n], in1=vb[:, b * 512:b * 512 + n], op=ALU.mult),
                                     reads=[pk, K("vb")], writes=[K("va")])
                            proj_T(va, [K("va")], wu, K("wu"), 256, hT, "hT", SEQ, evac=ev_gc, dst0=1)
                            proj_T(vc, [K("vc")], wu, K("wu"), 128, hT, "hT", SEQ)
                            P.op("dve", Q.tensor_scalar(out=vb[:], in0=va[:, 0:SEQ], scalar1=scw[:, j, 0:1], scalar2=None, op0=ALU.mult),
                                 reads=[K("va"), K("scw")], writes=[K("vb")])
                            for k in (1, 2):
                                P.op("dve", Q.scalar_tensor_tensor(out=vb[:], in0=va[:, k:k + SEQ], scalar=scw[:, j, k:k + 1], in1=vb[:],
                                                                                       op0=ALU.mult, op1=ALU.add),
                                     reads=[K("va"), K("scw"), K("vb")], writes=[K("vb")])
                            P.op("dve", Q.tensor_tensor(out=yT[:], in0=vb[:], in1=vc[:], op=ALU.mult),
                                 reads=[K("vb"), K("vc")], writes=[K("yT")])
                            out_proj_unit(l, j, yT, "yT", wo)
                        P.op("dve", Q.memset(va[:], 0.0), reads=[K("va")], writes=[K("va")])
                        for j in range(4):
                            P.op("pool", Q.dma_start(out=wu[:, :, 0:256], in_=L[1]["w_d"][j].rearrange("(kc p) n -> p kc n", p=128)),
                                 writes=[K("wu")], dma=True)

                            def ev_sig(b, n, pt, pk, o):
                                P.op("act", Q.activation(out=o, in_=pt[:, 0:n], func=AF.Sigmoid), reads=[pk], writes=[K("vb")])
                            proj_T(vb, [K("vb")], wu, K("wu"), 128, hT, "hT", SEQ, evac=ev_sig)

                            def ev_glu(b, n, pt, pk, o):
                                P.op("dve", Q.tensor_tensor(out=o, in0=pt[:, 0:n], in1=vb[:, b * 512:b * 512 + n], op=ALU.mult),
                                     reads=[pk, K("vb")], writes=[K("va")])
                            proj_T(va, [K("va")], wu, K("wu"), 0, hT, "hT", SEQ, evac=ev_glu, dst0=15)
                            P.op("dve", Q.tensor_scalar(out=ud[:, j, :], in0=va[:, 0:SEQ], scalar1=cfw[:, j, 0:1], scalar2=cfv[:, 0, j:j + 1],
                                                                       op0=ALU.mult, op1=ALU.add),
                                 reads=[K("va"), K("cfw"), K("cfv")], writes=[K("ud", j)])
                            for k in range(1, 31):
                                P.op("dve", Q.scalar_tensor_tensor(out=ud[:, j, :], in0=va[:, k:k + SEQ], scalar=cfw[:, j, k:k + 1], in1=ud[:, j, :],
                                                                                       op0=ALU.mult, op1=ALU.add),
                                     reads=[K("va"), K("cfw"), K("ud", j)], writes=[K("ud", j)])
                        for b in range(4):
                            sl = slice(b * 512, (b + 1) * 512)
                            for j in range(4):
                                P.op("pe", Q.matmul(PB[2][:], ones_f[:], ud[:, j, sl], start=(j == 0), stop=(j == 3)),
                                     reads=[K("ones_f"), K("ud", j)], writes=[K("pb", 2)])
                            for j in range(4):
                                P.op("act", Q.activation(out=vb[:, j * 512:(j + 1) * 512], in_=ud[:, j, sl], func=AF.Square),
                                     reads=[K("ud", j)], writes=[K("vb")])
                            for j in range(4):
                                P.op("pe", Q.matmul(PB[3][:], ones_f[:], vb[:, j * 512:(j + 1) * 512], start=(j == 0), stop=(j == 3)),
                                     reads=[K("ones_f"), K("vb")], writes=[K("pb", 3)])
                            mean, rstd, t0, t1 = vc[:, 0:512], vc[:, 512:1024], vc[:, 1024:1536], vc[:, 1536:2048]
                            P.op("dve", Q.tensor_scalar(out=mean, in0=PB[2][:], scalar1=1.0 / 512, scalar2=None, op0=ALU.mult),
                                 reads=[K("pb", 2)], writes=[K("vc")])
                            P.op("dve", Q.tensor_tensor(out=t0, in0=mean, in1=mean, op=ALU.mult), reads=[K("vc")], writes=[K("vc")])
                            P.op("dve", Q.scalar_tensor_tensor(out=rstd, in0=PB[3][:], scalar=1.0 / 512, in1=t0, op0=ALU.mult, op1=ALU.subtract),
                                 reads=[K("pb", 3), K("vc")], writes=[K("vc")])
                            P.op("dve", Q.tensor_scalar(out=rstd, in0=rstd, scalar1=EPS, scalar2=None, op0=ALU.add), reads=[K("vc")], writes=[K("vc")])
                            P.op("act", Q.activation(out=rstd, in_=rstd, func=AF.Sqrt), reads=[K("vc")], writes=[K("vc")])
                            P.op("dve", Q.reciprocal(out=rstd, in_=rstd), reads=[K("vc")], writes=[K("vc")])
                            for j in range(4):
                                P.op("dve", Q.tensor_tensor(out=t1, in0=ud[:, j, sl], in1=mean, op=ALU.subtract),
                                     reads=[K("ud", j), K("vc")], writes=[K("vc")])
                                P.op("dve", Q.tensor_tensor(out=t1, in0=t1, in1=rstd, op=ALU.mult), reads=[K("vc")], writes=[K("vc")])
                                P.op("act", Q.activation(out=t1, in_=t1, func=AF.Identity, scale=cfv[:, 1, j:j + 1], bias=cfv[:, 2, j:j + 1]),
                                     reads=[K("vc"), K("cfv")], writes=[K("vc")])
                                P.op("act", Q.activation(out=vb[:, 0:512], in_=t1, func=AF.Sigmoid), reads=[K("vc")], writes=[K("vb")])
                                P.op("dve", Q.tensor_tensor(out=ud[:, j, sl], in0=t1, in1=vb[:, 0:512], op=ALU.mult),
                                     reads=[K("vc"), K("vb")], writes=[K("ud", j)])
                        for j in range(4):
                            P.op("act", Q.activation(out=yT[:], in_=ud[:, j, :], func=AF.Copy), reads=[K("ud", j)], writes=[K("yT")])
                            out_proj_unit(l, 4 + j, yT, "yT", wo)
                P.barrier()
                tap(f"mix{l}", s)
                stage(3 if l == 0 else 6)

                make_gate(l, 1, s)
                with Phase() as ph:
                    def pa(name, shape, dt):
                        return ph.enter_context(nc.sbuf_tensor(un(name), list(shape), dt))
                    xs2 = pa("xs2", [128, NCH, D], BF16)
                    wr = pa("wr", [128, 8, 16], BF16)
                    aff_tm = pa("aff_tm", [128, NCH, 16], F32)
                    mask_f = pa("mask_f", [128, NCH, 16], F32)
                    mask_b = pa("mask_b", [128, NCH, 16], BF16)
                    pos_tm = pa("pos_tm", [128, NCH, 16], F32)
                    R = pa("R", [128, NCH, 16, 4], BF16)
                    hi_f = pa("hi_f", [128, NCH, 16], F32)
                    P.op("pool", Q.dma_start(out=wr[:], in_=L[l]["w_router"].rearrange("(kc p) n -> p kc n", p=128)),
                         writes=[K("wr")], dma=True)
                    make_AB(l, 1, s, 1)
                    with Phase() as ph2:
                        def pb2(name, shape, dt):
                            return ph2.enter_context(nc.sbuf_tensor(un(name), list(shape), dt))
                        hTg = pb2("hTg", [128, 8, 512], BF16)
                        expT = pb2("expT", [16, SEQ], F32)
                        affT = pb2("affT", [16, SEQ], F32)
                        work = pb2("work", [16, SEQ], F32)
                        maskT = pb2("maskT", [16, SEQ], F32)
                        m8 = pb2("m8", [16, 8], F32)
                        thr = pb2("thr", [16, 1], F32)
                        for g in range(4):
                            for j in range(4):
                                c = g * 4 + j
                                rms_xs(x[:, c, :], c, xs2[:, c, :], [K("x", c)], [K("xs2", c)])
                            for dc in range(8):
                                pt = PBb[dc % 2]
                                pk = K("pb", dc % 2)
                                for j in range(4):
                                    c = g * 4 + j
                                    P.op("pe", Q.transpose(pt[:, j * 128:(j + 1) * 128], xs2[:, c, dc * 128:(dc + 1) * 128], ident_b[:]),
                                         reads=[K("xs2", c), K("ident_b")], writes=[pk])
                                P.op("act", Q.activation(out=hTg[:, dc, :], in_=pt[:, 0:512], func=AF.Identity,
                                                                                 scale=AB[:, 0, dc:dc + 1], bias=AB[:, 1, dc:dc + 1]),
                                     reads=[pk, K("AB")], writes=[K("hTg")])
                            for kc in range(8):
                                P.op("pe", Q.matmul(PB[2][0:16, :], wr[:, kc, :], hTg[:, kc, :], start=(kc == 0), stop=(kc == 7)),
                                     reads=[K("wr"), K("hTg")], writes=[K("pb", 2)])
                            P.op("act", Q.activation(out=expT[:, g * 512:(g + 1) * 512], in_=PB[2][0:16, :], func=AF.Exp),
                                 reads=[K("pb", 2)], writes=[K("expT")])
                            for j in range(4):
                                for kc in range(8):
                                    P.op("pe", Q.matmul(PB[3][:, j * 16:(j + 1) * 16], hTg[:, kc, j * 128:(j + 1) * 128], wr[:, kc, :],
                                                                              start=(kc == 0), stop=(kc == 7)),
                                         reads=[K("wr"), K("hTg")], writes=[K("pb", 3)])
                            P.op("act", Q.activation(out=aff_tm[:, g * 4:(g + 1) * 4, :].rearrange("p a b -> p (a b)"), in_=PB[3][:, 0:64], func=AF.Exp),
                                 reads=[K("pb", 3)], writes=[K("aff_tm")])
                        stage(41 if l == 0 else 410)
                        for b in range(4):
                            P.op("pe", Q.matmul(PB[2][0:16, :], ones_f[0:16, 0:16], expT[:, b * 512:(b + 1) * 512], start=True, stop=True),
                                 reads=[K("ones_f"), K("expT")], writes=[K("pb", 2)])
                            P.op("dve", Q.reciprocal(out=work[:, b * 512:(b + 1) * 512], in_=PB[2][0:16, :]),
                                 reads=[K("pb", 2)], writes=[K("work")])
                        P.op("dve", Q.tensor_tensor(out=affT[:], in0=expT[:], in1=work[:], op=ALU.mult),
                             reads=[K("expT"), K("work")], writes=[K("affT")])
                        stage(42 if l == 0 else 420)
                        P.op("dve", Q.tensor_copy(out=work[:], in_=affT[:]), reads=[K("affT")], writes=[K("work")])
                        for r in range(CAP // 8):
                            P.op("dve", Q.max(out=m8[:], in_=work[:]), reads=[K("work")], writes=[K("m8")])
                            if r < CAP // 8 - 1:
                                P.op("dve", Q.match_replace(out=work[:], in_to_replace=m8[:], in_values=work[:], imm_value=-1.0),
                                     reads=[K("m8"), K("work")], writes=[K("work")])
                        P.op("dve", Q.tensor_reduce(out=thr[:], in_=m8[:], axis=AX.X, op=ALU.min), reads=[K("m8")], writes=[K("thr")])
                        P.op("dve", Q.tensor_scalar(out=maskT[:], in0=affT[:], scalar1=thr[:, 0:1], scalar2=None, op0=ALU.is_ge),
                             reads=[K("affT"), K("thr")], writes=[K("maskT")])
                        stage(43 if l == 0 else 430)
                        for c in range(NCH):
                            P.op("pe", Q.matmul(PB[0][:, c * 16:(c + 1) * 16], maskT[:, c * 128:(c + 1) * 128], ident_f[0:16, 0:16], start=True, stop=True),
                                 reads=[K("maskT"), K("ident_f")], writes=[K("pb", 0)])
                        stage(44)
                        P.op("act", Q.activation(out=mask_f[:].rearrange("p a b -> p (a b)"), in_=PB[0][:, 0:256], func=AF.Copy),
                             reads=[K("pb", 0)], writes=[K("mask_f")])
                        stage(45)
                        P.op("dve", Q.tensor_copy(out=mask_b[:], in_=mask_f[:]),
                             reads=[K("mask_f")], writes=[K("mask_b")])
                        stage(46)
                    P.barrier()
                    stage(4 if l == 0 else 40)
                    for c in range(NCH):
                        for c2 in range(c + 1):
                            lt = ltri_b if c2 == c else ones_b
                            P.op("pe", Q.matmul(PB[1][:, c * 16:(c + 1) * 16], lt[:], mask_b[:, c2, :], start=(c2 == 0), stop=(c2 == c)),
                                 reads=[K("ltri_b"), K("ones_b"), K("mask_b")], writes=[K("pb", 1)])
                    P.op("act", Q.activation(out=pos_tm[:].rearrange("p a b -> p (a b)"), in_=PB[1][:, 0:256], func=AF.Copy),
                         reads=[K("pb", 1)], writes=[K("pos_tm")])
                    P.op("dve", Q.reduce_sum(out=hi_f[:, :, 0], in_=aff_tm[:], axis=AX.X), reads=[K("aff_tm")], writes=[K("hi_f")])
                    P.op("dve", Q.reciprocal(out=hi_f[:, :, 1], in_=hi_f[:, :, 0]), reads=[K("hi_f")], writes=[K("hi_f")])
                    for c in range(NCH):
                        P.op("dve", Q.tensor_scalar(out=aff_tm[:, c, :], in0=aff_tm[:, c, :], scalar1=hi_f[:, c, 1:2], scalar2=None, op0=ALU.mult),
                             reads=[K("aff_tm"), K("hi_f")], writes=[K("aff_tm")])
                    P.op("dve", Q.tensor_copy(out=R[:, :, :, 0], in_=aff_tm[:]), reads=[K("aff_tm")], writes=[K("R")])
                    P.op("dve", Q.tensor_copy(out=hi_f[:], in_=R[:, :, :, 0]), reads=[K("R"), K("aff_tm")], writes=[K("hi_f")])
                    P.op("dve", Q.tensor_tensor(out=R[:, :, :, 1], in0=aff_tm[:], in1=hi_f[:], op=ALU.subtract),
                         reads=[K("aff_tm"), K("hi_f")], writes=[K("R")])
                    for q in range(2):
                        P.op("dve", Q.tensor_copy(out=R[:, :, :, 2 + q], in_=cidx[:, :, q:q + 1].to_broadcast([128, NCH, 16])),
                             reads=[K("cidx")], writes=[K("R")])

                    Se = pa("Se", [128, NCH, CAP], BF16)
                    SeT2 = [pa(f"SeT{k}", [128, 2, SEQ], BF16) for k in range(2)]
                    gi2 = [pa(f"gi{k}", [128, 2, 4], F32) for k in range(2)]
                    xinT = pa("xinT", [128, 8, CAP], BF16)
                    hidT = pa("hidT", [128, NFC, CAP], BF16)
                    sgt = pa("sgt", [128, CAP], F32)
                    yg2 = [pa(f"yg{k}", [128, 2, D], BF16) for k in range(2)]
                    w13 = [pa(f"w13_{k}", [128, 2, 8, 256], BF16) for k in range(3)]
                    w2b = [pa(f"w2_{k}", [128, 2, D], BF16) for k in range(3)]
                    wcount = [0, 0]

                    def emit_scatter(pb_, c_lo, c_hi):
                        for c in range(c_lo, c_hi):
                            for h in range(2):
                                bk = (c * 2 + h) % 2
                                for jc in range(2):
                                    P.op("pe", Q.matmul(
                                        PB[bk][:], SeT2[pb_][:, jc, c * 128:(c + 1) * 128], yg2[pb_][:, jc, h * 512:(h + 1) * 512],
                                        start=(jc == 0), stop=(jc == 1)),
                                        reads=[K("SeT", pb_, jc), K("yg", pb_)], writes=[K("pb", bk)])
                                P.op("dve", Q.tensor_tensor(
                                    out=x[:, c, h * 512:(h + 1) * 512], in0=PB[bk][:], in1=x[:, c, h * 512:(h + 1) * 512], op=ALU.add),
                                    reads=[K("pb", bk), K("x", c)], writes=[K("x", c)])

                    for ex in range(E):
                        eb_ = ex % 2
                        SeT, gi, yg = SeT2[eb_], gi2[eb_], yg2[eb_]
                        for c in range(NCH):
                            P.op("dve", Q.tensor_scalar(out=Se[:, c, :], in0=iota_t[:, 0:CAP], scalar1=pos_tm[:, c, ex:ex + 1],
                                                                              scalar2=mask_f[:, c, ex:ex + 1], op0=ALU.is_equal, op1=ALU.mult),
                                 reads=[K("iota_t"), K("pos_tm"), K("mask_f")], writes=[K("Se", c)])
                        for jc in range(2):
                            for c in range(NCH):
                                P.op("pe", Q.matmul(PB[0][:, jc * 4:(jc + 1) * 4], Se[:, c, jc * 128:(jc + 1) * 128], R[:, c, ex, :],
                                                                                start=(c == 0), stop=(c == NCH - 1)),
                                     reads=[K("Se", c), K("R")], writes=[K("pb", 0)])
                        P.op("act", Q.activation(out=gi[:].rearrange("p a b -> p (a b)"), in_=PB[0][:, 0:8], func=AF.Copy),
                             reads=[K("pb", 0)], writes=[K("gi", eb_)])
                        P.op("dve", Q.tensor_tensor(out=gi[:, :, 0], in0=gi[:, :, 0], in1=gi[:, :, 1], op=ALU.add), reads=[K("gi", eb_)], writes=[K("gi", eb_)])
                        P.op("dve", Q.scalar_tensor_tensor(out=gi[:, :, 2], in0=gi[:, :, 2], scalar=128.0, in1=gi[:, :, 3], op0=ALU.mult, op1=ALU.add),
                             reads=[K("gi", eb_)], writes=[K("gi", eb_)])
                        for jc in range(2):
                            P.op("dve", Q.tensor_scalar(out=SeT[:, jc, :], in0=iota_t[:], scalar1=gi[:, jc, 2:3], scalar2=None, op0=ALU.is_equal),
                                 reads=[K("iota_t"), K("gi", eb_)], writes=[K("SeT", eb_, jc)])
                        for dc in range(8):
                            bk = dc % 2
                            for c in range(NCH):
                                P.op("pe", Q.matmul(PB[bk][:, 0:CAP], xs2[:, c, dc * 128:(dc + 1) * 128], Se[:, c, :],
                                                                                start=(c == 0), stop=(c == NCH - 1)),
                                     reads=[K("xs2", c), K("Se", c)], writes=[K("pb", bk)])
                            P.op("act", Q.activation(out=xinT[:, dc, :], in_=PB[bk][:, 0:CAP], func=AF.Identity,
                                                                             scale=AB[:, 0, dc:dc + 1], bias=AB[:, 1, dc:dc + 1]),
                                 reads=[K("pb", bk), K("AB")], writes=[K("xinT")])
                        for fg in range(NFC // 2):
                            slot = wcount[0] % len(w13)
                            wcount[0] += 1
                            wt = w13[slot]
                            for wi, nm in enumerate(("w1", "w3")):
                                src = L[l][nm][ex][:, fg * 256:(fg + 1) * 256].rearrange("(kc p) n -> p kc n", p=128)
                                P.op("pool", Q.dma_start(out=wt[:, wi], in_=src), writes=[K("w13", slot, wi)], dma=True)
                            for f2 in range(2):
                                fc = fg * 2 + f2
                                bk = 2 + (fc % 2)
                                for wi in range(2):
                                    for kc in range(8):
                                        P.op("pe", Q.matmul(
                                            PB[bk][:, wi * CAP:(wi + 1) * CAP], wt[:, wi, kc, f2 * 128:(f2 + 1) * 128], xinT[:, kc, :],
                                            start=(kc == 0), stop=(kc == 7)),
                                            reads=[K("w13", slot, wi), K("xinT")], writes=[K("pb", bk)])
                                P.op("act", Q.activation(out=sgt[:], in_=PB[bk][:, 0:CAP], func=AF.Sigmoid), reads=[K("pb", bk)], writes=[K("sgt")])
                                P.op("dve", Q.tensor_tensor(out=sgt[:], in0=PB[bk][:, 0:CAP], in1=sgt[:], op=ALU.mult),
                                     reads=[K("pb", bk), K("sgt")], writes=[K("sgt")])
                                P.op("dve", Q.tensor_tensor(out=hidT[:, fc, :], in0=PB[bk][:, CAP:2 * CAP], in1=sgt[:], op=ALU.mult),
                                     reads=[K("pb", bk), K("sgt")], writes=[K("hidT")])
                            if ex > 0:
                                emit_scatter(1 - eb_, fg * NCH // (NFC // 2), (fg + 1) * NCH // (NFC // 2))
                        for fg in range(NFC // 2):
                            slot = wcount[1] % len(w2b)
                            wcount[1] += 1
                            wt = w2b[slot]
                            src = L[l]["w2"][ex][fg * 256:(fg + 1) * 256, :].rearrange("(a p) n -> p a n", p=128)
                            P.op("pool", Q.dma_start(out=wt[:], in_=src), writes=[K("w2b", slot)], dma=True)
                            for f2 in range(2):
                                fc = fg * 2 + f2
                                for jc in range(2):
                                    for h in range(2):
                                        bk = 4 + jc * 2 + h
                                        P.op("pe", Q.matmul(
                                            PB[bk][:], hidT[:, fc, jc * 128:(jc + 1) * 128], wt[:, f2, h * 512:(h + 1) * 512],
                                            start=(fc == 0), stop=(fc == NFC - 1)),
                                            reads=[K("hidT"), K("w2b", slot)], writes=[K("pb", bk)])
                        for jc in range(2):
                            for h in range(2):
                                bk = 4 + jc * 2 + h
                                P.op("dve", Q.scalar_tensor_tensor(
                                    out=yg[:, jc, h * 512:(h + 1) * 512], in0=PB[bk][:], scalar=gi[:, jc, 0:1], in1=gate_b[:, h * 512:(h + 1) * 512],
                                    op0=ALU.mult, op1=ALU.mult), reads=[K("pb", bk), K("gi", eb_), K("gate_b")], writes=[K("yg", eb_)])
                    emit_scatter((E - 1) % 2, 0, NCH)
                P.barrier()
                tap(f"moe{l}", s)
                stage(5 if l == 0 else 7)

            with Phase() as ph:
                ob = [ph.enter_context(nc.sbuf_tensor(un(f"ob{k}"), [128, D], F32)) for k in range(2)]
                fnorm_b = ph.enter_context(nc.sbuf_tensor(un("fnorm_b"), [128, D], F32))
                P.op("sp", Q.dma_start(out=fnorm_b[:], in_=fnorm_d.partition_broadcast(128)), writes=[K("fnorm_b")], dma=True)
                for c in range(NCH):
                    o = ob[c % 2]
                    ok = K("ob", c % 2)
                    sl = c % 16
                    sq, rs = stat[:, 2 * sl:2 * sl + 1], stat[:, 2 * sl + 1:2 * sl + 2]
                    sk = K("stat", sl)
                    P.op("act", Q.activation(out=o[:], in_=x[:, c, :], func=AF.Square),
                         reads=[K("x", c)], writes=[ok])
                    P.op("dve", Q.reduce_sum(out=sq, in_=o[:], axis=AX.X), reads=[ok], writes=[sk])
                    P.op("dve", Q.tensor_scalar(out=rs, in0=sq, scalar1=1.0 / D, scalar2=EPS, op0=ALU.mult, op1=ALU.add),
                         reads=[sk], writes=[sk])
                    P.op("act", Q.activation(out=rs, in_=rs, func=AF.Sqrt), reads=[sk], writes=[sk])
                    P.op("dve", Q.reciprocal(out=rs, in_=rs), reads=[sk], writes=[sk])
                    P.op("dve", Q.scalar_tensor_tensor(out=o[:], in0=x[:, c, :], scalar=rs, in1=fnorm_b[:], op0=ALU.mult, op1=ALU.mult),
                         reads=[K("x", c), sk, K("fnorm_b")], writes=[ok])
                    P.op("sp", Q.dma_start(out=out_d[s, c * 128:(c + 1) * 128, :], in_=o[:]), reads=[ok], dma=True)
            P.barrier()
    except _Stop:
        P.barrier()
        P.op("sp", Q.dma_start(out=out_d[0].rearrange("(c p) d -> p c d", p=128), in_=x[:]), reads=[K("x", c) for c in range(NCH)], dma=True)
        P.barrier()

    P.emit(st)
    st.close()
    return nc


def _col(v):
    return np.ascontiguousarray(v.reshape(-1, 128).T)


def prep_shared(inp, E):
    f = np.float32
    sh = {}
    perm = _rope_perm()
    for l in range(2):
        p = f"l{l}_"
        sh[p + "normcols"] = np.ascontiguousarray(np.stack([_col(inp[p + "norm1"]), _col(inp[p + "norm2"])], 1)).astype(f)
        sh[p + "w_mod"] = np.ascontiguousarray(inp[p + "w_mod"])
        sh[p + "b_mod"] = np.ascontiguousarray(inp[p + "b_mod"].reshape(1, -1))
        sh[p + "w_out"] = np.ascontiguousarray(inp[p + "w_out"])
        sh[p + "w_router"] = np.ascontiguousarray(inp[p + "w_router"])
        sh[p + "w1"] = np.ascontiguousarray(inp[p + "w1"][:E])
        sh[p + "w3"] = np.ascontiguousarray(inp[p + "w3"][:E])
        sh[p + "w2"] = np.ascontiguousarray(inp[p + "w2"][:E])
    w = inp["l0_w_in"]
    aq, bq, ak, bk, av, bv = (w[:, i * 512:(i + 1) * 512] for i in range(6))
    na = [np.concatenate([aq[:, i * 128:(i + 1) * 128], ak[:, i * 128:(i + 1) * 128], av[:, i * 128:(i + 1) * 128]], 1) for i in range(4)]
    sh["l0_w_na"] = np.ascontiguousarray(np.stack(na, 0))
    pidx = np.concatenate([perm, 64 + perm])
    df = []
    for h in range(4):
        q = bq[:, h * 128:(h + 1) * 128]
        k = bk[:, h * 128:(h + 1) * 128]
        df.append(np.concatenate([q, q[:, pidx], k, k[:, pidx], bv[:, h * 128:(h + 1) * 128]], 1))
    sh["l0_w_df"] = np.ascontiguousarray(np.stack(df, 0))
    sh["l0_na_bias"] = np.ascontiguousarray(_na_bias_tables(inp["l0_rpb"]).reshape(8, 128, 14 * 256))
    sh["l0_lam"] = np.ascontiguousarray(np.concatenate([inp["l0_lam_q1"], inp["l0_lam_k1"], inp["l0_lam_q2"], inp["l0_lam_k2"]], 0).reshape(1, 256))
    sh["l0_subln"] = np.ascontiguousarray(inp["l0_subln"].reshape(1, 128))
    w = inp["l1_w_in"]
    xc, gb, gc, ga, gg = (w[:, i * 512:(i + 1) * 512] for i in range(5))
    sh["l1_w_c"] = np.ascontiguousarray(np.stack([np.concatenate([xc[:, j * 128:(j + 1) * 128], gb[:, j * 128:(j + 1) * 128], gc[:, j * 128:(j + 1) * 128]], 1) for j in range(4)], 0))
    sh["l1_w_d"] = np.ascontiguousarray(np.stack([np.concatenate([ga[:, j * 128:(j + 1) * 128], gg[:, j * 128:(j + 1) * 128]], 1) for j in range(4)], 0))
    sh["l1_sc_w"] = np.ascontiguousarray(inp["l1_sc_w"].T.reshape(4, 128, 3).transpose(1, 0, 2))
    sh["l1_cf_w"] = np.ascontiguousarray(inp["l1_cf_w"].T.reshape(4, 128, 31).transpose(1, 0, 2))
    sh["l1_cf_vec"] = np.ascontiguousarray(np.stack([_col(inp["l1_cf_b"]), _col(inp["l1_ln_g"]), _col(inp["l1_ln_b"])], 1))
    sh["final_norm"] = np.ascontiguousarray(inp["final_norm"].reshape(1, -1))
    cos, sin = _rope_tables()
    sh["rope"] = np.ascontiguousarray(np.stack([cos, sin], 0))
    import ml_dtypes
    ci = np.zeros((128, 16, 2), np.float32)
    ci[:, :, 0] = np.arange(16)[None, :]
    ci[:, :, 1] = np.arange(128)[:, None]
    sh["cidx"] = ci.astype(ml_dtypes.bfloat16)
    sh["ident"] = np.eye(128, dtype=f)
    sh["ltri"] = np.triu(np.ones((128, 128), f), 1)
    sh["iota"] = np.arange(SEQ, dtype=f).reshape(1, SEQ)
    return sh


def core_inputs(inp, sh, samples):
    S = len(samples)
    m = dict(sh)
    m["x"] = np.ascontiguousarray(inp["x"][samples])
    m["ctx"] = np.ascontiguousarray(inp["ctx"][samples])
    cc = np.concatenate([inp["c"][samples], inp["c_ctx"][None, :]], 0)
    m["cT"] = np.ascontiguousarray(cc.reshape(S + 1, 8, 128).transpose(2, 1, 0))
    return m


NCORES = 8
_cache = {}


def kernel(**inputs):
    inp = {k: np.asarray(v) for k, v in inputs.items()}
    B = inp["x"].shape[0]
    S = B // NCORES
    E = 16
    key = (S, E)
    if key not in _cache:
        _cache[key] = build(S, E)
    nc = _cache[key]
    sh = prep_shared(inp, E)
    in_maps = [core_inputs(inp, sh, list(range(c * S, (c + 1) * S))) for c in range(NCORES)]
    res = run_bass_kernel_spmd(nc, in_maps, core_ids=list(range(NCORES)))
    out = np.concatenate([res.results[c]["out"] for c in range(NCORES)], 0)
    return out.astype(np.float32)
```

```python
import math
from contextlib import ExitStack
import numpy as np
import concourse.bass as bass
import concourse.mybir as mybir
from concourse.bass_utils import run_bass_kernel_spmd

F32 = mybir.dt.float32
BF16 = mybir.dt.bfloat16
AF = mybir.ActivationFunctionType
ALU = mybir.AluOpType
AX = mybir.AxisListType

D = 1024
SEQ = 2048
NCH = 16
CTX = 256
DE = 2816
NFC = 22
CAP = 256
EPS = 1e-6
GRID_W = 64
NEG = -30000.0


class _Rec:
    def __getattr__(self, name):
        def mk(*a, **k):
            return lambda e: getattr(e, name)(*a, **k)
        return mk


Q = _Rec()


class Prog:
    ENGS = ("pe", "act", "dve", "pool", "sp")
    INORDER = ("pe", "act", "dve")

    def __init__(self, nc):
        self.nc = nc
        self.ops = {e: [] for e in self.ENGS}
        self.last_w = {}
        self.readers = {}
        self.dma_since_barrier = []

    def op(self, eng, fn, reads=(), writes=(), dma=False):
        idx = len(self.ops[eng])
        deps = set()
        for k in reads:
            w = self.last_w.get(k)
            if w is not None:
                deps.add(w)
        for k in writes:
            w = self.last_w.get(k)
            if w is not None:
                deps.add(w)
            for r in self.readers.get(k, ()):
                deps.add(r)
        if eng == "pe":
            deps = {d for d in deps if not (d[0] == "pe" and not self.ops["pe"][d[1]]["dma"])}
        self.ops[eng].append(dict(fn=fn, deps=deps, inc=False, dma=dma))
        me = (eng, idx)
        for k in reads:
            lst = self.readers.setdefault(k, [])
            if not dma and eng in self.INORDER:
                lst[:] = [r for r in lst if not (r[0] == eng and not self.ops[eng][r[1]]["dma"])]
            lst.append(me)
        for k in writes:
            self.last_w[k] = me
            self.readers[k] = []
        if dma:
            self.dma_since_barrier.append(me)
        return me

    def barrier(self):
        targets = set(self.dma_since_barrier)
        for e in self.ENGS:
            for i in range(len(self.ops[e]) - 1, -1, -1):
                if not self.ops[e][i]["dma"] and self.ops[e][i]["fn"] is not None:
                    targets.add((e, i))
                    break
        for e in self.ENGS:
            self.ops[e].append(dict(fn=None, deps=set(targets), inc=False, dma=False))
        self.dma_since_barrier = []

    def emit(self, stack):
        nc = self.nc
        for e in self.ENGS:
            for o in self.ops[e]:
                for (de, di) in o["deps"]:
                    t = self.ops[de][di]
                    if not t["dma"]:
                        t["inc"] = True
        EPOCH = 30000
        ev = {}
        for e in self.ENGS:
            cnt = 0
            sem = None
            for i, o in enumerate(self.ops[e]):
                if o["dma"] or not o["inc"]:
                    continue
                if sem is None or cnt >= EPOCH:
                    sem = stack.enter_context(nc.semaphore(f"s_{e}_{i}"))
                    cnt = 0
                cnt += 1
                ev[(e, i)] = (sem, cnt)
                o["sem"] = sem
        NDS = 12
        for e in self.ENGS:
            if not any(o["dma"] for o in self.ops[e]):
                continue
            pool = [stack.enter_context(nc.semaphore(f"d_{e}_{k}")) for k in range(NDS)]
            vals = [0] * NDS
            prev = [None] * NDS
            k = 0
            for i, o in enumerate(self.ops[e]):
                if not o["dma"]:
                    continue
                slot = k % NDS
                k += 1
                vals[slot] += 16
                ev[(e, i)] = (pool[slot], vals[slot])
                o["sem"] = pool[slot]
                if prev[slot] is not None:
                    o["deps"].add(prev[slot])
                prev[slot] = (e, i)
        engobj = dict(pe=nc.tensor, act=nc.scalar, dve=nc.vector, pool=nc.gpsimd, sp=nc.sync)
        ops = self.ops

        def run(ename):
            def body(eng):
                waited = {}
                for o in ops[ename]:
                    for d in sorted(o["deps"]):
                        sem, val = ev[d]
                        key = id(sem)
                        if waited.get(key, 0) >= val:
                            continue
                        waited[key] = val
                        eng.wait_ge(sem, val)
                    if o["fn"] is None:
                        continue
                    ins = o["fn"](eng)
                    if o["dma"]:
                        ins.then_inc(o["sem"], 16)
                    elif o["inc"]:
                        ins.then_inc(o["sem"], 1)
            return body

        with nc.Block() as block:
            block.tensor(run("pe"))
            block.scalar(run("act"))
            block.vector(run("dve"))
            block.gpsimd(run("pool"))
            block.sync(run("sp"))


def _rope_tables():
    n_freq = 16
    freqs = (np.float32(10000.0) ** (-np.arange(n_freq, dtype=np.float32) / n_freq)).astype(np.float32)
    t = np.arange(SEQ)
    row = (t // GRID_W).astype(np.float32)
    col = (t % GRID_W).astype(np.float32)
    cos = np.zeros((64, SEQ), np.float32)
    sin = np.zeros((64, SEQ), np.float32)
    for f in range(64):
        pos = row if f < 32 else col
        i = f % 32
        ang = (pos * freqs[i % 16]).astype(np.float32)
        cos[f] = np.cos(ang)
        sin[f] = np.sin(ang) * (-1.0 if i < 16 else 1.0)
    return np.concatenate([cos, cos], 0), np.concatenate([sin, sin], 0)


def _rope_perm():
    p = np.zeros(64, np.int64)
    for f in range(64):
        base = 0 if f < 32 else 32
        i = f % 32
        p[f] = base + (i + 16 if i < 16 else i - 16)
    return p


def _na_blocks():
    out = []
    for b in range(8):
        if b == 0:
            out.append(list(range(0, 4)))
        elif b == 7:
            out.append(list(range(12, 16)))
        else:
            out.append(list(range(2 * b - 2, 2 * b + 4)))
    return out


def _na_class(b):
    return 0 if b == 0 else (2 if b == 7 else 1)


NA_CLS_OFF = [0, 4, 10]
NA_CLS_B = [0, 1, 7]


def _na_bias_tables(rpb):
    H = rpb.shape[0]
    tab = np.full((H, 128, 14, 256), NEG, np.float32)
    blocks = _na_blocks()
    kp = np.arange(128)
    kr_l = kp // 64
    kc = kp % 64
    qq = np.arange(256)
    qr_l = qq // 64
    qc = qq % 64
    cs = np.clip(qc - 8, 0, 48)
    for cls in range(3):
        b = NA_CLS_B[cls]
        for si, c in enumerate(blocks[b]):
            kr = 2 * c + kr_l
            r = 4 * b + qr_l
            rs = np.clip(r - 4, 0, 24)
            inrow = (kr[:, None] >= rs[None, :]) & (kr[:, None] <= rs[None, :] + 7)
            incol = (kc[:, None] >= cs[None, :]) & (kc[:, None] < cs[None, :] + 16)
            ok = inrow & incol
            dr = np.clip(kr[:, None] - r[None, :] + 7, 0, 14)
            dc = np.clip(kc[:, None] - qc[None, :] + 15, 0, 30)
            g = rpb[:, dr, dc]
            tab[:, :, NA_CLS_OFF[cls] + si, :] = np.where(ok[None], g, np.float32(NEG))
    return tab


class _Stop(Exception):
    pass


class Phase(ExitStack):
    def __exit__(self, et, ev, tb):
        super().__exit__(None, None, None)
        return False


def build(S, E, taps=(), stop=None):
    nc = bass.Bass("TRN2", target_bir_lowering=False)
    P = Prog(nc)
    st = ExitStack()

    def din(name, shape, dt=F32):
        return nc.dram_tensor(name, list(shape), dt, kind="ExternalInput").ap()

    x_d = din("x", [S, SEQ, D])
    c_d = din("cT", [128, 8, S + 1])
    ctx_d = din("ctx", [S, CTX, D])
    out_d = nc.dram_tensor("out", [S, SEQ, D], F32, kind="ExternalOutput").ap()
    tap_d = {t: nc.dram_tensor(f"tap_{t}", [SEQ, D], F32, kind="ExternalOutput").ap() for t in taps}
    L = []
    for l in range(2):
        p = {}
        p["normcols"] = din(f"l{l}_normcols", [128, 2, 8])
        p["w_mod"] = din(f"l{l}_w_mod", [D, 6 * D])
        p["b_mod"] = din(f"l{l}_b_mod", [1, 6 * D])
        p["w_out"] = din(f"l{l}_w_out", [D, D])
        p["w_router"] = din(f"l{l}_w_router", [D, 16])
        p["w1"] = din(f"l{l}_w1", [E, D, DE])
        p["w3"] = din(f"l{l}_w3", [E, D, DE])
        p["w2"] = din(f"l{l}_w2", [E, DE, D])
        if l == 0:
            p["w_na"] = din("l0_w_na", [4, D, 384])
            p["w_df"] = din("l0_w_df", [4, D, 640])
            p["na_bias"] = din("l0_na_bias", [8, 128, 14 * 256])
            p["lam"] = din("l0_lam", [1, 256])
            p["subln"] = din("l0_subln", [1, 128])
        else:
            p["w_c"] = din("l1_w_c", [4, D, 384])
            p["w_d"] = din("l1_w_d", [4, D, 256])
            p["sc_w"] = din("l1_sc_w", [128, 4, 3])
            p["cf_w"] = din("l1_cf_w", [128, 4, 31])
            p["cf_vec"] = din("l1_cf_vec", [128, 3, 4])
        L.append(p)
    fnorm_d = din("final_norm", [1, D])
    rope_d = din("rope", [2, 128, SEQ])
    cidx_d = din("cidx", [128, 16, 2], BF16)
    ident_d = din("ident", [128, 128])
    ltri_d = din("ltri", [128, 128])
    iota_d = din("iota", [1, SEQ])

    uid = [0]

    def un(name):
        uid[0] += 1
        return f"t{uid[0]}_{name}"

    def sb(name, shape, dt):
        return st.enter_context(nc.sbuf_tensor(un(name), list(shape), dt))

    def ps(name, shape, dt=F32):
        return st.enter_context(nc.psum_tensor(name, list(shape), dt))

    x = sb("x", [128, NCH, D], F32)
    ident_f = sb("ident_f", [128, 128], F32)
    ident_b = sb("ident_b", [128, 128], BF16)
    ones_f = sb("ones_f", [128, 128], F32)
    ltri_b = sb("ltri_b", [128, 128], BF16)
    ones_b = sb("ones_b", [128, 128], BF16)
    iota_t = sb("iota_t", [128, SEQ], F32)
    cidx = sb("cidx_sb", [128, 16, 2], BF16)
    modc = [sb(f"modc{l}", [128, 48, S + 1], F32) for l in range(2)]
    normc = [sb(f"normc{l}", [128, 2, 8], F32) for l in range(2)]
    siluc = sb("siluc", [128, 8, S + 1], BF16)
    AB = sb("AB", [128, 2, 8], F32)
    gate_b = sb("gate_b", [128, D], F32)
    small = sb("small", [128, 64], F32)
    stat = sb("stat", [128, 32], F32)
    junk = sb("junk", [128, D], F32)

    PB = [ps(f"pb{k}", [128, 512], F32) for k in range(8)]
    PBb = [t[:].bitcast(BF16) for t in PB]

    def K(*a):
        return a

    def stage(n):
        if stop is not None and n == stop:
            raise _Stop()

    P.op("sp", Q.dma_start(out=ident_f[:], in_=ident_d), writes=[K("ident_f")], dma=True)
    P.op("pool", Q.dma_start(out=ident_b[:], in_=ident_d), writes=[K("ident_b")], dma=True)
    P.op("pool", Q.dma_start(out=ltri_b[:], in_=ltri_d), writes=[K("ltri_b")], dma=True)
    P.op("sp", Q.dma_start(out=iota_t[:], in_=iota_d.partition_broadcast(128)), writes=[K("iota_t")], dma=True)
    P.op("sp", Q.dma_start(out=cidx[:], in_=cidx_d), writes=[K("cidx")], dma=True)
    P.op("dve", Q.memset(ones_f[:], 1.0), writes=[K("ones_f")])
    P.op("dve", Q.memset(ones_b[:], 1.0), writes=[K("ones_b")])
    for l in range(2):
        P.op("sp", Q.dma_start(out=normc[l][:], in_=L[l]["normcols"]), writes=[K("normc", l)], dma=True)

    with Phase() as ph:
        cT = ph.enter_context(nc.sbuf_tensor(un("cT_sb"), [128, 8, S + 1], F32))
        sg = ph.enter_context(nc.sbuf_tensor(un("sg_sb"), [128, 8, S + 1], F32))
        bm = ph.enter_context(nc.sbuf_tensor(un("bm_sb"), [1, 6 * D], BF16))
        wm = [ph.enter_context(nc.sbuf_tensor(un(f"wm{k}"), [128, 8, 512], BF16)) for k in range(2)]
        P.op("sp", Q.dma_start(out=cT[:], in_=c_d), writes=[K("cT")], dma=True)
        P.op("act", Q.activation(out=sg[:], in_=cT[:], func=AF.Sigmoid), reads=[K("cT")], writes=[K("sg")])
        P.op("dve", Q.tensor_tensor(out=siluc[:], in0=cT[:], in1=sg[:], op=ALU.mult),
             reads=[K("cT"), K("sg")], writes=[K("siluc")])
        blk = 0
        for l in range(2):
            P.op("pool", Q.dma_start(out=bm[:], in_=L[l]["b_mod"]), writes=[K("bm")], dma=True)
            for cb in range(12):
                w = wm[blk % 2]
                wk = K("wm", blk % 2)
                blk += 1
                src = L[l]["w_mod"][:, cb * 512:(cb + 1) * 512].rearrange("(kc p) n -> p kc n", p=128)
                P.op("pool", Q.dma_start(out=w[:], in_=src), writes=[wk], dma=True)
                pt = PB[cb % 2]
                pk = K("pb", cb % 2)
                for fcl in range(4):
                    o = pt[:, fcl * (S + 1):(fcl + 1) * (S + 1)]
                    for kc in range(8):
                        P.op("pe", Q.matmul(
                            o, w[:, kc, fcl * 128:(fcl + 1) * 128], siluc[:, kc, :], start=(kc == 0), stop=False),
                            reads=[wk, K("siluc")], writes=[pk])
                    f0 = cb * 512 + fcl * 128
                    P.op("pe", Q.matmul(
                        o, bm[0:1, f0:f0 + 128], ones_b[0:1, 0:S + 1], start=False, stop=True),
                        reads=[K("bm"), K("ones_b")], writes=[pk])
                P.op("dve", Q.tensor_copy(
                    out=modc[l][:, cb * 4:(cb + 1) * 4, :],
                    in_=pt[:, 0:4 * (S + 1)].rearrange("p (a b) -> p a b", b=S + 1)),
                    reads=[pk], writes=[K("modc", l)])
    P.barrier()

    def make_AB(l, which, col, nkey):
        sh = 0 if which == 0 else 24
        P.op("dve", Q.scalar_tensor_tensor(
            out=AB[:, 0, :], in0=modc[l][:, sh + 8:sh + 16, col], scalar=1.0, in1=normc[l][:, nkey, :],
            op0=ALU.add, op1=ALU.mult), reads=[K("modc", l), K("normc", l)], writes=[K("AB")])
        P.op("dve", Q.tensor_copy(out=AB[:, 1, :], in_=modc[l][:, sh:sh + 8, col]),
             reads=[K("modc", l)], writes=[K("AB")])

    def make_gate(l, which, col):
        g0 = 16 if which == 0 else 40
        with Phase() as ph:
            dg = ph.enter_context(nc.sbuf_tensor(un("dg"), [128, 8, 128], F32))
            for kc in range(8):
                P.op("dve", Q.tensor_scalar(
                    out=dg[:, kc, :], in0=ident_f[:], scalar1=modc[l][:, g0 + kc, col:col + 1], scalar2=None,
                    op0=ALU.mult), reads=[K("ident_f"), K("modc", l)], writes=[K("dg", kc)])
            for h in range(2):
                for k4 in range(4):
                    kc = h * 4 + k4
                    P.op("pe", Q.matmul(
                        PB[h][:, k4 * 128:(k4 + 1) * 128], ones_f[:], dg[:, kc, :], start=True, stop=True),
                        reads=[K("ones_f"), K("dg", kc)], writes=[K("pb", h)])
                P.op("act", Q.activation(out=gate_b[:, h * 512:(h + 1) * 512], in_=PB[h][:], func=AF.Copy),
                     reads=[K("pb", h)], writes=[K("gate_b")])
        P.barrier()

    def rms_xs(src, c, xs_out, keys_r, keys_w, width=D):
        sl = c % 16
        sq = stat[:, 2 * sl:2 * sl + 1]
        rs = stat[:, 2 * sl + 1:2 * sl + 2]
        sk = K("stat", sl)
        P.op("act", Q.activation(out=junk[:, 0:width], in_=src, func=AF.Square),
             reads=keys_r, writes=[K("junk")])
        P.op("dve", Q.reduce_sum(out=sq, in_=junk[:, 0:width], axis=AX.X),
             reads=[K("junk")], writes=[sk])
        P.op("dve", Q.tensor_scalar(out=rs, in0=sq, scalar1=1.0 / width, scalar2=EPS, op0=ALU.mult, op1=ALU.add),
             reads=[sk], writes=[sk])
        P.op("act", Q.activation(out=rs, in_=rs, func=AF.Sqrt), reads=[sk], writes=[sk])
        P.op("dve", Q.reciprocal(out=rs, in_=rs), reads=[sk], writes=[sk])
        P.op("dve", Q.tensor_scalar(out=xs_out, in0=src, scalar1=rs, scalar2=None, op0=ALU.mult),
             reads=keys_r + [sk], writes=keys_w)

    def norm_T(hT, hT_key, ntok_chunks, src_chunk, src_keys, xsg, group_cb=None, xs_keep=None):
        ng = ntok_chunks // 4
        for g in range(ng):
            for j in range(4):
                c = g * 4 + j
                if xs_keep is not None:
                    xo = xs_keep[:, c, :]
                    kw = [K("xs2", c)]
                else:
                    xo = xsg[:, j, :]
                    kw = [K("xsg", j)]
                rms_xs(src_chunk(c), c, xo, src_keys(c), kw)
            for dc in range(8):
                pt = PBb[dc % 2]
                pk = K("pb", dc % 2)
                for j in range(4):
                    c = g * 4 + j
                    if xs_keep is not None:
                        xi = xs_keep[:, c, dc * 128:(dc + 1) * 128]
                        kr = [K("xs2", c)]
                    else:
                        xi = xsg[:, j, dc * 128:(dc + 1) * 128]
                        kr = [K("xsg", j)]
                    P.op("pe", Q.transpose(pt[:, j * 128:(j + 1) * 128], xi, ident_b[:]),
                         reads=kr + [K("ident_b")], writes=[pk])
                P.op("act", Q.activation(
                    out=hT[:, dc, g * 512:(g + 1) * 512], in_=pt[:, 0:512], func=AF.Identity,
                    scale=AB[:, 0, dc:dc + 1], bias=AB[:, 1, dc:dc + 1]),
                    reads=[pk, K("AB")], writes=[K(hT_key, g)])
            if group_cb is not None:
                group_cb(g)

    def out_proj_unit(l, u, yT, yT_key, wo):
        P.op("pool", Q.dma_start(out=wo[0][:], in_=L[l]["w_out"][u * 128:(u + 1) * 128, :]),
             writes=[K("wo_f")], dma=True)
        P.op("dve", Q.tensor_tensor(out=wo[1][:], in0=wo[0][:], in1=gate_b[:], op=ALU.mult),
             reads=[K("wo_f"), K("gate_b")], writes=[K("wo_b")])
        for c in range(NCH):
            for h in range(2):
                bk = 6 + h
                P.op("pe", Q.matmul(
                    PB[bk][:], yT[:, c * 128:(c + 1) * 128], wo[1][:, h * 512:(h + 1) * 512], start=True, stop=True),
                    reads=[K(yT_key), K("wo_b")], writes=[K("pb", bk)])
                P.op("dve", Q.tensor_tensor(
                    out=x[:, c, h * 512:(h + 1) * 512], in0=PB[bk][:], in1=x[:, c, h * 512:(h + 1) * 512], op=ALU.add),
                    reads=[K("pb", bk), K("x", c)], writes=[K("x", c)])

    def proj_T(dst, dst_keys, w, wkey, col0, hT, hT_key, ntok, evac="act", tok0=0, dst0=0, pbs=(0, 1)):
        nb = (ntok + 511) // 512
        for b in range(nb):
            n = min(512, ntok - b * 512)
            pt = PB[pbs[b % len(pbs)]]
            pk = K("pb", pbs[b % len(pbs)])
            for kc in range(8):
                P.op("pe", Q.matmul(
                    pt[:, 0:n], w[:, kc, col0:col0 + 128], hT[:, kc, tok0 + b * 512: tok0 + b * 512 + n],
                    start=(kc == 0), stop=(kc == 7)), reads=[wkey, K(hT_key, (tok0 + b * 512) // 512)], writes=[pk])
            o = dst[:, dst0 + b * 512: dst0 + b * 512 + n]
            if evac == "act":
                P.op("act", Q.activation(out=o, in_=pt[:, 0:n], func=AF.Copy),
                     reads=[pk], writes=dst_keys)
            else:
                evac(b, n, pt, pk, o)

    def tap(name, s):
        if name in tap_d and s == 0:
            P.op("sp", Q.dma_start(out=tap_d[name].rearrange("(c p) d -> p c d", p=128), in_=x[:]),
                 reads=[K("x", c) for c in range(NCH)], dma=True)

    SC_BANKS = (2, 3, 4, 0, 1)
    sc_rot = [0]

    def attn_scores(pb, qb, qT, kT, chunks, eb_fn, Pt, Pt_key, etmp, qkey, kkey):
        nk = len(chunks)
        ngr = (nk + 1) // 2
        for gi in range(ngr):
            bk = SC_BANKS[sc_rot[0] % len(SC_BANKS)]
            sc_rot[0] += 1
            pt = PB[bk]
            pk = K("pb", bk)
            sub = chunks[gi * 2: gi * 2 + 2]
            for j, kc in enumerate(sub):
                P.op("pe", Q.matmul(
                    pt[:, j * 256:(j + 1) * 256], kT[pb:pb + 64, kc * 128:(kc + 1) * 128],
                    qT[pb:pb + 64, qb * 256:(qb + 1) * 256], start=True, stop=True),
                    reads=[qkey, kkey], writes=[pk])
            w = 256 * len(sub)
            dst = Pt[:, gi * 2: gi * 2 + len(sub), :].rearrange("p a b -> p (a b)")
            eb = eb_fn(gi * 2, len(sub)) if eb_fn is not None else None
            if eb is None:
                P.op("act", Q.activation(out=dst, in_=pt[:, 0:w], func=AF.Exp, scale=0.125),
                     reads=[pk], writes=[K(Pt_key, gi)])
            else:
                et = etmp[gi % 2]
                ek = K("etmp", gi % 2)
                P.op("act", Q.activation(out=et[:, 0:w], in_=pt[:, 0:w], func=AF.Exp, scale=0.125),
                     reads=[pk], writes=[ek])
                P.op("dve", Q.tensor_tensor(out=dst, in0=et[:, 0:w], in1=eb, op=ALU.mult),
                     reads=[ek, K("big16")], writes=[K(Pt_key, gi)])

    def attn_pv(qb, chunks, v_fn, dv, Pt, Pt_key, out_cb, vkey):
        nk = len(chunks)
        for qi in range(2):
            qc = qb * 2 + qi
            pt = PB[5]
            pk = K("pb", 5)
            for i, kc in enumerate(chunks):
                P.op("pe", Q.matmul(
                    pt[:, 0:dv + 1], Pt[:, i, qi * 128:(qi + 1) * 128], v_fn(kc), start=(i == 0), stop=(i == nk - 1)),
                    reads=[K(Pt_key, i // 2), vkey], writes=[pk])
            out_cb(qc, pt, pk)

    def attn_pipeline(blocks_list):
        pend = None
        for sa, pa_ in blocks_list:
            attn_scores(*sa)
            if pend is not None:
                attn_pv(*pend)
            pend = pa_
        if pend is not None:
            attn_pv(*pend)

    try:
        XK = [K("x", c) for c in range(NCH)]
        for s in range(S):
            P.op("sp", Q.dma_start(out=x[:], in_=x_d[s].rearrange("(c p) d -> p c d", p=128)),
                 writes=XK, dma=True)
            stage(0)
            for l in range(2):
                make_gate(l, 0, s)
                with Phase() as ph:
                    def pa(name, shape, dt):
                        return ph.enter_context(nc.sbuf_tensor(un(name), list(shape), dt))
                    hT = pa("hT", [128, 8, SEQ], BF16)
                    wo = (pa("wo_f", [128, D], F32), pa("wo_b", [128, D], BF16))
                    yT = pa("yT", [128, SEQ], BF16)
                    if l == 0:
                        Pt = [pa(f"Pt{k}", [128, 18, 256], BF16) for k in range(2)]
                        xsg = Pt[0][:, 0:16, :].rearrange("p (a b) c -> p a (b c)", a=4)
                    else:
                        xsg = pa("xsg", [128, 4, D], BF16)
                    make_AB(l, 0, s, 0)
                    norm_T(hT, "hT", NCH, lambda c: x[:, c, :], lambda c: [K("x", c)], xsg)
                    if l == 0:
                        hcT = pa("hcT", [128, 8, CTX], BF16)
                        big16 = pa("big16", [128, 2, SEQ], F32)
                        cx = big16[:, 0, :].rearrange("p (a b) -> p a b", a=2)
                        P.op("sp", Q.dma_start(out=cx, in_=ctx_d[s].rearrange("(c p) d -> p c d", p=128)),
                             writes=[K("big16")], dma=True)
                        make_AB(l, 0, S, 0)
                        for j in range(2):
                            rms_xs(cx[:, j, :], j, xsg[:, j, :], [K("big16")], [K("xsg", j)])
                        for dc in range(8):
                            pt = PBb[dc % 2]
                            pk = K("pb", dc % 2)
                            for j in range(2):
                                P.op("pe", Q.transpose(
                                    pt[:, j * 128:(j + 1) * 128], xsg[:, j, dc * 128:(dc + 1) * 128], ident_b[:]),
                                    reads=[K("xsg", j), K("ident_b")], writes=[pk])
                            P.op("act", Q.activation(
                                out=hcT[:, dc, :], in_=pt[:, 0:256], func=AF.Identity,
                                scale=AB[:, 0, dc:dc + 1], bias=AB[:, 1, dc:dc + 1]),
                                reads=[pk, K("AB")], writes=[K("hcT", 0)])
                        P.barrier()
                        stage(1)
                        wu = pa("wu", [128, 8, 640], BF16)
                        qT = pa("qT", [128, SEQ], BF16)
                        kT = pa("kT", [128, SEQ + CTX], BF16)
                        vv = pa("vv", [128, 18, 130], BF16)
                        etmp = [pa(f"etmp{k}", [128, 512], F32) for k in range(2)]
                        EB = big16[:].rearrange("p a b -> p (a b)")[:, 0:14 * 256].rearrange("p (a b) -> p a b", a=14)
                        rope = big16
                        ytm = pa("ytm", [128, NCH, 128], BF16)
                        tmpA = pa("tmpA", [128, 512], F32)
                        tmpB = pa("tmpB", [128, 512], F32)
                        lamv = pa("lamv", [128, 4, 64], F32)
                        lam = pa("lam", [128, 4], F32)
                        subl = pa("subl", [128, 128], F32)
                        o0 = pa("o0", [128, 2, 129], F32)
                        P.op("sp", Q.dma_start(out=lamv[:].rearrange("p a b -> p (a b)"), in_=L[0]["lam"].partition_broadcast(128)),
                             writes=[K("lamv")], dma=True)
                        P.op("sp", Q.dma_start(out=subl[:], in_=L[0]["subln"].partition_broadcast(128)),
                             writes=[K("subl")], dma=True)
                        lam_init = 0.8 - 0.6 * math.exp(-0.3 * 0)
                        for j in range(2):
                            P.op("dve", Q.tensor_tensor(out=tmpA[:, 0:64], in0=lamv[:, 2 * j, :], in1=lamv[:, 2 * j + 1, :], op=ALU.mult),
                                 reads=[K("lamv")], writes=[K("tmpA")])
                            P.op("dve", Q.reduce_sum(out=lam[:, j:j + 1], in_=tmpA[:, 0:64], axis=AX.X),
                                 reads=[K("tmpA")], writes=[K("lam")])
                        P.op("act", Q.activation(out=lam[:, 0:2], in_=lam[:, 0:2], func=AF.Exp), reads=[K("lam")], writes=[K("lam")])
                        P.op("dve", Q.tensor_tensor(out=lam[:, 2:3], in0=lam[:, 0:1], in1=lam[:, 1:2], op=ALU.subtract),
                             reads=[K("lam")], writes=[K("lam")])
                        P.op("dve", Q.tensor_scalar(out=lam[:, 3:4], in0=lam[:, 2:3], scalar1=lam_init, scalar2=-1.0, op0=ALU.add, op1=ALU.mult),
                             reads=[K("lam")], writes=[K("lam")])
                        P.op("dve", Q.tensor_scalar(out=subl[:], in0=subl[:], scalar1=1.0 - lam_init, scalar2=None, op0=ALU.mult),
                             reads=[K("subl")], writes=[K("subl")])
                        blocks = _na_blocks()
                        for u in range(8):
                            is_na = u < 4
                            ncol = 384 if is_na else 640
                            wsrc = (L[0]["w_na"][u] if is_na else L[0]["w_df"][u - 4]).rearrange("(kc p) n -> p kc n", p=128)
                            P.op("pool", Q.dma_start(out=wu[:, :, 0:ncol], in_=wsrc),
                                 writes=[K("wu")], dma=True)
                            if is_na:
                                proj_T(qT, [K("qT")], wu, K("wu"), 0, hT, "hT", SEQ)
                                proj_T(kT, [K("kT")], wu, K("wu"), 128, hT, "hT", SEQ)
                                proj_T(kT, [K("kT")], wu, K("wu"), 128, hcT, "hcT", CTX, dst0=SEQ)
                                vcol, dv = 256, 64
                            else:
                                if u == 4:
                                    P.op("sp", Q.dma_start(out=rope[:], in_=rope_d.rearrange("a p t -> p a t")),
                                         writes=[K("big16")], dma=True)
                                for (dst, dkey, c1, c2) in ((qT, K("qT"), 0, 128), (kT, K("kT"), 256, 384)):
                                    for b in range(4):
                                        for (pi, cc) in ((0, c1), (1, c2)):
                                            for kc in range(8):
                                                P.op("pe", Q.matmul(
                                                    PB[pi][:], wu[:, kc, cc:cc + 128], hT[:, kc, b * 512:(b + 1) * 512],
                                                    start=(kc == 0), stop=(kc == 7)),
                                                    reads=[K("wu"), K("hT", b)], writes=[K("pb", pi)])
                                        P.op("dve", Q.tensor_tensor(out=tmpA[:], in0=PB[0][:], in1=rope[:, 0, b * 512:(b + 1) * 512], op=ALU.mult),
                                             reads=[K("pb", 0), K("big16")], writes=[K("tmpA")])
                                        P.op("dve", Q.tensor_tensor(out=tmpB[:], in0=PB[1][:], in1=rope[:, 1, b * 512:(b + 1) * 512], op=ALU.mult),
                                             reads=[K("pb", 1), K("big16")], writes=[K("tmpB")])
                                        P.op("dve", Q.tensor_tensor(out=dst[:, b * 512:(b + 1) * 512], in0=tmpA[:], in1=tmpB[:], op=ALU.add),
                                             reads=[K("tmpA"), K("tmpB")], writes=[dkey])
                                proj_T(kT, [K("kT")], wu, K("wu"), 256, hcT, "hcT", CTX, dst0=SEQ)
                                vcol, dv = 512, 128
                            for c in range(18):
                                src = hT[:, :, c * 128:(c + 1) * 128] if c < 16 else hcT[:, :, (c - 16) * 128:(c - 15) * 128]
                                srck = K("hT", c // 4) if c < 16 else K("hcT", 0)
                                pt = PB[c % 2]
                                pk = K("pb", c % 2)
                                for kc in range(8):
                                    P.op("pe", Q.matmul(
                                        pt[:, 0:128], src[:, kc, :], wu[:, kc, vcol:vcol + 128], start=(kc == 0), stop=(kc == 7)),
                                        reads=[K("wu"), srck], writes=[pk])
                                if is_na:
                                    P.op("act", Q.activation(
                                        out=vv[:, c, :].rearrange("p (h d) -> p h d", h=2)[:, :, 0:64],
                                        in_=pt[:, 0:128].rearrange("p (h d) -> p h d", h=2), func=AF.Copy),
                                        reads=[pk], writes=[K("vv")])
                                else:
                                    P.op("act", Q.activation(out=vv[:, c, 0:128], in_=pt[:, 0:128], func=AF.Copy),
                                         reads=[pk], writes=[K("vv")])
                            if is_na:
                                P.op("dve", Q.memset(vv[:].rearrange("p c (h d) -> p c h d", h=2)[:, :, :, 64:65], 1.0),
                                     writes=[K("vv")])
                            else:
                                P.op("dve", Q.memset(vv[:, :, 128:129], 1.0), writes=[K("vv")])
                            if is_na:
                                for hh in range(2):
                                    head = 2 * u + hh
                                    P.op("sp", Q.dma_start(
                                        out=EB.rearrange("p a b -> p (a b)"), in_=L[0]["na_bias"][head]),
                                        writes=[K("big16")], dma=True)
                                    for a in range(2):
                                        P.op("act", Q.activation(out=EB[:, a * 7:(a + 1) * 7, :], in_=EB[:, a * 7:(a + 1) * 7, :], func=AF.Exp),
                                             reads=[K("big16")], writes=[K("big16")])

                                    def ocb(qc, pt, pk, hh=hh):
                                        P.op("dve", Q.reciprocal(out=small[:, 2:3], in_=pt[:, 64:65]),
                                             reads=[pk], writes=[K("small")])
                                        P.op("dve", Q.tensor_scalar(out=ytm[:, qc, hh * 64:(hh + 1) * 64], in0=pt[:, 0:64],
                                                                              scalar1=small[:, 2:3], scalar2=None, op0=ALU.mult),
                                             reads=[pk, K("small")], writes=[K("ytm", qc)])

                                    bl = []
                                    for qb in range(8):
                                        nb = len(blocks[qb])
                                        off = NA_CLS_OFF[_na_class(qb)]

                                        def ebf(i0, n, nb=nb, off=off):
                                            if i0 >= nb:
                                                return None
                                            assert i0 + n <= nb
                                            return EB[:, off + i0:off + i0 + n, :].rearrange("p a b -> p (a b)")

                                        ch = blocks[qb] + [16, 17]
                                        pi = qb % 2
                                        vf = (lambda kc, hh=hh: vv[:, kc, hh * 65:(hh + 1) * 65])
                                        bl.append(((64 * hh, qb, qT, kT, ch, ebf, Pt[pi], ("Pt", pi), etmp, K("qT"), K("kT")),
                                                   (qb, ch, vf, 64, Pt[pi], ("Pt", pi), ocb, K("vv"))))
                                    attn_pipeline(bl)
                            else:
                                def ocb0(qc, pt, pk):
                                    P.op("act", Q.activation(out=o0[:, qc % 2, :], in_=pt[:, 0:129], func=AF.Copy),
                                         reads=[pk], writes=[K("o0", qc % 2)])

                                def ocb1(qc, pt, pk):
                                    r0, r1, ssq, rs = small[:, 4:5], small[:, 5:6], small[:, 6:7], small[:, 7:8]
                                    ok = K("o0", qc % 2)
                                    P.op("dve", Q.reciprocal(out=r0, in_=o0[:, qc % 2, 128:129]), reads=[ok], writes=[K("small")])
                                    P.op("dve", Q.reciprocal(out=r1, in_=pt[:, 128:129]), reads=[pk], writes=[K("small")])
                                    P.op("dve", Q.tensor_tensor(out=r1, in0=r1, in1=lam[:, 3:4], op=ALU.mult),
                                         reads=[K("small"), K("lam")], writes=[K("small")])
                                    P.op("dve", Q.tensor_scalar(out=tmpA[:, 0:128], in0=o0[:, qc % 2, 0:128], scalar1=r0, scalar2=None, op0=ALU.mult),
                                         reads=[ok, K("small")], writes=[K("tmpA")])
                                    P.op("dve", Q.scalar_tensor_tensor(out=tmpA[:, 0:128], in0=pt[:, 0:128], scalar=r1, in1=tmpA[:, 0:128],
                                                                                 op0=ALU.mult, op1=ALU.add),
                                         reads=[pk, K("small"), K("tmpA")], writes=[K("tmpA")])
                                    P.op("act", Q.activation(out=tmpB[:, 0:128], in_=tmpA[:, 0:128], func=AF.Square),
                                         reads=[K("tmpA")], writes=[K("tmpB")])
                                    P.op("dve", Q.reduce_sum(out=ssq, in_=tmpB[:, 0:128], axis=AX.X), reads=[K("tmpB")], writes=[K("small")])
                                    P.op("dve", Q.tensor_scalar(out=rs, in0=ssq, scalar1=1.0 / 128, scalar2=EPS, op0=ALU.mult, op1=ALU.add),
                                         reads=[K("small")], writes=[K("small")])
                                    P.op("act", Q.activation(out=rs, in_=rs, func=AF.Sqrt), reads=[K("small")], writes=[K("small")])
                                    P.op("dve", Q.reciprocal(out=rs, in_=rs), reads=[K("small")], writes=[K("small")])
                                    P.op("dve", Q.scalar_tensor_tensor(out=ytm[:, qc, :], in0=tmpA[:, 0:128], scalar=rs, in1=subl[:],
                                                                                 op0=ALU.mult, op1=ALU.mult),
                                         reads=[K("tmpA"), K("small"), K("subl")], writes=[K("ytm", qc)])

                                bl = []
                                ch = list(range(18))
                                vf = (lambda kc: vv[:, kc, 0:129])
                                for qb in range(8):
                                    for m in range(2):
                                        bl.append(((64 * m, qb, qT, kT, ch, None, Pt[m], ("Pt", m), etmp, K("qT"), K("kT")),
                                                   (qb, ch, vf, 128, Pt[m], ("Pt", m), ocb0 if m == 0 else ocb1, K("vv"))))
                                attn_pipeline(bl)
                            for g in range(4):
                                pt = PBb[g % 2]
                                pk = K("pb", g % 2)
                                for j in range(4):
                                    c = g * 4 + j
                                    P.op("pe", Q.transpose(pt[:, j * 128:(j + 1) * 128], ytm[:, c, :], ident_b[:]),
                                         reads=[K("ytm", c), K("ident_b")], writes=[pk])
                                P.op("act", Q.activation(out=yT[:, g * 512:(g + 1) * 512], in_=pt[:, 0:512], func=AF.Copy),
                                     reads=[pk], writes=[K("yT")])
                            out_proj_unit(l, u, yT, "yT", wo)
                            stage(20 + u)
                    else:
                        P.barrier()
                        wu = pa("wu", [128, 8, 384], BF16)
                        scw = pa("scw", [128, 4, 3], F32)
                        cfw = pa("cfw", [128, 4, 31], F32)
                        cfv = pa("cfv", [128, 3, 4], F32)
                        P.op("sp", Q.dma_start(out=scw[:], in_=L[1]["sc_w"]), writes=[K("scw")], dma=True)
                        P.op("sp", Q.dma_start(out=cfw[:], in_=L[1]["cf_w"]), writes=[K("cfw")], dma=True)
                        P.op("sp", Q.dma_start(out=cfv[:], in_=L[1]["cf_vec"]), writes=[K("cfv")], dma=True)
                        va = pa("va", [128, SEQ + 32], F32)
                        vb = pa("vb", [128, SEQ], F32)
                        vc = pa("vc", [128, SEQ], F32)
                        ud = pa("ud", [128, 4, SEQ], F32)
                        P.op("dve", Q.memset(va[:], 0.0), writes=[K("va")])
                        for j in range(4):
                            P.op("pool", Q.dma_start(out=wu[:, :, 0:384], in_=L[1]["w_c"][j].rearrange("(kc p) n -> p kc n", p=128)),
                                 writes=[K("wu")], dma=True)
                            proj_T(vb, [K("vb")], wu, K("wu"), 0, hT, "hT", SEQ)

                            def ev_gc(b, n, pt, pk, o):
                                P.op("dve", Q.tensor_tensor(out=o, in0=pt[:, 0:n], in1=vb[:, b * 512:b * 512 + n], op=ALU.mult),
                                     reads=[pk, K("vb")], writes=[K("va")])
                            proj_T(va, [K("va")], wu, K("wu"), 256, hT, "hT", SEQ, evac=ev_gc, dst0=1)
                            proj_T(vc, [K("vc")], wu, K("wu"), 128, hT, "hT", SEQ)
                            P.op("dve", Q.tensor_scalar(out=vb[:], in0=va[:, 0:SEQ], scalar1=scw[:, j, 0:1], scalar2=None, op0=ALU.mult),
                                 reads=[K("va"), K("scw")], writes=[K("vb")])
                            for k in (1, 2):
                                P.op("dve", Q.scalar_tensor_tensor(out=vb[:], in0=va[:, k:k + SEQ], scalar=scw[:, j, k:k + 1], in1=vb[:],
                                                                                       op0=ALU.mult, op1=ALU.add),
                                     reads=[K("va"), K("scw"), K("vb")], writes=[K("vb")])
                            P.op("dve", Q.tensor_tensor(out=yT[:], in0=vb[:], in1=vc[:], op=ALU.mult),
                                 reads=[K("vb"), K("vc")], writes=[K("yT")])
                            out_proj_unit(l, j, yT, "yT", wo)
                        P.op("dve", Q.memset(va[:], 0.0), reads=[K("va")], writes=[K("va")])
                        for j in range(4):
                            P.op("pool", Q.dma_start(out=wu[:, :, 0:256], in_=L[1]["w_d"][j].rearrange("(kc p) n -> p kc n", p=128)),
                                 writes=[K("wu")], dma=True)

                            def ev_sig(b, n, pt, pk, o):
                                P.op("act", Q.activation(out=o, in_=pt[:, 0:n], func=AF.Sigmoid), reads=[pk], writes=[K("vb")])
                            proj_T(vb, [K("vb")], wu, K("wu"), 128, hT, "hT", SEQ, evac=ev_sig)

                            def ev_glu(b, n, pt, pk, o):
                                P.op("dve", Q.tensor_tensor(out=o, in0=pt[:, 0:n], in1=vb[:, b * 512:b * 512 + n], op=ALU.mult),
                                     reads=[pk, K("vb")], writes=[K("va")])
                            proj_T(va, [K("va")], wu, K("wu"), 0, hT, "hT", SEQ, evac=ev_glu, dst0=15)
                            P.op("dve", Q.tensor_scalar(out=ud[:, j, :], in0=va[:, 0:SEQ], scalar1=cfw[:, j, 0:1], scalar2=cfv[:, 0, j:j + 1],
                                                                       op0=ALU.mult, op1=ALU.add),
                                 reads=[K("va"), K("cfw"), K("cfv")], writes=[K("ud", j)])
                            for k in range(1, 31):
                                P.op("dve", Q.scalar_tensor_tensor(out=ud[:, j, :], in0=va[:, k:k + SEQ], scalar=cfw[:, j, k:k + 1], in1=ud[:, j, :],
                                                                                       op0=ALU.mult, op1=ALU.add),
                                     reads=[K("va"), K("cfw"), K("ud", j)], writes=[K("ud", j)])
                        for b in range(4):
                            sl = slice(b * 512, (b + 1) * 512)
                            for j in range(4):
                                P.op("pe", Q.matmul(PB[2][:], ones_f[:], ud[:, j, sl], start=(j == 0), stop=(j == 3)),
                                     reads=[K("ones_f"), K("ud", j)], writes=[K("pb", 2)])
                            for j in range(4):
                                P.op("act", Q.activation(out=vb[:, j * 512:(j + 1) * 512], in_=ud[:, j, sl], func=AF.Square),
                                     reads=[K("ud", j)], writes=[K("vb")])
                            for j in range(4):
                                P.op("pe", Q.matmul(PB[3][:], ones_f[:], vb[:, j * 512:(j + 1) * 512], start=(j == 0), stop=(j == 3)),
                                     reads=[K("ones_f"), K("vb")], writes=[K("pb", 3)])
                            mean, rstd, t0, t1 = vc[:, 0:512], vc[:, 512:1024], vc[:, 1024:1536], vc[:, 1536:2048]
                            P.op("dve", Q.tensor_scalar(out=mean, in0=PB[2][:], scalar1=1.0 / 512, scalar2=None, op0=ALU.mult),
                                 reads=[K("pb", 2)], writes=[K("vc")])
                            P.op("dve", Q.tensor_tensor(out=t0, in0=mean, in1=mean, op=ALU.mult), reads=[K("vc")], writes=[K("vc")])
                            P.op("dve", Q.scalar_tensor_tensor(out=rstd, in0=PB[3][:], scalar=1.0 / 512, in1=t0, op0=ALU.mult, op1=ALU.subtract),
                                 reads=[K("pb", 3), K("vc")], writes=[K("vc")])
                            P.op("dve", Q.tensor_scalar(out=rstd, in0=rstd, scalar1=EPS, scalar2=None, op0=ALU.add), reads=[K("vc")], writes=[K("vc")])
                            P.op("act", Q.activation(out=rstd, in_=rstd, func=AF.Sqrt), reads=[K("vc")], writes=[K("vc")])
                            P.op("dve", Q.reciprocal(out=rstd, in_=rstd), reads=[K("vc")], writes=[K("vc")])
                            for j in range(4):
                                P.op("dve", Q.tensor_tensor(out=t1, in0=ud[:, j, sl], in1=mean, op=ALU.subtract),
                                     reads=[K("ud", j), K("vc")], writes=[K("vc")])
                                P.op("dve", Q.tensor_tensor(out=t1, in0=t1, in1=rstd, op=ALU.mult), reads=[K("vc")], writes=[K("vc")])
                                P.op("act", Q.activation(out=t1, in_=t1, func=AF.Identity, scale=cfv[:, 1, j:j + 1], bias=cfv[:, 2, j:j + 1]),
                                     reads=[K("vc"), K("cfv")], writes=[K("vc")])
                                P.op("act", Q.activation(out=vb[:, 0:512], in_=t1, func=AF.Sigmoid), reads=[K("vc")], writes=[K("vb")])
                                P.op("dve", Q.tensor_tensor(out=ud[:, j, sl], in0=t1, in1=vb[:, 0:512], op=ALU.mult),
                                     reads=[K("vc"), K("vb")], writes=[K("ud", j)])
                        for j in range(4):
                            P.op("act", Q.activation(out=yT[:], in_=ud[:, j, :], func=AF.Copy), reads=[K("ud", j)], writes=[K("yT")])
                            out_proj_unit(l, 4 + j, yT, "yT", wo)
                P.barrier()
                tap(f"mix{l}", s)
                stage(3 if l == 0 else 6)

                make_gate(l, 1, s)
                with Phase() as ph:
                    def pa(name, shape, dt):
                        return ph.enter_context(nc.sbuf_tensor(un(name), list(shape), dt))
                    xs2 = pa("xs2", [128, NCH, D], BF16)
                    wr = pa("wr", [128, 8, 16], BF16)
                    aff_tm = pa("aff_tm", [128, NCH, 16], F32)
                    mask_f = pa("mask_f", [128, NCH, 16], F32)
                    mask_b = pa("mask_b", [128, NCH, 16], BF16)
                    pos_tm = pa("pos_tm", [128, NCH, 16], F32)
                    R = pa("R", [128, NCH, 16, 4], BF16)
                    hi_f = pa("hi_f", [128, NCH, 16], F32)
                    P.op("pool", Q.dma_start(out=wr[:], in_=L[l]["w_router"].rearrange("(kc p) n -> p kc n", p=128)),
                         writes=[K("wr")], dma=True)
                    make_AB(l, 1, s, 1)
                    with Phase() as ph2:
                        def pb2(name, shape, dt):
                            return ph2.enter_context(nc.sbuf_tensor(un(name), list(shape), dt))
                        hTg = pb2("hTg", [128, 8, 512], BF16)
                        expT = pb2("expT", [16, SEQ], F32)
                        affT = pb2("affT", [16, SEQ], F32)
                        work = pb2("work", [16, SEQ], F32)
                        maskT = pb2("maskT", [16, SEQ], F32)
                        m8 = pb2("m8", [16, 8], F32)
                        thr = pb2("thr", [16, 1], F32)
                        for g in range(4):
                            for j in range(4):
                                c = g * 4 + j
                                rms_xs(x[:, c, :], c, xs2[:, c, :], [K("x", c)], [K("xs2", c)])
                            for dc in range(8):
                                pt = PBb[dc % 2]
                                pk = K("pb", dc % 2)
                                for j in range(4):
                                    c = g * 4 + j
                                    P.op("pe", Q.transpose(pt[:, j * 128:(j + 1) * 128], xs2[:, c, dc * 128:(dc + 1) * 128], ident_b[:]),
                                         reads=[K("xs2", c), K("ident_b")], writes=[pk])
                                P.op("act", Q.activation(out=hTg[:, dc, :], in_=pt[:, 0:512], func=AF.Identity,
                                                                                 scale=AB[:, 0, dc:dc + 1], bias=AB[:, 1, dc:dc + 1]),
                                     reads=[pk, K("AB")], writes=[K("hTg")])
                            for kc in range(8):
                                P.op("pe", Q.matmul(PB[2][0:16, :], wr[:, kc, :], hTg[:, kc, :], start=(kc == 0), stop=(kc == 7)),
                                     reads=[K("wr"), K("hTg")], writes=[K("pb", 2)])
                            P.op("act", Q.activation(out=expT[:, g * 512:(g + 1) * 512], in_=PB[2][0:16, :], func=AF.Exp),
                                 reads=[K("pb", 2)], writes=[K("expT")])
                            for j in range(4):
                                for kc in range(8):
                                    P.op("pe", Q.matmul(PB[3][:, j * 16:(j + 1) * 16], hTg[:, kc, j * 128:(j + 1) * 128], wr[:, kc, :],
                                                                              start=(kc == 0), stop=(kc == 7)),
                                         reads=[K("wr"), K("hTg")], writes=[K("pb", 3)])
                            P.op("act", Q.activation(out=aff_tm[:, g * 4:(g + 1) * 4, :].rearrange("p a b -> p (a b)"), in_=PB[3][:, 0:64], func=AF.Exp),
                                 reads=[K("pb", 3)], writes=[K("aff_tm")])
                        stage(41 if l == 0 else 410)
                        for b in range(4):
                            P.op("pe", Q.matmul(PB[2][0:16, :], ones_f[0:16, 0:16], expT[:, b * 512:(b + 1) * 512], start=True, stop=True),
                                 reads=[K("ones_f"), K("expT")], writes=[K("pb", 2)])
                            P.op("dve", Q.reciprocal(out=work[:, b * 512:(b + 1) * 512], in_=PB[2][0:16, :]),
                                 reads=[K("pb", 2)], writes=[K("work")])
                        P.op("dve", Q.tensor_tensor(out=affT[:], in0=expT[:], in1=work[:], op=ALU.mult),
                             reads=[K("expT"), K("work")], writes=[K("affT")])
                        stage(42 if l == 0 else 420)
                        P.op("dve", Q.tensor_copy(out=work[:], in_=affT[:]), reads=[K("affT")], writes=[K("work")])
                        for r in range(CAP // 8):
                            P.op("dve", Q.max(out=m8[:], in_=work[:]), reads=[K("work")], writes=[K("m8")])
                            if r < CAP // 8 - 1:
                                P.op("dve", Q.match_replace(out=work[:], in_to_replace=m8[:], in_values=work[:], imm_value=-1.0),
                                     reads=[K("m8"), K("work")], writes=[K("work")])
                        P.op("dve", Q.tensor_reduce(out=thr[:], in_=m8[:], axis=AX.X, op=ALU.min), reads=[K("m8")], writes=[K("thr")])
                        P.op("dve", Q.tensor_scalar(out=maskT[:], in0=affT[:], scalar1=thr[:, 0:1], scalar2=None, op0=ALU.is_ge),
                             reads=[K("affT"), K("thr")], writes=[K("maskT")])
                        stage(43 if l == 0 else 430)
                        for c in range(NCH):
                            P.op("pe", Q.matmul(PB[0][:, c * 16:(c + 1) * 16], maskT[:, c * 128:(c + 1) * 128], ident_f[0:16, 0:16], start=True, stop=True),
                                 reads=[K("maskT"), K("ident_f")], writes=[K("pb", 0)])
                        stage(44)
                        P.op("act", Q.activation(out=mask_f[:].rearrange("p a b -> p (a b)"), in_=PB[0][:, 0:256], func=AF.Copy),
                             reads=[K("pb", 0)], writes=[K("mask_f")])
                        stage(45)
                        P.op("dve", Q.tensor_copy(out=mask_b[:], in_=mask_f[:]),
                             reads=[K("mask_f")], writes=[K("mask_b")])
                        stage(46)
                    P.barrier()
                    stage(4 if l == 0 else 40)
                    for c in range(NCH):
                        for c2 in range(c + 1):
                            lt = ltri_b if c2 == c else ones_b
                            P.op("pe", Q.matmul(PB[1][:, c * 16:(c + 1) * 16], lt[:], mask_b[:, c2, :], start=(c2 == 0), stop=(c2 == c)),
                                 reads=[K("ltri_b"), K("ones_b"), K("mask_b")], writes=[K("pb", 1)])
                    P.op("act", Q.activation(out=pos_tm[:].rearrange("p a b -> p (a b)"), in_=PB[1][:, 0:256], func=AF.Copy),
                         reads=[K("pb", 1)], writes=[K("pos_tm")])
                    P.op("dve", Q.reduce_sum(out=hi_f[:, :, 0], in_=aff_tm[:], axis=AX.X), reads=[K("aff_tm")], writes=[K("hi_f")])
                    P.op("dve", Q.reciprocal(out=hi_f[:, :, 1], in_=hi_f[:, :, 0]), reads=[K("hi_f")], writes=[K("hi_f")])
                    for c in range(NCH):
                        P.op("dve", Q.tensor_scalar(out=aff_tm[:, c, :], in0=aff_tm[:, c, :], scalar1=hi_f[:, c, 1:2], scalar2=None, op0=ALU.mult),
                             reads=[K("aff_tm"), K("hi_f")], writes=[K("aff_tm")])
                    P.op("dve", Q.tensor_copy(out=R[:, :, :, 0], in_=aff_tm[:]), reads=[K("aff_tm")], writes=[K("R")])
                    P.op("dve", Q.tensor_copy(out=hi_f[:], in_=R[:, :, :, 0]), reads=[K("R"), K("aff_tm")], writes=[K("hi_f")])
                    P.op("dve", Q.tensor_tensor(out=R[:, :, :, 1], in0=aff_tm[:], in1=hi_f[:], op=ALU.subtract),
                         reads=[K("aff_tm"), K("hi_f")], writes=[K("R")])
                    for q in range(2):
                        P.op("dve", Q.tensor_copy(out=R[:, :, :, 2 + q], in_=cidx[:, :, q:q + 1].to_broadcast([128, NCH, 16])),
                             reads=[K("cidx")], writes=[K("R")])

                    Se = pa("Se", [128, NCH, CAP], BF16)
                    SeT2 = [pa(f"SeT{k}", [128, 2, SEQ], BF16) for k in range(2)]
                    gi2 = [pa(f"gi{k}", [128, 2, 4], F32) for k in range(2)]
                    xinT = pa("xinT", [128, 8, CAP], BF16)
                    hidT = pa("hidT", [128, NFC, CAP], BF16)
                    sgt = pa("sgt", [128, CAP], F32)
                    yg2 = [pa(f"yg{k}", [128, 2, D], BF16) for k in range(2)]
                    w13 = [pa(f"w13_{k}", [128, 2, 8, 256], BF16) for k in range(3)]
                    w2b = [pa(f"w2_{k}", [128, 2, D], BF16) for k in range(3)]
                    wcount = [0, 0]

                    def emit_scatter(pb_, c_lo, c_hi):
                        for c in range(c_lo, c_hi):
                            for h in range(2):
                                bk = (c * 2 + h) % 2
                                for jc in range(2):
                                    P.op("pe", Q.matmul(
                                        PB[bk][:], SeT2[pb_][:, jc, c * 128:(c + 1) * 128], yg2[pb_][:, jc, h * 512:(h + 1) * 512],
                                        start=(jc == 0), stop=(jc == 1)),
                                        reads=[K("SeT", pb_, jc), K("yg", pb_)], writes=[K("pb", bk)])
                                P.op("dve", Q.tensor_tensor(
                                    out=x[:, c, h * 512:(h + 1) * 512], in0=PB[bk][:], in1=x[:, c, h * 512:(h + 1) * 512], op=ALU.add),
                                    reads=[K("pb", bk), K("x", c)], writes=[K("x", c)])

                    def build_Se(ex_):
                        for c in range(NCH):
                            P.op("dve", Q.tensor_scalar(out=Se[:, c, :], in0=iota_t[:, 0:CAP], scalar1=pos_tm[:, c, ex_:ex_ + 1],
                                                        scalar2=mask_f[:, c, ex_:ex_ + 1], op0=ALU.is_equal, op1=ALU.mult),
                                 reads=[K("iota_t"), K("pos_tm"), K("mask_f")], writes=[K("Se", c)])

                    build_Se(0)
                    for ex in range(E):
                        eb_ = ex % 2
                        SeT, gi, yg = SeT2[eb_], gi2[eb_], yg2[eb_]
                        for jc in range(2):
                            for c in range(NCH):
                                P.op("pe", Q.matmul(PB[0][:, jc * 4:(jc + 1) * 4], Se[:, c, jc * 128:(jc + 1) * 128], R[:, c, ex, :],
                                                                                start=(c == 0), stop=(c == NCH - 1)),
                                     reads=[K("Se", c), K("R")], writes=[K("pb", 0)])
                        P.op("act", Q.activation(out=gi[:].rearrange("p a b -> p (a b)"), in_=PB[0][:, 0:8], func=AF.Copy),
                             reads=[K("pb", 0)], writes=[K("gi", eb_)])
                        P.op("dve", Q.tensor_tensor(out=gi[:, :, 0], in0=gi[:, :, 0], in1=gi[:, :, 1], op=ALU.add), reads=[K("gi", eb_)], writes=[K("gi", eb_)])
                        P.op("dve", Q.scalar_tensor_tensor(out=gi[:, :, 2], in0=gi[:, :, 2], scalar=128.0, in1=gi[:, :, 3], op0=ALU.mult, op1=ALU.add),
                             reads=[K("gi", eb_)], writes=[K("gi", eb_)])
                        for jc in range(2):
                            P.op("dve", Q.tensor_scalar(out=SeT[:, jc, :], in0=iota_t[:], scalar1=gi[:, jc, 2:3], scalar2=None, op0=ALU.is_equal),
                                 reads=[K("iota_t"), K("gi", eb_)], writes=[K("SeT", eb_, jc)])
                        for dc in range(8):
                            bk = dc % 2
                            for c in range(NCH):
                                P.op("pe", Q.matmul(PB[bk][:, 0:CAP], xs2[:, c, dc * 128:(dc + 1) * 128], Se[:, c, :],
                                                                                start=(c == 0), stop=(c == NCH - 1)),
                                     reads=[K("xs2", c), K("Se", c)], writes=[K("pb", bk)])
                            P.op("act", Q.activation(out=xinT[:, dc, :], in_=PB[bk][:, 0:CAP], func=AF.Identity,
                                                                             scale=AB[:, 0, dc:dc + 1], bias=AB[:, 1, dc:dc + 1]),
                                 reads=[K("pb", bk), K("AB")], writes=[K("xinT")])
                        for fg in range(NFC // 2):
                            slot = wcount[0] % len(w13)
                            wcount[0] += 1
                            wt = w13[slot]
                            for wi, nm in enumerate(("w1", "w3")):
                                src = L[l][nm][ex][:, fg * 256:(fg + 1) * 256].rearrange("(kc p) n -> p kc n", p=128)
                                P.op("pool", Q.dma_start(out=wt[:, wi], in_=src), writes=[K("w13", slot, wi)], dma=True)
                            for f2 in range(2):
                                fc = fg * 2 + f2
                                bk = 2 + (fc % 2)
                                for wi in range(2):
                                    for kc in range(8):
                                        P.op("pe", Q.matmul(
                                            PB[bk][:, wi * CAP:(wi + 1) * CAP], wt[:, wi, kc, f2 * 128:(f2 + 1) * 128], xinT[:, kc, :],
                                            start=(kc == 0), stop=(kc == 7)),
                                            reads=[K("w13", slot, wi), K("xinT")], writes=[K("pb", bk)])
                                P.op("act", Q.activation(out=sgt[:], in_=PB[bk][:, 0:CAP], func=AF.Sigmoid), reads=[K("pb", bk)], writes=[K("sgt")])
                                P.op("dve", Q.tensor_tensor(out=sgt[:], in0=PB[bk][:, 0:CAP], in1=sgt[:], op=ALU.mult),
                                     reads=[K("pb", bk), K("sgt")], writes=[K("sgt")])
                                P.op("dve", Q.tensor_tensor(out=hidT[:, fc, :], in0=PB[bk][:, CAP:2 * CAP], in1=sgt[:], op=ALU.mult),
                                     reads=[K("pb", bk), K("sgt")], writes=[K("hidT")])
                            if ex > 0:
                                emit_scatter(1 - eb_, fg * NCH // (NFC // 2), (fg + 1) * NCH // (NFC // 2))
                        if ex + 1 < E:
                            build_Se(ex + 1)
                        for fg in range(NFC // 2):
                            slot = wcount[1] % len(w2b)
                            wcount[1] += 1
                            wt = w2b[slot]
                            src = L[l]["w2"][ex][fg * 256:(fg + 1) * 256, :].rearrange("(a p) n -> p a n", p=128)
                            P.op("pool", Q.dma_start(out=wt[:], in_=src), writes=[K("w2b", slot)], dma=True)
                            for f2 in range(2):
                                fc = fg * 2 + f2
                                for jc in range(2):
                                    for h in range(2):
                                        bk = 4 + jc * 2 + h
                                        P.op("pe", Q.matmul(
                                            PB[bk][:], hidT[:, fc, jc * 128:(jc + 1) * 128], wt[:, f2, h * 512:(h + 1) * 512],
                                            start=(fc == 0), stop=(fc == NFC - 1)),
                                            reads=[K("hidT"), K("w2b", slot)], writes=[K("pb", bk)])
                        for jc in range(2):
                            for h in range(2):
                                bk = 4 + jc * 2 + h
                                P.op("dve", Q.scalar_tensor_tensor(
                                    out=yg[:, jc, h * 512:(h + 1) * 512], in0=PB[bk][:], scalar=gi[:, jc, 0:1], in1=gate_b[:, h * 512:(h + 1) * 512],
                                    op0=ALU.mult, op1=ALU.mult), reads=[K("pb", bk), K("gi", eb_), K("gate_b")], writes=[K("yg", eb_)])
                    emit_scatter((E - 1) % 2, 0, NCH)
                P.barrier()
                tap(f"moe{l}", s)
                stage(5 if l == 0 else 7)

            with Phase() as ph:
                ob = [ph.enter_context(nc.sbuf_tensor(un(f"ob{k}"), [128, D], F32)) for k in range(2)]
                fnorm_b = ph.enter_context(nc.sbuf_tensor(un("fnorm_b"), [128, D], F32))
                P.op("sp", Q.dma_start(out=fnorm_b[:], in_=fnorm_d.partition_broadcast(128)), writes=[K("fnorm_b")], dma=True)
                for c in range(NCH):
                    o = ob[c % 2]
                    ok = K("ob", c % 2)
                    sl = c % 16
                    sq, rs = stat[:, 2 * sl:2 * sl + 1], stat[:, 2 * sl + 1:2 * sl + 2]
                    sk = K("stat", sl)
                    P.op("act", Q.activation(out=o[:], in_=x[:, c, :], func=AF.Square),
                         reads=[K("x", c)], writes=[ok])
                    P.op("dve", Q.reduce_sum(out=sq, in_=o[:], axis=AX.X), reads=[ok], writes=[sk])
                    P.op("dve", Q.tensor_scalar(out=rs, in0=sq, scalar1=1.0 / D, scalar2=EPS, op0=ALU.mult, op1=ALU.add),
                         reads=[sk], writes=[sk])
                    P.op("act", Q.activation(out=rs, in_=rs, func=AF.Sqrt), reads=[sk], writes=[sk])
                    P.op("dve", Q.reciprocal(out=rs, in_=rs), reads=[sk], writes=[sk])
                    P.op("dve", Q.scalar_tensor_tensor(out=o[:], in0=x[:, c, :], scalar=rs, in1=fnorm_b[:], op0=ALU.mult, op1=ALU.mult),
                         reads=[K("x", c), sk, K("fnorm_b")], writes=[ok])
                    P.op("sp", Q.dma_start(out=out_d[s, c * 128:(c + 1) * 128, :], in_=o[:]), reads=[ok], dma=True)
            P.barrier()
    except _Stop:
        P.barrier()
        P.op("sp", Q.dma_start(out=out_d[0].rearrange("(c p) d -> p c d", p=128), in_=x[:]), reads=[K("x", c) for c in range(NCH)], dma=True)
        P.barrier()

    P.emit(st)
    st.close()
    return nc


def _col(v):
    return np.ascontiguousarray(v.reshape(-1, 128).T)


def prep_shared(inp, E):
    f = np.float32
    sh = {}
    perm = _rope_perm()
    for l in range(2):
        p = f"l{l}_"
        sh[p + "normcols"] = np.ascontiguousarray(np.stack([_col(inp[p + "norm1"]), _col(inp[p + "norm2"])], 1)).astype(f)
        sh[p + "w_mod"] = np.ascontiguousarray(inp[p + "w_mod"])
        sh[p + "b_mod"] = np.ascontiguousarray(inp[p + "b_mod"].reshape(1, -1))
        sh[p + "w_out"] = np.ascontiguousarray(inp[p + "w_out"])
        sh[p + "w_router"] = np.ascontiguousarray(inp[p + "w_router"])
        sh[p + "w1"] = np.ascontiguousarray(inp[p + "w1"][:E])
        sh[p + "w3"] = np.ascontiguousarray(inp[p + "w3"][:E])
        sh[p + "w2"] = np.ascontiguousarray(inp[p + "w2"][:E])
    w = inp["l0_w_in"]
    aq, bq, ak, bk, av, bv = (w[:, i * 512:(i + 1) * 512] for i in range(6))
    na = [np.concatenate([aq[:, i * 128:(i + 1) * 128], ak[:, i * 128:(i + 1) * 128], av[:, i * 128:(i + 1) * 128]], 1) for i in range(4)]
    sh["l0_w_na"] = np.ascontiguousarray(np.stack(na, 0))
    pidx = np.concatenate([perm, 64 + perm])
    df = []
    for h in range(4):
        q = bq[:, h * 128:(h + 1) * 128]
        k = bk[:, h * 128:(h + 1) * 128]
        df.append(np.concatenate([q, q[:, pidx], k, k[:, pidx], bv[:, h * 128:(h + 1) * 128]], 1))
    sh["l0_w_df"] = np.ascontiguousarray(np.stack(df, 0))
    sh["l0_na_bias"] = np.ascontiguousarray(_na_bias_tables(inp["l0_rpb"]).reshape(8, 128, 14 * 256))
    sh["l0_lam"] = np.ascontiguousarray(np.concatenate([inp["l0_lam_q1"], inp["l0_lam_k1"], inp["l0_lam_q2"], inp["l0_lam_k2"]], 0).reshape(1, 256))
    sh["l0_subln"] = np.ascontiguousarray(inp["l0_subln"].reshape(1, 128))
    w = inp["l1_w_in"]
    xc, gb, gc, ga, gg = (w[:, i * 512:(i + 1) * 512] for i in range(5))
    sh["l1_w_c"] = np.ascontiguousarray(np.stack([np.concatenate([xc[:, j * 128:(j + 1) * 128], gb[:, j * 128:(j + 1) * 128], gc[:, j * 128:(j + 1) * 128]], 1) for j in range(4)], 0))
    sh["l1_w_d"] = np.ascontiguousarray(np.stack([np.concatenate([ga[:, j * 128:(j + 1) * 128], gg[:, j * 128:(j + 1) * 128]], 1) for j in range(4)], 0))
    sh["l1_sc_w"] = np.ascontiguousarray(inp["l1_sc_w"].T.reshape(4, 128, 3).transpose(1, 0, 2))
    sh["l1_cf_w"] = np.ascontiguousarray(inp["l1_cf_w"].T.reshape(4, 128, 31).transpose(1, 0, 2))
    sh["l1_cf_vec"] = np.ascontiguousarray(np.stack([_col(inp["l1_cf_b"]), _col(inp["l1_ln_g"]), _col(inp["l1_ln_b"])], 1))
    sh["final_norm"] = np.ascontiguousarray(inp["final_norm"].reshape(1, -1))
    cos, sin = _rope_tables()
    sh["rope"] = np.ascontiguousarray(np.stack([cos, sin], 0))
    import ml_dtypes
    ci = np.zeros((128, 16, 2), np.float32)
    ci[:, :, 0] = np.arange(16)[None, :]
    ci[:, :, 1] = np.arange(128)[:, None]
    sh["cidx"] = ci.astype(ml_dtypes.bfloat16)
    sh["ident"] = np.eye(128, dtype=f)
    sh["ltri"] = np.triu(np.ones((128, 128), f), 1)
    sh["iota"] = np.arange(SEQ, dtype=f).reshape(1, SEQ)
    return sh


def core_inputs(inp, sh, samples):
    S = len(samples)
    m = dict(sh)
    m["x"] = np.ascontiguousarray(inp["x"][samples])
    m["ctx"] = np.ascontiguousarray(inp["ctx"][samples])
    cc = np.concatenate([inp["c"][samples], inp["c_ctx"][None, :]], 0)
    m["cT"] = np.ascontiguousarray(cc.reshape(S + 1, 8, 128).transpose(2, 1, 0))
    return m


NCORES = 8
_cache = {}


def kernel(**inputs):
    inp = {k: np.asarray(v) for k, v in inputs.items()}
    B = inp["x"].shape[0]
    S = B // NCORES
    E = 16
    key = (S, E)
    if key not in _cache:
        _cache[key] = build(S, E)
    nc = _cache[key]
    sh = prep_shared(inp, E)
    in_maps = [core_inputs(inp, sh, list(range(c * S, (c + 1) * S))) for c in range(NCORES)]
    res = run_bass_kernel_spmd(nc, in_maps, core_ids=list(range(NCORES)))
    out = np.concatenate([res.results[c]["out"] for c in range(NCORES)], 0)
    return out.astype(np.float32)
```
